# Optimizing a Trainium2 kernel written in Bass

```python
import jax
import jax.numpy as jnp
from jax import lax
import numpy as np

D_MODEL = 1024
BATCH = 16
SEQ = 4096
DEPTH = 4

GRID_W = 64
CTX_LEN = 256
HEAD_DIM = 64
BLOCK = 128
ROPE_THETA = 10000.0
NEG_INF = -1e30
EPS = 1e-6

NA_HEADS = 4
NA_ROWS = 8
NA_COLS = 16
GQA_HEADS = 4
GQA_KV_HEADS = 2
WIN_HEADS = 4
WIN_KV_HEADS = 2
WINDOW = 128
MLA_HEADS = 4
MLA_Q_RANK = 256
MLA_KV_RANK = 128
MLA_NOPE = 64
MLA_ROPE = 32
MLA_V = 64

D_MIX = NA_HEADS * HEAD_DIM + GQA_HEADS * HEAD_DIM + WIN_HEADS * HEAD_DIM + MLA_HEADS * MLA_V
PROJ_SPLITS = ((NA_HEADS * HEAD_DIM,) * 3
               + (GQA_HEADS * HEAD_DIM, GQA_KV_HEADS * HEAD_DIM, GQA_KV_HEADS * HEAD_DIM)
               + (WIN_HEADS * HEAD_DIM, WIN_KV_HEADS * HEAD_DIM, WIN_KV_HEADS * HEAD_DIM)
               + (MLA_Q_RANK, MLA_KV_RANK, MLA_ROPE))
D_PROJ = sum(PROJ_SPLITS)
PROJ_OFFSETS = tuple(int(o) for o in np.cumsum(PROJ_SPLITS)[:-1])

N_GROUPS = 4
EXPERTS_PER_GROUP = 4
N_EXPERTS = N_GROUPS * EXPERTS_PER_GROUP
TOP_K = 2
D_EXPERT = 512

ALPHA = (2.0 * DEPTH) ** 0.25
BETA = (8.0 * DEPTH) ** -0.25

kernel_name = 'hybrid_dit_parallel_heads_hmoe'


def _layer_norm(x, g=None, b=None):
    xf = x.astype(jnp.float32)
    mu = jnp.mean(xf, -1, keepdims=True)
    var = jnp.mean(jnp.square(xf - mu), -1, keepdims=True)
    y = (xf - mu) * lax.rsqrt(var + EPS)
    if g is not None:
        y = y * g.astype(jnp.float32) + b.astype(jnp.float32)
    return y.astype(x.dtype)


def _rms_norm(x, g):
    xf = x.astype(jnp.float32)
    y = xf * lax.rsqrt(jnp.mean(xf * xf, -1, keepdims=True) + EPS) * g.astype(jnp.float32)
    return y.astype(x.dtype)


def _heads(z, n):
    return z.reshape(z.shape[0], z.shape[1], n, -1)


def _rope_1d(x, pos):
    d = x.shape[-1]
    freq = ROPE_THETA ** (-jnp.arange(0, d, 2, dtype=jnp.float32) / d)
    ang = pos.astype(jnp.float32)[:, None] * freq[None, :]
    cos = jnp.cos(ang)[:, None, :]
    sin = jnp.sin(ang)[:, None, :]
    x1 = x[..., : d // 2].astype(jnp.float32)
    x2 = x[..., d // 2:].astype(jnp.float32)
    return jnp.concatenate([x1 * cos - x2 * sin, x1 * sin + x2 * cos], -1).astype(x.dtype)


def _rope_2d(x, row, col):
    h = x.shape[-1] // 2
    return jnp.concatenate([_rope_1d(x[..., :h], row), _rope_1d(x[..., h:], col)], -1)


def _gqa_dense(q, k, v, sink=None):
    b, tq, hq, dk = q.shape
    hkv, dv = k.shape[2], v.shape[-1]
    g = hq // hkv
    scale = dk ** -0.5
    qb = q.reshape(b, tq // BLOCK, BLOCK, hkv, g, dk).transpose(1, 0, 2, 3, 4, 5)

    def one(qi):
        s = jnp.einsum('bqkgd,bskd->bkgqs', qi, k).astype(jnp.float32) * scale
        if sink is not None:
            s_sink = jnp.broadcast_to(sink.astype(jnp.float32).reshape(1, hkv, g, 1, 1), s.shape[:-1] + (1,))
            p = jax.nn.softmax(jnp.concatenate([s, s_sink], -1), -1)[..., :-1]
        else:
            p = jax.nn.softmax(s, -1)
        return jnp.einsum('bkgqs,bskd->bqkgd', p.astype(v.dtype), v)

    o = lax.map(one, qb)
    return o.transpose(1, 0, 2, 3, 4, 5).reshape(b, tq, hq * dv)


def _window_attn(q, k, v, kc, vc, sink):
    b, t, hq, dk = q.shape
    hkv, dv = k.shape[2], v.shape[-1]
    g = hq // hkv
    n_blk = t // BLOCK
    span = BLOCK + 2 * WINDOW
    scale = dk ** -0.5
    pad = ((0, 0), (WINDOW, WINDOW), (0, 0), (0, 0))
    kp = jnp.pad(k, pad)
    vp = jnp.pad(v, pad)
    k_rel = jnp.arange(span) - WINDOW
    band = jnp.abs(jnp.arange(BLOCK)[:, None] - k_rel[None, :]) <= WINDOW
    sink_l = sink.astype(jnp.float32).reshape(1, hkv, g, 1, 1)
    qb = q.reshape(b, n_blk, BLOCK, hkv, g, dk).transpose(1, 0, 2, 3, 4, 5)

    def one(args):
        n, qi = args
        start = n * BLOCK
        kw = lax.dynamic_slice_in_dim(kp, start, span, axis=1)
        vw = lax.dynamic_slice_in_dim(vp, start, span, axis=1)
        k_abs = start + k_rel
        mask = band & ((k_abs >= 0) & (k_abs < t))[None, :]
        s_loc = jnp.einsum('bqkgd,bskd->bkgqs', qi, kw).astype(jnp.float32) * scale
        s_loc = jnp.where(mask, s_loc, NEG_INF)
        s_ctx = jnp.einsum('bqkgd,blkd->bkgql', qi, kc).astype(jnp.float32) * scale
        s_sink = jnp.broadcast_to(sink_l, s_ctx.shape[:-1] + (1,))
        p = jax.nn.softmax(jnp.concatenate([s_loc, s_ctx, s_sink], -1), -1).astype(v.dtype)
        return (jnp.einsum('bkgqs,bskd->bqkgd', p[..., :span], vw)
                + jnp.einsum('bkgql,blkd->bqkgd', p[..., span:-1], vc))

    o = lax.map(one, (jnp.arange(n_blk), qb))
    return o.transpose(1, 0, 2, 3, 4, 5).reshape(b, t, hq * dv)


def _neighbourhood_attn(q, k, v, kc, vc, bias):
    b, t, h, d = q.shape
    rows = t // GRID_W
    kr = min(NA_ROWS, rows)
    nk = kr * NA_COLS
    scale = d ** -0.5
    cols = jnp.arange(GRID_W)
    c0 = jnp.clip(cols - NA_COLS // 2, 0, GRID_W - NA_COLS)
    key_col = c0[:, None, None] + jnp.arange(NA_COLS)[None, None, :]
    col_off = key_col - cols[:, None, None] + (NA_COLS - 1)
    dr = jnp.arange(kr)
    qb = q.reshape(b, rows, GRID_W, h, d).transpose(1, 0, 2, 3, 4)

    def one(args):
        r, qi = args
        r0 = jnp.clip(r - kr // 2, 0, rows - kr)
        key_row = r0 + dr
        idx = (key_row[None, :, None] * GRID_W + key_col).reshape(GRID_W, nk)
        rb = bias[:, (key_row - r + NA_ROWS - 1)[None, :, None], col_off].reshape(h, GRID_W, nk)
        kg = jnp.take(k, idx, axis=1)
        vg = jnp.take(v, idx, axis=1)
        s_loc = jnp.einsum('bqhd,bqkhd->bhqk', qi, kg).astype(jnp.float32) * scale + rb.astype(jnp.float32)[None]
        s_ctx = jnp.einsum('bqhd,blhd->bhql', qi, kc).astype(jnp.float32) * scale
        p = jax.nn.softmax(jnp.concatenate([s_loc, s_ctx], -1), -1).astype(v.dtype)
        return (jnp.einsum('bhqk,bqkhd->bqhd', p[..., :nk], vg)
                + jnp.einsum('bhql,blhd->bqhd', p[..., nk:], vc))

    o = lax.map(one, (jnp.arange(rows), qb))
    return o.transpose(1, 0, 2, 3, 4).reshape(b, t, h * d)


def _mla_q(cq, gain, w_qb):
    return _heads(_rms_norm(cq, gain) @ w_qb, MLA_HEADS)


def _mla_kv(ckv, gain, w_kvb):
    kv = _heads(_rms_norm(ckv, gain) @ w_kvb, MLA_HEADS)
    return kv[..., :MLA_NOPE], kv[..., MLA_NOPE:]


def _mla_key(k_nope, k_rope):
    return jnp.concatenate([k_nope, jnp.broadcast_to(k_rope, k_nope.shape[:3] + (MLA_ROPE,))], -1)


def _token_mix(h, hc, row, col, w_in, na_bias, gqa_q_gain, gqa_k_gain, win_sink,
               mla_q_gain, mla_w_qb, mla_kv_gain, mla_w_kvb, w_out, with_ctx):
    aq, ak, av, bq, bk, bv, cq, ck, cv, dq, dkv, dkr = jnp.split(h @ w_in, PROJ_OFFSETS, axis=-1)
    aqc, akc, avc, bqc, bkc, bvc, cqc, ckc, cvc, dqc, dkvc, dkrc = jnp.split(hc @ w_in, PROJ_OFFSETS, axis=-1)

    ka_c, va_c = _heads(akc, NA_HEADS), _heads(avc, NA_HEADS)
    out_a = _neighbourhood_attn(_heads(aq, NA_HEADS), _heads(ak, NA_HEADS), _heads(av, NA_HEADS),
                                ka_c, va_c, na_bias)

    qb = _rope_2d(_rms_norm(_heads(bq, GQA_HEADS), gqa_q_gain), row, col)
    kb = _rope_2d(_rms_norm(_heads(bk, GQA_KV_HEADS), gqa_k_gain), row, col)
    kb_c = _rms_norm(_heads(bkc, GQA_KV_HEADS), gqa_k_gain)
    vb_c = _heads(bvc, GQA_KV_HEADS)
    out_b = _gqa_dense(qb, jnp.concatenate([kb, kb_c], 1),
                       jnp.concatenate([_heads(bv, GQA_KV_HEADS), vb_c], 1))

    kc_c, vc_c = _heads(ckc, WIN_KV_HEADS), _heads(cvc, WIN_KV_HEADS)
    out_c = _window_attn(_rope_2d(_heads(cq, WIN_HEADS), row, col),
                         _rope_2d(_heads(ck, WIN_KV_HEADS), row, col),
                         _heads(cv, WIN_KV_HEADS), kc_c, vc_c, win_sink)

    qd = _mla_q(dq, mla_q_gain, mla_w_qb)
    qd = jnp.concatenate([qd[..., :MLA_NOPE], _rope_2d(qd[..., MLA_NOPE:], row, col)], -1)
    kn, vd = _mla_kv(dkv, mla_kv_gain, mla_w_kvb)
    kd = _mla_key(kn, _rope_2d(dkr[:, :, None, :], row, col))
    kn_c, vd_c = _mla_kv(dkvc, mla_kv_gain, mla_w_kvb)
    kd_c = _mla_key(kn_c, dkrc[:, :, None, :])
    out_d = _gqa_dense(qd, jnp.concatenate([kd, kd_c], 1), jnp.concatenate([vd, vd_c], 1))

    mix = jnp.concatenate([out_a, out_b, out_c, out_d], -1) @ w_out
    if not with_ctx:
        return mix, None
    mix_c = jnp.concatenate([
        _gqa_dense(_heads(aqc, NA_HEADS), ka_c, va_c),
        _gqa_dense(_rms_norm(_heads(bqc, GQA_HEADS), gqa_q_gain), kb_c, vb_c),
        _gqa_dense(_heads(cqc, WIN_HEADS), kc_c, vc_c, win_sink),
        _gqa_dense(_mla_q(dqc, mla_q_gain, mla_w_qb), kd_c, vd_c),
    ], -1) @ w_out
    return mix, mix_c


def _hier_moe(h, wg, bg, we, be, w_gate, w_up, w_down):
    shp = h.shape
    hf = h.reshape(-1, shp[-1])
    lg = (hf @ wg).astype(jnp.float32) + bg.astype(jnp.float32)
    grp = jnp.argmax(lg, -1)
    p_grp = jnp.max(jax.nn.softmax(lg, -1), -1, keepdims=True)
    le = ((hf @ we).astype(jnp.float32) + be.astype(jnp.float32)).reshape(-1, N_GROUPS, EXPERTS_PER_GROUP)
    le_sel = jnp.take_along_axis(le, grp[:, None, None], axis=1)[:, 0]
    top_v, top_i = lax.top_k(le_sel, TOP_K)
    w_top = jax.nn.softmax(top_v, -1) * p_grp
    eid = grp[:, None] * EXPERTS_PER_GROUP + top_i
    gate = jnp.einsum('nke,nk->ne', jax.nn.one_hot(eid, N_EXPERTS, dtype=jnp.float32), w_top)
    y = jnp.zeros(hf.shape, jnp.float32)
    for e in range(N_EXPERTS):
        a = jax.nn.silu(hf @ w_gate[e]) * (hf @ w_up[e])
        y = y + gate[:, e:e + 1] * (a @ w_down[e]).astype(jnp.float32)
    return y.astype(h.dtype).reshape(shp)


def setup_inputs(seed: int = 0) -> dict:
    key = jax.random.key(seed)
    ks = iter(jax.random.split(key, 32))

    def nrm(shape, s):
        return jax.random.normal(next(ks), shape, jnp.float32) * s

    L, D = DEPTH, D_MODEL
    return {
        'x': nrm((BATCH, SEQ, D), 1.0),
        'c': nrm((BATCH, D), 1.0),
        'ctx': nrm((BATCH, CTX_LEN, D), 1.0),
        'c_ctx': nrm((D,), 1.0),
        'w_ada': nrm((L, D, 6 * D), 0.5 * D ** -0.5),
        'b_ada': nrm((L, 6 * D), 0.02),
        'w_in': nrm((L, D, D_PROJ), D ** -0.5),
        'na_bias': nrm((L, NA_HEADS, 2 * NA_ROWS - 1, 2 * NA_COLS - 1), 0.1),
        'gqa_q_gain': 1.0 + nrm((L, HEAD_DIM), 0.02),
        'gqa_k_gain': 1.0 + nrm((L, HEAD_DIM), 0.02),
        'win_sink': nrm((L, WIN_HEADS), 0.5),
        'mla_q_gain': 1.0 + nrm((L, MLA_Q_RANK), 0.02),
        'mla_w_qb': nrm((L, MLA_Q_RANK, MLA_HEADS * (MLA_NOPE + MLA_ROPE)), MLA_Q_RANK ** -0.5),
        'mla_kv_gain': 1.0 + nrm((L, MLA_KV_RANK), 0.02),
        'mla_w_kvb': nrm((L, MLA_KV_RANK, MLA_HEADS * (MLA_NOPE + MLA_V)), MLA_KV_RANK ** -0.5),
        'w_out': nrm((L, D_MIX, D), BETA * D_MIX ** -0.5),
        'ln1_g': 1.0 + nrm((L, D), 0.02),
        'ln1_b': nrm((L, D), 0.02),
        'router_group_w': nrm((L, D, N_GROUPS), D ** -0.5),
        'router_group_b': nrm((L, N_GROUPS), 0.01),
        'router_expert_w': nrm((L, D, N_EXPERTS), D ** -0.5),
        'router_expert_b': nrm((L, N_EXPERTS), 0.01),
        'moe_w_gate': nrm((L, N_EXPERTS, D, D_EXPERT), D ** -0.5),
        'moe_w_up': nrm((L, N_EXPERTS, D, D_EXPERT), D ** -0.5),
        'moe_w_down': nrm((L, N_EXPERTS, D_EXPERT, D), BETA * D_EXPERT ** -0.5),
        'ln2_g': 1.0 + nrm((L, D), 0.02),
        'ln2_b': nrm((L, D), 0.02),
    }


def reference(x, c, ctx, c_ctx, w_ada, b_ada, w_in, na_bias, gqa_q_gain, gqa_k_gain, win_sink,
              mla_q_gain, mla_w_qb, mla_kv_gain, mla_w_kvb, w_out, ln1_g, ln1_b,
              router_group_w, router_group_b, router_expert_w, router_expert_b,
              moe_w_gate, moe_w_up, moe_w_down, ln2_g, ln2_b):
    t = x.shape[1]
    pos = jnp.arange(t)
    row = pos // GRID_W
    col = pos % GRID_W
    xc = ctx
    for l in range(DEPTH):
        with_ctx = l < DEPTH - 1
        mod = jax.nn.silu(c) @ w_ada[l] + b_ada[l]
        sh1, sc1, g1, sh2, sc2, g2 = jnp.split(mod[:, None, :], 6, axis=-1)
        sh1c, sc1c, g1c, sh2c, sc2c, g2c = jnp.split(jax.nn.silu(c_ctx) @ w_ada[l] + b_ada[l], 6, axis=-1)

        h = _layer_norm(x) * (1 + sc1) + sh1
        hc = _layer_norm(xc) * (1 + sc1c) + sh1c
        mix, mix_c = _token_mix(h, hc, row, col, w_in[l], na_bias[l], gqa_q_gain[l], gqa_k_gain[l],
                                win_sink[l], mla_q_gain[l], mla_w_qb[l], mla_kv_gain[l], mla_w_kvb[l],
                                w_out[l], with_ctx)
        x = _layer_norm(ALPHA * x + g1 * mix, ln1_g[l], ln1_b[l])
        moe_p = (router_group_w[l], router_group_b[l], router_expert_w[l], router_expert_b[l],
                 moe_w_gate[l], moe_w_up[l], moe_w_down[l])
        x = _layer_norm(ALPHA * x + g2 * _hier_moe(_layer_norm(x) * (1 + sc2) + sh2, *moe_p),
                        ln2_g[l], ln2_b[l])
        if with_ctx:
            xc = _layer_norm(ALPHA * xc + g1c * mix_c, ln1_g[l], ln1_b[l])
            xc = _layer_norm(ALPHA * xc + g2c * _hier_moe(_layer_norm(xc) * (1 + sc2c) + sh2c, *moe_p),
                             ln2_g[l], ln2_b[l])
    return x
```

```python
import os
import numpy as np
from contextlib import ExitStack
import concourse.bass as bass
import concourse.mybir as mybir
from concourse.bass_utils import run_bass_kernel_spmd

F32 = mybir.dt.float32
BF16 = mybir.dt.bfloat16
AF = mybir.ActivationFunctionType
ALU = mybir.AluOpType
AX = mybir.AxisListType

D = 1024
TL = 4096
CT = 256
TS = TL + CT
NT = TS // 128
NS = 2
NTOK = NS * TS
DEPTH = 4
DPROJ = 2208
NEG = -30000.0
ALPHA = 8.0 ** 0.25
EPS = 1e-6
NVAR = 21


class T:
    __slots__ = ("ap", "name", "w", "r")

    def __init__(self, ap, name=""):
        self.ap = ap
        self.name = name
        self.w = {}
        self.r = {}

    def __getitem__(self, idx):
        return self.ap[idx]


def _merge(deps, d, skip=None):
    for s, v in d.items():
        if s is skip:
            continue
        if deps.get(s, 0) < v:
            deps[s] = v


class Eng:
    def __init__(self, fw, eng, name):
        self.fw = fw
        self.eng = eng
        self.name = name
        self.sem = fw.new_sem("s_" + name)
        self.n = 0
        self.waited = {}
        self.ring = None
        self.dma_count = 0

    def need(self, deps):
        for sem, val in deps.items():
            if self.waited.get(sem, 0) < val:
                self.eng.wait_ge(sem, val)
                self.waited[sem] = val

    def op(self, build, reads=(), writes=(), acc=False):
        deps = {}
        skip = self.sem if acc else None
        for t in reads:
            _merge(deps, t.w)
        for t in writes:
            _merge(deps, t.w, skip)
            _merge(deps, t.r, skip)
        self.need(deps)
        ins = build(self.eng)
        self.n += 1
        ins.then_inc(self.sem, 1)
        for t in reads:
            t.r[self.sem] = self.n
        for t in writes:
            if acc:
                t.w[self.sem] = self.n
            else:
                t.w = {self.sem: self.n}
                t.r = {}
        return ins

    def dma(self, out, in_, reads=(), writes=(), **kw):
        K = len(self.ring)
        m = self.dma_count
        k, j = m % K, m // K
        deps = {}
        if j > 0:
            deps[self.ring[k]] = 16 * j
        for t in reads:
            _merge(deps, t.w)
        for t in writes:
            _merge(deps, t.w)
            _merge(deps, t.r)
        self.need(deps)
        ins = self.eng.dma_start(out=out, in_=in_, **kw)
        ins.then_inc(self.ring[k], 16)
        self.dma_count += 1
        val = 16 * (j + 1)
        for t in reads:
            t.r[self.ring[k]] = val
        for t in writes:
            t.w = {self.ring[k]: val}
            t.r = {}
        return ins


class FW:
    def __init__(self, nc, stack, ring=16):
        self.nc = nc
        self.stack = stack
        self.gen = 0
        self.pe = Eng(self, nc.tensor, "pe")
        self.act = Eng(self, nc.scalar, "act")
        self.dve = Eng(self, nc.vector, "dve")
        self.pool = Eng(self, nc.gpsimd, "pool")
        self.sp = Eng(self, nc.sync, "sp")
        self.sp.ring = [self.new_sem(f"dq_sp{i}") for i in range(ring)]
        self.pool.ring = [self.new_sem(f"dq_pl{i}") for i in range(ring)]
        self.engs = [self.pe, self.act, self.dve, self.pool, self.sp]

    def new_sem(self, name):
        return self.stack.enter_context(self.nc.semaphore(name))

    def barrier(self):
        deps = {}
        for e in self.engs:
            if e.n:
                deps[e.sem] = e.n
            if e.ring is not None and e.dma_count:
                K = len(e.ring)
                for k in range(K):
                    cnt = (e.dma_count - k + K - 1) // K
                    if cnt:
                        deps[e.ring[k]] = 16 * cnt
        for e in self.engs:
            d = {s: v for s, v in deps.items() if s is not e.sem}
            e.need(d)
        self.gen += 1
        for e in self.engs:
            if e.n:
                e.sem = self.new_sem(f"s_{e.name}_g{self.gen}")
                e.n = 0


class Scope:
    _n = 0

    def __init__(self, fw):
        self.fw = fw
        self.st = ExitStack()
        Scope._n += 1
        self.tag = f"_s{Scope._n}"

    def __enter__(self):
        self.st.__enter__()
        return self

    def __exit__(self, *a):
        self.fw.barrier()
        return self.st.__exit__(*a)

    def sb(self, name, shape, dt):
        return T(self.st.enter_context(self.fw.nc.sbuf_tensor(name + self.tag, list(shape), dt)), name)

    def ps(self, name, shape, dt):
        return T(self.st.enter_context(self.fw.nc.psum_tensor(name + self.tag, list(shape), dt)), name)


class _SkipBody(Exception):
    pass


class _Skip:
    def __enter__(self):
        return self

    def __exit__(self, et, ev, tb):
        return et is _SkipBody

    def sb(self, *a, **k):
        raise _SkipBody()

    ps = sb


class Rot:
    def __init__(self, tiles):
        self.tiles = tiles
        self.i = 0

    def next(self):
        t = self.tiles[self.i % len(self.tiles)]
        self.i += 1
        return t


def _rope_tables(d):
    h = d // 2
    q = h // 2
    pos = np.arange(TL)
    row = (pos // 64).astype(np.float32)
    col = (pos % 64).astype(np.float32)
    freq = (10000.0 ** (-np.arange(0, h, 2, dtype=np.float32) / h)).astype(np.float32)
    C = np.ones((TL + 128, d), np.float32)
    S = np.zeros((TL + 128, d), np.float32)
    for o, p in ((0, row), (h, col)):
        ang = p[:, None] * freq[None, :]
        c, s = np.cos(ang).astype(np.float32), np.sin(ang).astype(np.float32)
        C[:TL, o:o + q] = c
        C[:TL, o + q:o + h] = c
        S[:TL, o:o + q] = -s
        S[:TL, o + q:o + h] = s
    return C, S


def _na_variants():
    keys = {}
    for i in range(32):
        rows = [2 * i, 2 * i + 1]
        ks = set()
        for r in rows:
            r0 = min(max(r - 4, 0), 56)
            for kr in range(r0, r0 + 8):
                ks.add(kr // 2)
        keys[i] = sorted(ks)
    vid = {}
    nxt = 5
    for i in range(32):
        for j in keys[i]:
            if 2 <= i <= 29:
                vid[(i, j)] = j - i + 2
            else:
                vid[(i, j)] = nxt
                nxt += 1
    assert nxt == NVAR, nxt
    return keys, vid


def _na_bias_tables(na_bias):
    keys, vid = _na_variants()
    L = na_bias.shape[0]
    out = np.full((L, NVAR, 128, 4, 128), NEG, np.float32)
    done = set()
    kk = np.arange(128)
    for (i, j), v in vid.items():
        if v in done:
            continue
        done.add(v)
        q_row = 2 * i + kk // 64
        q_col = kk % 64
        k_row = 2 * j + kk // 64
        k_col = kk % 64
        r0 = np.clip(q_row - 4, 0, 56)
        c0 = np.clip(q_col - 8, 0, 48)
        ok = ((k_row[:, None] >= r0[None, :]) & (k_row[:, None] < r0[None, :] + 8)
              & (k_col[:, None] >= c0[None, :]) & (k_col[:, None] < c0[None, :] + 16))
        dr = np.clip(k_row[:, None] - q_row[None, :] + 7, 0, 14)
        dc = np.clip(k_col[:, None] - q_col[None, :] + 15, 0, 30)
        for l in range(L):
            for h in range(4):
                g = na_bias[l, h][dr, dc]
                out[l, v, :, h, :] = np.where(ok, g, np.float32(NEG))
    return out


def build(n_layers=DEPTH, dbg=(), stop_after=None, stop_layer=0, single=None):
    DD = DEPTH if single is None else 1
    if single is not None:
        n_layers = 1
    nc = bass.Bass("TRN2", target_bir_lowering=False)

    def din(name, shape, dt=F32):
        return nc.dram_tensor(name, list(shape), dt, kind="ExternalInput")

    def dscr(name, shape, dt):
        return nc.dram_tensor(name, list(shape), dt, kind="Internal")

    x_in = din("x_in", [NTOK, D])
    c3T = din("c3T", [128, 8, 3])
    w_ada = din("w_ada", [DD, D, 6 * D])
    b_ada = din("b_ada", [DD, 6 * D])
    w_in = din("w_in", [DD, D, DPROJ])
    nab = din("nab", [DD, NVAR, 128, 4, 128])
    gq_gain = din("gqa_q_gain", [DD, 64])
    gk_gain = din("gqa_k_gain", [DD, 64])
    win_sink = din("win_sink", [DD, 4])
    mq_gain = din("mla_q_gain", [DD, 256])
    w_qb = din("mla_w_qb", [DD, 256, 384])
    mkv_gain = din("mla_kv_gain", [DD, 128])
    w_kvb = din("mla_w_kvb", [DD, 128, 512])
    w_out = din("w_out", [DD, D, D])
    ln1_g = din("ln1_g", [DD, D])
    ln1_b = din("ln1_b", [DD, D])
    rg_w = din("router_group_w", [DD, D, 4])
    rg_b = din("router_group_b", [DD, 4])
    re_w = din("router_expert_w", [DD, D, 16])
    re_b = din("router_expert_b", [DD, 16])
    mw_gate = din("moe_w_gate", [DD, 16, D, 512])
    mw_up = din("moe_w_up", [DD, 16, D, 512])
    mw_down = din("moe_w_down", [DD, 16, 512, D])
    ln2_g = din("ln2_g", [DD, D])
    ln2_b = din("ln2_b", [DD, D])
    ident_in = din("ident", [128, 128])
    ropeC64 = din("ropeC64", [TL + 128, 64])
    ropeS64 = din("ropeS64", [TL + 128, 64])
    ropeC32 = din("ropeC32", [TL + 128, 32])
    ropeS32 = din("ropeS32", [TL + 128, 32])
    wmask = din("wmask", [128, 2, 128])
    y_out = nc.dram_tensor("y", [NTOK if single == "mid" else NS * TL, D], F32, kind="ExternalOutput")

    X = dscr("X", [NTOK, D], F32)
    MOD = dscr("MOD", [DD, 3, 6 * D], F32)
    QT = {"A": dscr("QT_A", [NS, 2, 128, TS], BF16), "B": dscr("QT_B", [NS, 2, 128, TS], BF16),
          "C": dscr("QT_C", [NS, 2, 128, TS], BF16), "D": dscr("QT_D", [NS, 4, 96, TS], BF16)}
    KT = {"A": dscr("KT_A", [NS, 2, 128, TS], BF16), "B": dscr("KT_B", [NS, 1, 128, TS], BF16),
          "C": dscr("KT_C", [NS, 1, 128, TS], BF16), "D": dscr("KT_D", [NS, 4, 96, TS], BF16)}
    VV = {"A": dscr("V_A", [NS, TS, 4 * 65], BF16), "B": dscr("V_B", [NS, TS, 2 * 65], BF16),
          "C": dscr("V_C", [NS, TS, 2 * 65], BF16), "D": dscr("V_D", [NS, TS, 4 * 65], BF16)}
    AO = dscr("AO", [NTOK, D], BF16)
    H2T = dscr("H2T", [128, 8, NTOK], BF16)
    dbg_out = {}
    for nm, shp, dt in dbg:
        dbg_out[nm] = nc.dram_tensor("dbg_" + nm, list(shp), dt, kind="ExternalOutput")

    tX = [T(None, f"X{g}") for g in range(NTOK // 128)]
    tMOD = [T(None, f"MOD{l}") for l in range(DEPTH)]
    tQKV = {m: [T(None, f"qkv{m}{s}") for s in range(NS)] for m in "ABCD"}
    tAO = [T(None, f"AO{g}") for g in range(NTOK // 128)]
    tH2T = [T(None, f"H2T{g}") for g in range(NTOK // 128)]
    tY = T(None, "Y")

    def bcast_ap(handle, offset, n, parts=128):
        return bass.AP(handle, offset, [[0, parts], [1, n]])

    with ExitStack() as gst:
        fw = FW(nc, gst)
        pe, act, dve, pool, sp = fw.pe, fw.act, fw.dve, fw.pool, fw.sp

        def gsb(name, shape, dt):
            return T(gst.enter_context(nc.sbuf_tensor(name, list(shape), dt)), name)

        identf = gsb("identf", [128, 128], F32)
        identb = gsb("identb", [128, 128], BF16)
        epsb = gsb("epsb", [128, 1], F32)
        gate_all = gsb("gate_all", [128, NS * NT, 16], F32)
        sp.dma(identf[:], ident_in.ap()[:, :], writes=[identf])
        dve.op(lambda e: e.tensor_copy(out=identb[:], in_=identf[:]), reads=[identf], writes=[identb])
        dve.op(lambda e: e.memset(epsb[:], EPS), writes=[epsb])

        def layer_norm(src, dst, stats, mv, rstd, reads_extra=()):
            for c in range(2):
                dve.op(lambda e: e.bn_stats(out=stats[:, c, :], in_=src[:, c * 512:(c + 1) * 512]),
                       reads=[src], writes=[stats])
            dve.op(lambda e: e.bn_aggr(out=mv[:], in_=stats[:].rearrange("p a b -> p (a b)")),
                   reads=[stats], writes=[mv])
            act.op(lambda e: e.activation(out=rstd[:], in_=mv[:, 1:2], func=AF.Ln, bias=epsb[:, 0:1]),
                   reads=[mv, epsb], writes=[rstd])
            act.op(lambda e: e.activation(out=rstd[:], in_=rstd[:], func=AF.Exp, scale=-0.5),
                   reads=[rstd], writes=[rstd])
            dve.op(lambda e: e.tensor_scalar(out=dst[:], in0=src[:], scalar1=mv[:, 0:1], scalar2=rstd[:, 0:1],
                                             op0=ALU.subtract, op1=ALU.mult),
                   reads=[src, mv, rstd], writes=[dst])

        def transposes(src, ncols, blk, pst, ident, nparts=128):
            for c in range(ncols // blk):
                pe.op(lambda e: e.transpose(pst[0:blk, c, :], src[:, c * blk:(c + 1) * blk], ident[:]),
                      reads=[src, ident], writes=[pst], acc=True)

        def load_bc(dst, handle, offset, n=D, eng=None):
            (eng or sp).dma(dst[:, 0:n], bcast_ap(handle, offset, n), writes=[dst])

        with Scope(fw) as sc:
            c3 = sc.sb("c3", [128, 8, 3], F32)
            sc3 = sc.sb("sc3", [128, 8, 3], F32)
            bada = sc.sb("bada", [3, 6 * D], F32)
            modsb = sc.sb("modsb", [3, 6 * D], F32)
            wch = Rot([sc.sb(f"wch{i}", [128, 8, 512], F32) for i in range(2)])
            pmod = Rot([sc.ps(f"pmod{i}", [3, 512], F32) for i in range(2)])
            sp.dma(c3[:], c3T.ap()[:, :, :], writes=[c3])
            act.op(lambda e: e.activation(out=sc3[:], in_=c3[:], func=AF.Silu), reads=[c3], writes=[sc3])
            for l in range(n_layers):
                sp.dma(bada[:], bcast_ap(b_ada, l * 6 * D, 6 * D, parts=3), writes=[bada])
                for cc in range(12):
                    wt = wch.next()
                    sp.dma(wt[:], w_ada.ap()[l, :, cc * 512:(cc + 1) * 512].rearrange("(c p) n -> p c n", p=128),
                           writes=[wt])
                    pm = pmod.next()
                    for k in range(8):
                        pe.op(lambda e: e.matmul(pm[:], sc3[:, k, :], wt[:, k, :], start=(k == 0), stop=(k == 7)),
                              reads=[sc3, wt], writes=[pm], acc=True)
                    dve.op(lambda e: e.tensor_tensor(out=modsb[:, cc * 512:(cc + 1) * 512], in0=pm[:],
                                                     in1=bada[:, cc * 512:(cc + 1) * 512], op=ALU.add),
                           reads=[pm, bada], writes=[modsb])
                for o in (1, 4):
                    dve.op(lambda e: e.tensor_scalar_add(out=modsb[:, o * D:(o + 1) * D], in0=modsb[:, o * D:(o + 1) * D],
                                                         scalar1=1.0), reads=[modsb], writes=[modsb])
                sp.dma(MOD.ap()[l, :, :], modsb[:], reads=[modsb], writes=[tMOD[l]])

        if "MOD" in dbg_out:
            sp.dma(dbg_out["MOD"].ap()[:, :, :], MOD.ap()[:, :, :], reads=tMOD[:n_layers], writes=[tY])
            fw.barrier()
        if stop_after == "p0":
            n_layers = 0

        def mod_off(l, src, which):
            return (l * 3 + src) * 6 * D + which * D

        keysA, vidA = _na_variants()

        for l in range(n_layers):
            last = (l == DEPTH - 1) if single is None else (single == "last")
            with_ctx = not last
            if stop_after == "lstart" and l == stop_layer:
                break

            with Scope(fw) as sc:
                winb = sc.sb("winb", [128, 8, DPROJ], BF16)
                wqbb = sc.sb("wqbb", [128, 2, 384], BF16)
                wkvbb = sc.sb("wkvbb", [128, 512], BF16)
                pool.dma(winb[:], w_in.ap()[l].rearrange("(c p) n -> p c n", p=128), writes=[winb])
                pool.dma(wqbb[:], w_qb.ap()[l].rearrange("(c p) n -> p c n", p=128), writes=[wqbb])
                pool.dma(wkvbb[:], w_kvb.ap()[l], writes=[wkvbb])
                bc_sc = [sc.sb(f"bc_sc{i}", [128, D], F32) for i in range(3)]
                bc_sh = [sc.sb(f"bc_sh{i}", [128, D], F32) for i in range(3)]
                for src in range(3):
                    sp.dma(bc_sh[src][:], bcast_ap(MOD, mod_off(l, src, 0), D), reads=[tMOD[l]], writes=[bc_sh[src]])
                    sp.dma(bc_sc[src][:], bcast_ap(MOD, mod_off(l, src, 1), D), reads=[tMOD[l]], writes=[bc_sc[src]])
                gainB = sc.sb("gainB", [128, 6, 64], F32)
                for hh in range(4):
                    sp.dma(gainB[:, hh, :], bcast_ap(gq_gain, l * 64, 64), writes=[gainB])
                for hh in range(2):
                    sp.dma(gainB[:, 4 + hh, :], bcast_ap(gk_gain, l * 64, 64), writes=[gainB])
                gainQ = sc.sb("gainQ", [128, 256], F32)
                gainKV = sc.sb("gainKV", [128, 128], F32)
                sp.dma(gainQ[:], bcast_ap(mq_gain, l * 256, 256), writes=[gainQ])
                sp.dma(gainKV[:], bcast_ap(mkv_gain, l * 128, 128), writes=[gainKV])

                xin = Rot([sc.sb(f"xin{i}", [128, D], F32) for i in range(2)])
                rC64 = Rot([sc.sb(f"rC64_{i}", [128, 64], F32) for i in range(2)])
                rS64 = Rot([sc.sb(f"rS64_{i}", [128, 64], F32) for i in range(2)])
                rC32 = Rot([sc.sb(f"rC32_{i}", [128, 32], F32) for i in range(2)])
                rS32 = Rot([sc.sb(f"rS32_{i}", [128, 32], F32) for i in range(2)])
                stats = sc.sb("stats", [128, 2, 6], F32)
                mv = sc.sb("mv", [128, 2], F32)
                rstd = sc.sb("rstd", [128, 1], F32)
                tmpA = sc.sb("tmpA", [128, D], F32)
                hb = sc.sb("hb", [128, D], BF16)
                hT = Rot([sc.sb(f"hT{i}", [128, 8, 128], BF16) for i in range(2)])
                sq = sc.sb("sq", [128, 384], F32)
                ss6 = sc.sb("ss6", [128, 6], F32)
                t1 = sc.sb("t1", [128, 6, 64], F32)
                t2 = sc.sb("t2", [128, 6, 64], F32)
                t3 = sc.sb("t3", [128, 6, 64], F32)
                ssD = sc.sb("ssD", [128, 2], F32)
                junk = sc.sb("junk", [128, 256], F32)
                cqkv = sc.sb("cqkv", [128, 384], BF16)
                cT = sc.sb("cT", [128, 3, 128], BF16)
                u1 = sc.sb("u1", [128, 4, 32], F32)
                u2 = sc.sb("u2", [128, 4, 32], F32)
                u3 = sc.sb("u3", [128, 4, 32], F32)
                kr1 = sc.sb("kr1", [128, 32], F32)
                kr2 = sc.sb("kr2", [128, 32], F32)
                qkA = Rot([sc.sb(f"qkA{i}", [128, 512], BF16) for i in range(2)])
                qkB = Rot([sc.sb(f"qkB{i}", [128, 384], BF16) for i in range(2)])
                qkC = Rot([sc.sb(f"qkC{i}", [128, 384], BF16) for i in range(2)])
                qdb = Rot([sc.sb(f"qdb{i}", [128, 4, 96], BF16) for i in range(2)])
                kdb = Rot([sc.sb(f"kdb{i}", [128, 4, 96], BF16) for i in range(2)])
                stgA = Rot([sc.sb(f"stgA{i}", [128, 4, 128], BF16) for i in range(2)])
                stgB = Rot([sc.sb(f"stgB{i}", [128, 3, 128], BF16) for i in range(2)])
                stgC = Rot([sc.sb(f"stgC{i}", [128, 3, 128], BF16) for i in range(2)])
                stgD = Rot([sc.sb(f"stgD{i}", [128, 8, 128], BF16) for i in range(2)])
                vA = [sc.sb(f"vA{i}", [128, 4, 65], BF16) for i in range(2)]
                vB = [sc.sb(f"vB{i}", [128, 2, 65], BF16) for i in range(2)]
                vC = [sc.sb(f"vC{i}", [128, 2, 65], BF16) for i in range(2)]
                vD = [sc.sb(f"vD{i}", [128, 4, 65], BF16) for i in range(2)]
                for vt in vA + vB + vC + vD:
                    pool.op(lambda e: e.memset(vt[:], 1.0), writes=[vt])
                vA, vB, vC, vD = Rot(vA), Rot(vB), Rot(vC), Rot(vD)
                pT = Rot([sc.ps(f"pT{i}", [128, 8, 128], BF16) for i in range(2)])
                pj = Rot([sc.ps(f"pj{i}", [128, 512], F32) for i in range(4)])
                pq = Rot([sc.ps(f"pq{i}", [128, 8, 128], BF16) for i in range(2)])

                x_src = x_in if l == 0 else X
                order = [(s, j) for s in range(NS) for j in range(NT)]
                P1S = float(os.environ.get("P1_STEP", "99"))
                if P1S < 99:
                    order = order[:2]

                def p1_load(s, j):
                    g = s * NT + j
                    xt = xin.next()
                    rd = [] if l == 0 else [tX[g]]
                    sp.dma(xt[:], x_src.ap()[g * 128:(g + 1) * 128, :], reads=rd, writes=[xt])
                    r0 = j * 128 if j < 32 else TL
                    tabs = []
                    for rot, src_t, n in ((rC64, ropeC64, 64), (rS64, ropeS64, 64), (rC32, ropeC32, 32), (rS32, ropeS32, 32)):
                        tt = rot.next()
                        sp.dma(tt[:], src_t.ap()[r0:r0 + 128, :], writes=[tt])
                        tabs.append(tt)
                    return xt, tabs

                def rope(dst_views, src, nh, dd, Ct, St, ta, tb, src_t):
                    q = dd // 4
                    Cb = Ct[:].unsqueeze(1).broadcast_to([128, nh, dd])
                    pool_or_dve = dve
                    dve.op(lambda e: e.tensor_tensor(out=ta[:, 0:nh, :], in0=src, in1=Cb, op=ALU.mult),
                           reads=[src_t, Ct], writes=[ta])
                    sv = src.rearrange("p h (r f e) -> p h r f e", r=2, f=2)
                    tv = tb[:, 0:nh, :].rearrange("p h (r f e) -> p h r f e", r=2, f=2)
                    Sv = St[:].rearrange("p (r f e) -> p r f e", r=2, f=2)
                    for f in range(2):
                        Sb = Sv[:, :, f, :].unsqueeze(1).broadcast_to([128, nh, 2, q])
                        dve.op(lambda e: e.tensor_tensor(out=tv[:, :, :, f, :], in0=sv[:, :, :, 1 - f, :], in1=Sb, op=ALU.mult),
                               reads=[src_t, St], writes=[tb])
                    for (o_ap, a_ap, b_ap) in dst_views(ta, tb):
                        dve.op(lambda e: e.tensor_tensor(out=o_ap[0], in0=a_ap, in1=b_ap, op=ALU.add),
                               reads=[ta, tb], writes=[o_ap[1]])

                nxt = p1_load(*order[0])
                for idx, (s, j) in enumerate(order):
                    g = s * NT + j
                    tok0 = j * 128
                    xt, (C64, S64, C32, S32) = nxt
                    if idx + 1 < len(order):
                        nxt = p1_load(*order[idx + 1])
                    src = s if j < 32 else 2
                    if l == 0:
                        sp.dma(X.ap()[g * 128:(g + 1) * 128, :], xt[:], reads=[xt], writes=[tX[g]])
                    if P1S < 1:
                        continue
                    layer_norm(xt, tmpA, stats, mv, rstd)
                    pool.op(lambda e: e.tensor_tensor(out=tmpA[:], in0=tmpA[:], in1=bc_sc[src][:], op=ALU.mult),
                            reads=[tmpA, bc_sc[src]], writes=[tmpA])
                    dve.op(lambda e: e.tensor_tensor(out=hb[:], in0=tmpA[:], in1=bc_sh[src][:], op=ALU.add),
                           reads=[tmpA, bc_sh[src]], writes=[hb])
                    pt = pT.next()
                    transposes(hb, D, 128, pt, identb)
                    ht = hT.next()
                    act.op(lambda e: e.copy(out=ht[:], in_=pt[:]), reads=[pt], writes=[ht])

                    def proj(c0, n):
                        p = pj.next()
                        for k in range(8):
                            pe.op(lambda e: e.matmul(p[:, 0:n], ht[:, k, :], winb[:, k, c0:c0 + n], start=(k == 0), stop=(k == 7)),
                                  reads=[ht, winb], writes=[p], acc=True)
                        return p

                    if P1S < 2:
                        continue
                    pa0 = proj(0, 512)
                    pa1 = proj(512, 256)
                    qa = qkA.next()
                    act.op(lambda e: e.activation(out=qa[:, 0:256], in_=pa0[:, 0:256], func=AF.Copy, scale=0.125),
                           reads=[pa0], writes=[qa])
                    act.op(lambda e: e.copy(out=qa[:, 256:512], in_=pa0[:, 256:512]), reads=[pa0], writes=[qa])
                    va = vA.next()
                    dve.op(lambda e: e.tensor_copy(out=va[:, :, 0:64], in_=pa1[:, 0:256].rearrange("p (h d) -> p h d", h=4)),
                           reads=[pa1], writes=[va])
                    pqa = pq.next()
                    transposes(qa, 512, 128, pqa, identb)
                    sa = stgA.next()
                    act.op(lambda e: e.copy(out=sa[:], in_=pqa[:, 0:4, :]), reads=[pqa], writes=[sa])
                    sp.dma(QT["A"].ap()[s, :, :, tok0:tok0 + 128].rearrange("b p t -> p b t"), sa[:, 0:2, :],
                           reads=[sa], writes=[tQKV["A"][s]])
                    sp.dma(KT["A"].ap()[s, :, :, tok0:tok0 + 128].rearrange("b p t -> p b t"), sa[:, 2:4, :],
                           reads=[sa], writes=[tQKV["A"][s]])
                    sp.dma(VV["A"].ap()[s, tok0:tok0 + 128, :], va[:].rearrange("p h d -> p (h d)"),
                           reads=[va], writes=[tQKV["A"][s]])

                    if P1S < 2.05:
                        continue
                    for m in ("B", "C"):
                        pb = proj(768 if m == "B" else 1280, 512)
                        qk_view = pb[:, 0:384].rearrange("p (h d) -> p h d", h=6)
                        if m == "B":
                            act.op(lambda e: e.activation(out=sq[:], in_=pb[:, 0:384], func=AF.Square), reads=[pb], writes=[sq])
                            dve.op(lambda e: e.tensor_reduce(out=ss6[:], in_=sq[:].rearrange("p (h d) -> p h d", h=6),
                                                             axis=AX.X, op=ALU.add), reads=[sq], writes=[ss6])
                            act.op(lambda e: e.activation(out=ss6[:], in_=ss6[:], func=AF.Ln, bias=epsb[:, 0:1], scale=1.0 / 64),
                                   reads=[ss6, epsb], writes=[ss6])
                            act.op(lambda e: e.activation(out=ss6[:], in_=ss6[:], func=AF.Exp, scale=-0.5), reads=[ss6], writes=[ss6])
                            if P1S < 2.2:
                                break
                            dve.op(lambda e: e.tensor_tensor(out=t3[:], in0=qk_view, in1=ss6[:].unsqueeze(2).broadcast_to([128, 6, 64]),
                                                             op=ALU.mult), reads=[pb, ss6], writes=[t3])
                            dve.op(lambda e: e.tensor_tensor(out=t3[:], in0=t3[:], in1=gainB[:], op=ALU.mult),
                                   reads=[t3, gainB], writes=[t3])
                            r_src, r_t = t3[:], t3
                        else:
                            act.op(lambda e: e.copy(out=t3[:], in_=qk_view), reads=[pb], writes=[t3])
                            r_src, r_t = t3[:], t3
                        if P1S < 2.3:
                            break
                        qb_ = (qkB if m == "B" else qkC).next()

                        def views(ta, tb, qb_=qb_):
                            oq = qb_[:, 0:256].rearrange("p (gi kv d) -> p kv gi d", gi=2, kv=2)
                            aq = ta[:, 0:4, :].rearrange("p (kv gi) d -> p kv gi d", kv=2)
                            bq = tb[:, 0:4, :].rearrange("p (kv gi) d -> p kv gi d", kv=2)
                            ok = qb_[:, 256:384].rearrange("p (h d) -> p h d", h=2)
                            return [((oq, qb_), aq, bq), ((ok, qb_), ta[:, 4:6, :], tb[:, 4:6, :])]

                        rope(views, r_src, 6, 64, C64, S64, t1, t2, r_t)
                        if P1S < 2.4:
                            break
                        vb = (vB if m == "B" else vC).next()
                        act.op(lambda e: e.copy(out=vb[:, :, 0:64], in_=pb[:, 384:512].rearrange("p (h d) -> p h d", h=2)),
                               reads=[pb], writes=[vb])
                        pqb = pq.next()
                        transposes(qb_, 384, 128, pqb, identb)
                        if P1S < 2.5:
                            break
                        sb_ = (stgB if m == "B" else stgC).next()
                        act.op(lambda e: e.copy(out=sb_[:], in_=pqb[:, 0:3, :]), reads=[pqb], writes=[sb_])
                        if P1S >= 2.6:
                            sp.dma(QT[m].ap()[s, :, :, tok0:tok0 + 128].rearrange("b p t -> p b t"), sb_[:, 0:2, :],
                                   reads=[sb_], writes=[tQKV[m][s]])
                        if P1S >= 2.7:
                            sp.dma(KT[m].ap()[s, 0, :, tok0:tok0 + 128], sb_[:, 2, :], reads=[sb_], writes=[tQKV[m][s]])
                        if P1S >= 2.8:
                            sp.dma(VV[m].ap()[s, tok0:tok0 + 128, :], vb[:].rearrange("p h d -> p (h d)"),
                                   reads=[vb], writes=[tQKV[m][s]])
                        if P1S < 2.9:
                            break

                    if P1S < 4:
                        continue
                    pd = proj(1792, 416)
                    act.op(lambda e: e.activation(out=junk[:, 0:256], in_=pd[:, 0:256], func=AF.Square, accum_out=ssD[:, 0:1]),
                           reads=[pd], writes=[junk, ssD])
                    act.op(lambda e: e.activation(out=junk[:, 0:128], in_=pd[:, 256:384], func=AF.Square, accum_out=ssD[:, 1:2]),
                           reads=[pd], writes=[junk, ssD])
                    act.op(lambda e: e.activation(out=ssD[:, 0:1], in_=ssD[:, 0:1], func=AF.Ln, bias=epsb[:, 0:1], scale=1.0 / 256),
                           reads=[ssD, epsb], writes=[ssD])
                    act.op(lambda e: e.activation(out=ssD[:, 1:2], in_=ssD[:, 1:2], func=AF.Ln, bias=epsb[:, 0:1], scale=1.0 / 128),
                           reads=[ssD, epsb], writes=[ssD])
                    act.op(lambda e: e.activation(out=ssD[:], in_=ssD[:], func=AF.Exp, scale=-0.5), reads=[ssD], writes=[ssD])
                    dve.op(lambda e: e.scalar_tensor_tensor(out=cqkv[:, 0:256], in0=pd[:, 0:256], scalar=ssD[:, 0:1], in1=gainQ[:],
                                                            op0=ALU.mult, op1=ALU.mult), reads=[pd, ssD, gainQ], writes=[cqkv])
                    dve.op(lambda e: e.scalar_tensor_tensor(out=cqkv[:, 256:384], in0=pd[:, 256:384], scalar=ssD[:, 1:2], in1=gainKV[:],
                                                            op0=ALU.mult, op1=ALU.mult), reads=[pd, ssD, gainKV], writes=[cqkv])
                    pqc = pq.next()
                    transposes(cqkv, 384, 128, pqc, identb)
                    act.op(lambda e: e.copy(out=cT[:], in_=pqc[:, 0:3, :]), reads=[pqc], writes=[cT])
                    pqd = pj.next()
                    for k in range(2):
                        pe.op(lambda e: e.matmul(pqd[:, 0:384], cT[:, k, :], wqbb[:, k, :], start=(k == 0), stop=(k == 1)),
                              reads=[cT, wqbb], writes=[pqd], acc=True)
                    pkv = pj.next()
                    pe.op(lambda e: e.matmul(pkv[:, 0:512], cT[:, 2, :], wkvbb[:], start=True, stop=True),
                          reads=[cT, wkvbb], writes=[pkv], acc=True)
                    qd = qdb.next()
                    kd = kdb.next()
                    qv = pqd[:, 0:384].rearrange("p (h d) -> p h d", h=4)
                    kvv = pkv[:, 0:512].rearrange("p (h d) -> p h d", h=4)
                    act.op(lambda e: e.copy(out=qd[:, :, 0:64], in_=qv[:, :, 0:64]), reads=[pqd], writes=[qd])

                    def views_q(ta, tb, qd=qd):
                        return [((qd[:, :, 64:96], qd), ta[:, 0:4, :], tb[:, 0:4, :])]

                    act.op(lambda e: e.copy(out=u3[:], in_=qv[:, :, 64:96]), reads=[pqd], writes=[u3])
                    rope(views_q, u3[:], 4, 32, C32, S32, u1, u2, u3)
                    act.op(lambda e: e.copy(out=kd[:, :, 0:64], in_=kvv[:, :, 0:64]), reads=[pkv], writes=[kd])
                    act.op(lambda e: e.copy(out=kr1[:], in_=pd[:, 384:416]), reads=[pd], writes=[kr1])
                    kr_src = kr1[:].unsqueeze(1)

                    def views_k(ta, tb, kd=kd):
                        return [((kd[:, :, 64:96], kd), ta[:, 0:1, :].broadcast_to([128, 4, 32]), tb[:, 0:1, :].broadcast_to([128, 4, 32]))]

                    rope(views_k, kr_src, 1, 32, C32, S32, u1, u2, kr1)
                    vd = vD.next()
                    dve.op(lambda e: e.tensor_copy(out=vd[:, :, 0:64], in_=kvv[:, :, 64:128]), reads=[pkv], writes=[vd])
                    pqe = pq.next()
                    for h in range(4):
                        pe.op(lambda e: e.transpose(pqe[0:96, h, :], qd[:, h, :], identb[:]), reads=[qd, identb], writes=[pqe], acc=True)
                        pe.op(lambda e: e.transpose(pqe[0:96, 4 + h, :], kd[:, h, :], identb[:]), reads=[kd, identb], writes=[pqe], acc=True)
                    sd = stgD.next()
                    act.op(lambda e: e.copy(out=sd[0:96, :, :], in_=pqe[0:96, :, :]), reads=[pqe], writes=[sd])
                    sp.dma(QT["D"].ap()[s, :, :, tok0:tok0 + 128].rearrange("b p t -> p b t"), sd[0:96, 0:4, :],
                           reads=[sd], writes=[tQKV["D"][s]])
                    sp.dma(KT["D"].ap()[s, :, :, tok0:tok0 + 128].rearrange("b p t -> p b t"), sd[0:96, 4:8, :],
                           reads=[sd], writes=[tQKV["D"][s]])
                    sp.dma(VV["D"].ap()[s, tok0:tok0 + 128, :], vd[:].rearrange("p h d -> p (h d)"),
                           reads=[vd], writes=[tQKV["D"][s]])

            if stop_after == "p1" and l == stop_layer:
                break
            if os.environ.get("SKIP_REST") == "1":
                continue
            SKP = os.environ.get("SKIP_PH", "").split(",")
            with (Scope(fw) if "2" not in SKP else _Skip()) as sc:
                ktb = sc.sb("ktb", [128, 4, TS], BF16)
                vtb = sc.sb("vtb", [128, NT, 260], BF16)
                qtb = Rot([sc.sb(f"qtb{i}", [128, 4, 512], BF16) for i in range(2)])
                ptb = Rot([sc.sb(f"ptb{i}", [128, 512], BF16) for i in range(4)])
                ptw = Rot([sc.sb(f"ptw{i}", [128, 7, 128], BF16) for i in range(3)])
                ssb = Rot([sc.sb(f"ssb{i}", [128, 5, 128], F32) for i in range(2)])
                aos = Rot([sc.sb(f"aos{i}", [128, 4, 256], BF16) for i in range(2)])
                rec = Rot([sc.sb(f"rec{i}", [128, 4, 1], F32) for i in range(4)])
                biasI = sc.sb("biasI", [128, 5, 4, 128], F32)
                biasE = Rot([sc.sb(f"biasE{i}", [128, 4, 4, 128], F32) for i in range(2)])
                wm = sc.sb("wm", [128, 2, 128], F32)
                esink = sc.sb("esink", [128, 4], F32)
                pS = Rot([sc.ps(f"pS{i}", [128, 512], F32) for i in range(4)])
                pO = Rot([sc.ps(f"pO{i}", [128, 512], F32) for i in range(4)])
                sp.dma(wm[:], wmask.ap()[:, :, :], writes=[wm])
                sp.dma(esink[:], bcast_ap(win_sink, l * 4, 4), writes=[esink])
                act.op(lambda e: e.activation(out=esink[:], in_=esink[:], func=AF.Exp), reads=[esink], writes=[esink])
                sp.dma(biasI[:], nab.ap()[l, 0:5].rearrange("v k h q -> k v h q"), writes=[biasI])

                MIX = {"A": dict(nb=2, rows=128, nvh=4, col=0, scale=1.0),
                       "B": dict(nb=1, rows=128, nvh=2, col=256, scale=0.125),
                       "C": dict(nb=1, rows=128, nvh=2, col=512, scale=0.125),
                       "D": dict(nb=4, rows=96, nvh=4, col=768, scale=96.0 ** -0.5)}

                def heads_of(m):
                    if m == "A":
                        return [(c, c, r * 64, 64, 2 * c + r, 2 * c + r) for c in range(2) for r in range(2)]
                    if m in ("B", "C"):
                        return [(c, 0, r * 64, 64, r, c + 2 * r) for c in range(2) for r in range(2)]
                    return [(h, h, 0, 96, h, h) for h in range(4)]

                def finish_head(pos, ao, oh, sink_h=None):
                    for t, po in enumerate(pos):
                        rc = rec.next()
                        if sink_h is not None:
                            dve.op(lambda e: e.tensor_scalar(out=rc[:, 0, :], in0=po[:, 64:65], scalar1=esink[:, sink_h:sink_h + 1],
                                                             scalar2=None, op0=ALU.add), reads=[po, esink], writes=[rc])
                            dve.op(lambda e: e.reciprocal(out=rc[:, 0, :], in_=rc[:, 0, :]), reads=[rc], writes=[rc])
                        else:
                            dve.op(lambda e: e.reciprocal(out=rc[:, 0, :], in_=po[:, 64:65]), reads=[po], writes=[rc])
                        dve.op(lambda e: e.tensor_scalar(out=ao[:, t, oh * 64:(oh + 1) * 64], in0=po[:, 0:64], scalar1=rc[:, 0, :],
                                                         scalar2=None, op0=ALU.mult), reads=[po, rc], writes=[ao])

                def dense_block(m, s, q0, nq, key_tiles):
                    cfg = MIX[m]
                    nq_t = nq // 128
                    qt = qtb.next()
                    R = cfg["rows"]
                    sp.dma(qt[0:R, 0:(2 if m != "D" else 4), 0:nq],
                           QT[m].ap()[s, :, :, q0:q0 + nq].rearrange("b p t -> p b t"), reads=[tQKV[m][s]], writes=[qt])
                    ao = aos.next()
                    for (qb_i, kb_i, r0, nr, vh, oh) in heads_of(m):
                        pos = [pO.next() for _ in range(nq_t)]
                        for ki, kt in enumerate(key_tiles):
                            ps_ = pS.next()
                            pe.op(lambda e: e.matmul(ps_[:, 0:nq], ktb[r0:r0 + nr, kb_i, kt * 128:(kt + 1) * 128],
                                                     qt[r0:r0 + nr, qb_i, 0:nq], start=True, stop=True),
                                  reads=[ktb, qt], writes=[ps_], acc=True)
                            pt_ = ptb.next()
                            act.op(lambda e: e.activation(out=pt_[:, 0:nq], in_=ps_[:, 0:nq], func=AF.Exp, scale=cfg["scale"]),
                                   reads=[ps_], writes=[pt_])
                            for t in range(nq_t):
                                pe.op(lambda e: e.matmul(pos[t][:, 0:65], pt_[:, t * 128:(t + 1) * 128], vtb[:, kt, vh * 65:(vh + 1) * 65],
                                                         start=(ki == 0), stop=(ki == len(key_tiles) - 1)),
                                      reads=[pt_, vtb], writes=[pos[t]], acc=True)
                        finish_head(pos, ao, oh, sink_h=(oh if m == "C" else None))
                    g0 = s * TS + q0
                    sp.dma(AO.ap()[g0:g0 + nq, cfg["col"]:cfg["col"] + 256].rearrange("(t p) c -> p t c", p=128),
                           ao[:, 0:nq_t, :], reads=[ao], writes=[tAO[g0 // 128 + t] for t in range(nq_t)])

                def load_kv(m, s):
                    cfg = MIX[m]
                    R, nb = cfg["rows"], cfg["nb"]
                    sp.dma(ktb[0:R, 0:nb, :], KT[m].ap()[s].rearrange("b p t -> p b t"), reads=[tQKV[m][s]], writes=[ktb])
                    w = cfg["nvh"] * 65
                    sp.dma(vtb[:, :, 0:w], VV[m].ap()[s].rearrange("(t p) c -> p t c", p=128), reads=[tQKV[m][s]], writes=[vtb])

                def local_tile_A(s, i):
                    klist = keysA[i]
                    nl = len(klist)
                    qt = qtb.next()
                    sp.dma(qt[:, 0:2, 0:128], QT["A"].ap()[s, :, :, i * 128:(i + 1) * 128].rearrange("b p t -> p b t"),
                           reads=[tQKV["A"][s]], writes=[qt])
                    if 2 <= i <= 29:
                        bt, bsl = biasI, [vidA[(i, j)] for j in klist]
                    else:
                        bt = biasE.next()
                        v0 = vidA[(i, klist[0])]
                        sp.dma(bt[:, 0:nl], nab.ap()[l, v0:v0 + nl].rearrange("v k h q -> k v h q"), writes=[bt])
                        bsl = list(range(nl))
                    ao = aos.next()
                    for (qb_i, kb_i, r0, nr, vh, oh) in heads_of("A"):
                        psx, psy = pS.next(), pS.next()
                        slots = [(psx, k_) for k_ in range(min(nl, 4))] + ([(psy, 0)] if nl == 5 else [])
                        cslots = [(psy, 1), (psy, 2)]
                        for (p_, sl), kt in zip(slots + cslots, klist + [32, 33]):
                            pe.op(lambda e: e.matmul(p_[:, sl * 128:(sl + 1) * 128], ktb[r0:r0 + nr, kb_i, kt * 128:(kt + 1) * 128],
                                                     qt[r0:r0 + nr, qb_i, 0:128], start=True, stop=True),
                                  reads=[ktb, qt], writes=[p_], acc=True)
                        sb_ = ssb.next()
                        n1 = min(nl, 4)
                        assert bsl[:n1] == list(range(bsl[0], bsl[0] + n1))
                        dve.op(lambda e: e.tensor_tensor(out=sb_[:, 0:n1, :], in0=psx[:, 0:n1 * 128].rearrange("p (k q) -> p k q", k=n1),
                                                         in1=bt[:, bsl[0]:bsl[0] + n1, oh, :], op=ALU.add),
                               reads=[psx, bt], writes=[sb_])
                        if nl == 5:
                            dve.op(lambda e: e.tensor_tensor(out=sb_[:, 4, :], in0=psy[:, 0:128], in1=bt[:, bsl[4], oh, :], op=ALU.add),
                                   reads=[psy, bt], writes=[sb_])
                        pw = ptw.next()
                        act.op(lambda e: e.activation(out=pw[:, 0:nl, :], in_=sb_[:, 0:nl, :], func=AF.Exp), reads=[sb_], writes=[pw])
                        act.op(lambda e: e.activation(out=pw[:, 5:7, :], in_=psy[:, 128:384].rearrange("p (k q) -> p k q", k=2), func=AF.Exp),
                               reads=[psy], writes=[pw])
                        po = pO.next()
                        plist = [(k_, kt) for k_, kt in enumerate(klist)] + [(5, 32), (6, 33)]
                        for n_, (k_, kt) in enumerate(plist):
                            pe.op(lambda e: e.matmul(po[:, 0:65], pw[:, k_, :], vtb[:, kt, vh * 65:(vh + 1) * 65],
                                                     start=(n_ == 0), stop=(n_ == len(plist) - 1)),
                                  reads=[pw, vtb], writes=[po], acc=True)
                        finish_head([po], ao, oh)
                    g0 = s * TS + i * 128
                    sp.dma(AO.ap()[g0:g0 + 128, 0:256], ao[:, 0, :], reads=[ao], writes=[tAO[g0 // 128]])

                def local_tile_C(s, i):
                    qt = qtb.next()
                    sp.dma(qt[:, 0:2, 0:128], QT["C"].ap()[s, :, :, i * 128:(i + 1) * 128].rearrange("b p t -> p b t"),
                           reads=[tQKV["C"][s]], writes=[qt])
                    ao = aos.next()
                    loc = [(0, i - 1)] * (i > 0) + [(1, i)] + [(2, i + 1)] * (i < 31)
                    for (qb_i, kb_i, r0, nr, vh, oh) in heads_of("C"):
                        psx, psy = pS.next(), pS.next()
                        for (p_, sl, kt) in [(psx, sl, kt) for sl, kt in loc] + [(psy, 0, 32), (psy, 1, 33)]:
                            pe.op(lambda e: e.matmul(p_[:, sl * 128:(sl + 1) * 128], ktb[r0:r0 + nr, kb_i, kt * 128:(kt + 1) * 128],
                                                     qt[r0:r0 + nr, qb_i, 0:128], start=True, stop=True),
                                  reads=[ktb, qt], writes=[p_], acc=True)
                        sb_ = ssb.next()
                        for sl, kt in loc:
                            if sl == 1:
                                continue
                            mi = 0 if sl == 0 else 1
                            dve.op(lambda e: e.tensor_tensor(out=sb_[:, sl, :], in0=psx[:, sl * 128:(sl + 1) * 128], in1=wm[:, mi, :], op=ALU.add),
                                   reads=[psx, wm], writes=[sb_])
                        pw = ptw.next()
                        for sl, kt in loc:
                            if sl == 1:
                                act.op(lambda e: e.activation(out=pw[:, 1, :], in_=psx[:, 128:256], func=AF.Exp, scale=0.125),
                                       reads=[psx], writes=[pw])
                            else:
                                act.op(lambda e: e.activation(out=pw[:, sl, :], in_=sb_[:, sl, :], func=AF.Exp, scale=0.125),
                                       reads=[sb_], writes=[pw])
                        act.op(lambda e: e.activation(out=pw[:, 3:5, :], in_=psy[:, 0:256].rearrange("p (k q) -> p k q", k=2), func=AF.Exp, scale=0.125),
                               reads=[psy], writes=[pw])
                        po = pO.next()
                        plist = [(sl, kt) for sl, kt in loc] + [(3, 32), (4, 33)]
                        for n_, (k_, kt) in enumerate(plist):
                            pe.op(lambda e: e.matmul(po[:, 0:65], pw[:, k_, :], vtb[:, kt, vh * 65:(vh + 1) * 65],
                                                     start=(n_ == 0), stop=(n_ == len(plist) - 1)),
                                  reads=[pw, vtb], writes=[po], acc=True)
                        finish_head([po], ao, oh, sink_h=oh)
                    g0 = s * TS + i * 128
                    sp.dma(AO.ap()[g0:g0 + 128, 512:768], ao[:, 0, :], reads=[ao], writes=[tAO[g0 // 128]])

                for s in range(NS):
                    for m in ("A", "C", "B", "D"):
                        load_kv(m, s)
                        if m == "A":
                            for i in range(32):
                                local_tile_A(s, i)
                        elif m == "C":
                            for i in range(32):
                                local_tile_C(s, i)
                        else:
                            for qb in range(8):
                                dense_block(m, s, qb * 512, 512, list(range(NT)))
                        if with_ctx:
                            dense_block(m, s, TL, CT, [32, 33])

            if stop_after == "p2" and l == stop_layer:
                break
            if "AO" in dbg_out and l == 0:
                for g8 in range(0, NTOK // 128, 4):
                    sp.dma(dbg_out["AO"].ap()[g8 * 128:(g8 + 4) * 128, :], AO.ap()[g8 * 128:(g8 + 4) * 128, :],
                           reads=[tAO[g8 + t] for t in range(4)], writes=[tY])
                fw.barrier()
            tiles3 = [(s, j) for s in range(NS) for j in range(NT if with_ctx else 32)]
            with (Scope(fw) if "3" not in SKP else _Skip()) as sc:
                woutb = sc.sb("woutb", [128, 8, D], BF16)
                pool.dma(woutb[:], w_out.ap()[l].rearrange("(c p) n -> p c n", p=128), writes=[woutb])
                wr = sc.sb("wr", [128, 8, 20], F32)
                sp.dma(wr[:, :, 0:4], rg_w.ap()[l].rearrange("(c p) n -> p c n", p=128), writes=[wr])
                sp.dma(wr[:, :, 4:20], re_w.ap()[l].rearrange("(c p) n -> p c n", p=128), writes=[wr])
                rb = sc.sb("rb", [128, 20], F32)
                sp.dma(rb[:, 0:4], bcast_ap(rg_b, l * 4, 4), writes=[rb])
                sp.dma(rb[:, 4:20], bcast_ap(re_b, l * 16, 16), writes=[rb])
                bc_g1 = [sc.sb(f"bc_g1{i}", [128, D], F32) for i in range(3)]
                bc_sc2 = [sc.sb(f"bc_sc2{i}", [128, D], F32) for i in range(3)]
                bc_sh2 = [sc.sb(f"bc_sh2{i}", [128, D], F32) for i in range(3)]
                for src in range(3):
                    sp.dma(bc_g1[src][:], bcast_ap(MOD, mod_off(l, src, 2), D), reads=[tMOD[l]], writes=[bc_g1[src]])
                    sp.dma(bc_sh2[src][:], bcast_ap(MOD, mod_off(l, src, 3), D), reads=[tMOD[l]], writes=[bc_sh2[src]])
                    sp.dma(bc_sc2[src][:], bcast_ap(MOD, mod_off(l, src, 4), D), reads=[tMOD[l]], writes=[bc_sc2[src]])
                bc_lg = sc.sb("bc_lg", [128, D], F32)
                bc_lb = sc.sb("bc_lb", [128, D], F32)
                sp.dma(bc_lg[:], bcast_ap(ln1_g, l * D, D), writes=[bc_lg])
                sp.dma(bc_lb[:], bcast_ap(ln1_b, l * D, D), writes=[bc_lb])
                aoin = Rot([sc.sb(f"aoin{i}", [128, D], BF16) for i in range(2)])
                xin = Rot([sc.sb(f"xin3_{i}", [128, D], F32) for i in range(2)])
                aoT = sc.sb("aoT", [128, 8, 128], BF16)
                tA = sc.sb("tA3", [128, D], F32)
                tB = sc.sb("tB3", [128, D], F32)
                x1 = Rot([sc.sb(f"x1_{i}", [128, D], F32) for i in range(2)])
                h2 = sc.sb("h2", [128, D], F32)
                h2b = sc.sb("h2b", [128, D], BF16)
                h2Ts = Rot([sc.sb(f"h2Ts{i}", [128, 8, 128], BF16) for i in range(2)])
                h2Tf = sc.sb("h2Tf", [128, 8, 128], F32)
                stats = sc.sb("stats3", [128, 2, 6], F32)
                mv = sc.sb("mv3", [128, 2], F32)
                rstd = sc.sb("rstd3", [128, 1], F32)
                lg = sc.sb("lg", [128, 20], F32)
                r1 = sc.sb("r1", [128, 16], F32)
                r2 = sc.sb("r2", [128, 16], F32)
                oh1 = sc.sb("oh1", [128, 16], F32)
                oh2 = sc.sb("oh2", [128, 16], F32)
                sm = sc.sb("sm", [128, 8], F32)
                pT = Rot([sc.ps(f"pT3_{i}", [128, 8, 128], BF16) for i in range(2)])
                pM = sc.ps("pM", [128, D], F32)
                pF = [sc.ps(f"pF{i}", [128, 4, 128], F32) for i in range(2)]
                pL = sc.ps("pL", [128, 32], F32)

                def p3_load(s, j):
                    g = s * NT + j
                    a = aoin.next()
                    sp.dma(a[:], AO.ap()[g * 128:(g + 1) * 128, :], reads=[tAO[g]], writes=[a])
                    xt = xin.next()
                    sp.dma(xt[:], X.ap()[g * 128:(g + 1) * 128, :], reads=[tX[g]], writes=[xt])
                    return a, xt

                nxt = p3_load(*tiles3[0])
                for idx, (s, j) in enumerate(tiles3):
                    g = s * NT + j
                    src = s if j < 32 else 2
                    a, xt = nxt
                    if idx + 1 < len(tiles3):
                        nxt = p3_load(*tiles3[idx + 1])
                    pt = pT.next()
                    transposes(a, D, 128, pt, identb)
                    act.op(lambda e: e.copy(out=aoT[:], in_=pt[:]), reads=[pt], writes=[aoT])
                    for half in range(2):
                        for k in range(8):
                            pe.op(lambda e: e.matmul(pM[:, half * 512:(half + 1) * 512], aoT[:, k, :], woutb[:, k, half * 512:(half + 1) * 512],
                                                     start=(k == 0), stop=(k == 7)), reads=[aoT, woutb], writes=[pM], acc=True)
                    dve.op(lambda e: e.tensor_tensor(out=tA[:], in0=pM[:], in1=bc_g1[src][:], op=ALU.mult),
                           reads=[pM, bc_g1[src]], writes=[tA])
                    dve.op(lambda e: e.scalar_tensor_tensor(out=tA[:], in0=xt[:], scalar=ALPHA, in1=tA[:], op0=ALU.mult, op1=ALU.add),
                            reads=[xt, tA], writes=[tA])
                    layer_norm(tA, tB, stats, mv, rstd)
                    pool.op(lambda e: e.tensor_tensor(out=tB[:], in0=tB[:], in1=bc_lg[:], op=ALU.mult), reads=[tB, bc_lg], writes=[tB])
                    xo = x1.next()
                    dve.op(lambda e: e.tensor_tensor(out=xo[:], in0=tB[:], in1=bc_lb[:], op=ALU.add), reads=[tB, bc_lb], writes=[xo])
                    sp.dma(X.ap()[g * 128:(g + 1) * 128, :], xo[:], reads=[xo], writes=[tX[g]])
                    if ("x1", ) and "x1" in dbg_out and j < 32:
                        sp.dma(dbg_out["x1"].ap()[(s * 32 + j) * 128:(s * 32 + j + 1) * 128, :], xo[:], reads=[xo], writes=[tY])
                    layer_norm(xo, tA, stats, mv, rstd)
                    pool.op(lambda e: e.tensor_tensor(out=tA[:], in0=tA[:], in1=bc_sc2[src][:], op=ALU.mult),
                            reads=[tA, bc_sc2[src]], writes=[tA])
                    dve.op(lambda e: e.tensor_tensor(out=h2[:], in0=tA[:], in1=bc_sh2[src][:], op=ALU.add),
                           reads=[tA, bc_sh2[src]], writes=[h2])
                    act.op(lambda e: e.copy(out=h2b[:], in_=h2[:]), reads=[h2], writes=[h2b])
                    pt = pT.next()
                    transposes(h2b, D, 128, pt, identb)
                    hs = h2Ts.next()
                    act.op(lambda e: e.copy(out=hs[:], in_=pt[:]), reads=[pt], writes=[hs])
                    sp.dma(H2T.ap()[:, :, g * 128:(g + 1) * 128], hs[:], reads=[hs], writes=[tH2T[g]])
                    for c in range(8):
                        pe.op(lambda e: e.transpose(pF[c // 4][:, c % 4, :], h2[:, c * 128:(c + 1) * 128], identf[:]),
                              reads=[h2, identf], writes=[pF[c // 4]], acc=True)
                    for hf in range(2):
                        dve.op(lambda e: e.tensor_copy(out=h2Tf[:, hf * 4:(hf + 1) * 4, :], in_=pF[hf][:]), reads=[pF[hf]], writes=[h2Tf])
                    for k in range(8):
                        pe.op(lambda e: e.matmul(pL[:, 0:20], h2Tf[:, k, :], wr[:, k, :], start=(k == 0), stop=(k == 7)),
                              reads=[h2Tf, wr], writes=[pL], acc=True)
                    dve.op(lambda e: e.tensor_tensor(out=lg[:], in0=pL[:, 0:20], in1=rb[:], op=ALU.add), reads=[pL, rb], writes=[lg])
                    dve.op(lambda e: e.tensor_reduce(out=sm[:, 0:1], in_=lg[:, 0:4], axis=AX.X, op=ALU.max), reads=[lg], writes=[sm])
                    dve.op(lambda e: e.tensor_scalar(out=r1[:, 0:4], in0=lg[:, 0:4], scalar1=sm[:, 0:1], scalar2=None, op0=ALU.subtract),
                           reads=[lg, sm], writes=[r1])
                    act.op(lambda e: e.activation(out=r2[:, 0:4], in_=r1[:, 0:4], func=AF.Exp, accum_out=sm[:, 1:2]),
                           reads=[r1], writes=[r2, sm])
                    dve.op(lambda e: e.reciprocal(out=sm[:, 2:3], in_=sm[:, 1:2]), reads=[sm], writes=[sm])
                    dve.op(lambda e: e.tensor_scalar(out=r1[:, 4:8], in0=r1[:, 0:4], scalar1=0.0, scalar2=-1.0e9, op0=ALU.is_lt, op1=ALU.mult),
                           reads=[r1], writes=[r1])
                    dve.op(lambda e: e.tensor_tensor(out=r2[:].rearrange("p (g e) -> p g e", g=4), in0=lg[:, 4:20].rearrange("p (g e) -> p g e", g=4),
                                                     in1=r1[:, 4:8].unsqueeze(2).broadcast_to([128, 4, 4]), op=ALU.add),
                           reads=[lg, r1], writes=[r2])
                    dve.op(lambda e: e.tensor_reduce(out=sm[:, 3:4], in_=r2[:], axis=AX.X, op=ALU.max), reads=[r2], writes=[sm])
                    dve.op(lambda e: e.tensor_scalar(out=oh1[:], in0=r2[:], scalar1=sm[:, 3:4], scalar2=None, op0=ALU.is_equal),
                           reads=[r2, sm], writes=[oh1])
                    dve.op(lambda e: e.scalar_tensor_tensor(out=r1[:], in0=oh1[:], scalar=-1.0e9, in1=r2[:], op0=ALU.mult, op1=ALU.add),
                           reads=[oh1, r2], writes=[r1])
                    dve.op(lambda e: e.tensor_reduce(out=sm[:, 4:5], in_=r1[:], axis=AX.X, op=ALU.max), reads=[r1], writes=[sm])
                    dve.op(lambda e: e.tensor_scalar(out=oh2[:], in0=r1[:], scalar1=sm[:, 4:5], scalar2=None, op0=ALU.is_equal),
                           reads=[r1, sm], writes=[oh2])
                    dve.op(lambda e: e.tensor_tensor(out=sm[:, 5:6], in0=sm[:, 4:5], in1=sm[:, 3:4], op=ALU.subtract), reads=[sm], writes=[sm])
                    act.op(lambda e: e.activation(out=sm[:, 5:6], in_=sm[:, 5:6], func=AF.Exp), reads=[sm], writes=[sm])
                    dve.op(lambda e: e.tensor_scalar_add(out=sm[:, 5:6], in0=sm[:, 5:6], scalar1=1.0), reads=[sm], writes=[sm])
                    dve.op(lambda e: e.reciprocal(out=sm[:, 5:6], in_=sm[:, 5:6]), reads=[sm], writes=[sm])
                    dve.op(lambda e: e.tensor_scalar(out=sm[:, 6:7], in0=sm[:, 5:6], scalar1=-1.0, scalar2=1.0, op0=ALU.mult, op1=ALU.add),
                           reads=[sm], writes=[sm])
                    dve.op(lambda e: e.tensor_scalar(out=sm[:, 5:7], in0=sm[:, 5:7], scalar1=sm[:, 2:3], scalar2=None, op0=ALU.mult),
                           reads=[sm], writes=[sm])
                    dve.op(lambda e: e.tensor_scalar(out=oh1[:], in0=oh1[:], scalar1=sm[:, 5:6], scalar2=None, op0=ALU.mult),
                           reads=[oh1, sm], writes=[oh1])
                    dve.op(lambda e: e.scalar_tensor_tensor(out=gate_all[:, g, :], in0=oh2[:], scalar=sm[:, 6:7], in1=oh1[:], op0=ALU.mult, op1=ALU.add),
                           reads=[oh2, sm, oh1], writes=[gate_all])

            if stop_after == "p3" and l == stop_layer:
                break
            blocks = []
            for b in range(4):
                s, j0 = b // 2, (b % 2) * 16
                blocks.append([(s, j0 + t) for t in range(16)])
            if with_ctx:
                blocks.append([(0, 32), (0, 33), (1, 32), (1, 33)])
            with (Scope(fw) if "4" not in SKP else _Skip()) as sc:
                h2t = sc.sb("h2t", [128, 8, 2048], BF16)
                yacc = sc.sb("yacc", [128, 16, D], F32)
                wg = Rot([sc.sb(f"wg{i}", [128, 8, 512], BF16) for i in range(2)])
                wu = Rot([sc.sb(f"wu{i}", [128, 8, 512], BF16) for i in range(2)])
                wd = Rot([sc.sb(f"wd{i}", [128, 4, D], BF16) for i in range(2)])
                sg = Rot([sc.sb(f"sg{i}", [128, 512], BF16) for i in range(2)])
                am = Rot([sc.sb(f"am{i}", [128, 4, 512], BF16) for i in range(2)])
                bc_g2 = [sc.sb(f"bc_g2{i}", [128, D], F32) for i in range(3)]
                for src in range(3):
                    sp.dma(bc_g2[src][:], bcast_ap(MOD, mod_off(l, src, 5), D), reads=[tMOD[l]], writes=[bc_g2[src]])
                bc_lg = sc.sb("bc_lg2", [128, D], F32)
                bc_lb = sc.sb("bc_lb2", [128, D], F32)
                sp.dma(bc_lg[:], bcast_ap(ln2_g, l * D, D), writes=[bc_lg])
                sp.dma(bc_lb[:], bcast_ap(ln2_b, l * D, D), writes=[bc_lb])
                xin = Rot([sc.sb(f"xin4_{i}", [128, D], F32) for i in range(1)])
                tA = sc.sb("tA4", [128, D], F32)
                xo4 = Rot([sc.sb(f"xo4_{i}", [128, D], F32) for i in range(1)])
                stats = sc.sb("stats4", [128, 2, 6], F32)
                mv = sc.sb("mv4", [128, 2], F32)
                rstd = sc.sb("rstd4", [128, 1], F32)
                pG = Rot([sc.ps(f"pG{i}", [128, 512], F32) for i in range(2)])
                pU = Rot([sc.ps(f"pU{i}", [128, 512], F32) for i in range(2)])
                pY = Rot([sc.ps(f"pY{i}", [128, 512], F32) for i in range(4)])

                def load_w(e):
                    a, b_, c_ = wg.next(), wu.next(), wd.next()
                    pool.dma(a[:], mw_gate.ap()[l, e].rearrange("(c p) n -> p c n", p=128), writes=[a])
                    pool.dma(b_[:], mw_up.ap()[l, e].rearrange("(c p) n -> p c n", p=128), writes=[b_])
                    pool.dma(c_[:], mw_down.ap()[l, e].rearrange("(c p) n -> p c n", p=128), writes=[c_])
                    return a, b_, c_

                for blk in blocks:
                    nt_b = len(blk)
                    runs = []
                    for (s, j) in blk:
                        g = s * NT + j
                        if runs and runs[-1][0] + runs[-1][1] == g:
                            runs[-1][1] += 1
                        else:
                            runs.append([g, 1])
                    off = 0
                    for g0, n in runs:
                        sp.dma(h2t[:, :, off * 128:(off + n) * 128], H2T.ap()[:, :, g0 * 128:(g0 + n) * 128],
                               reads=[tH2T[g] for g in range(g0, g0 + n)], writes=[h2t])
                        off += n
                    gl = [s * NT + j for (s, j) in blk]
                    wnext = load_w(0)
                    for e_ in range(16):
                        wgt, wut, wdt = wnext
                        if e_ + 1 < 16:
                            wnext = load_w(e_ + 1)
                        for sb0 in range(0, nt_b, 4):
                            nsub = min(4, nt_b - sb0)
                            ntk = nsub * 128
                            at = am.next()
                            for f in range(4):
                                pg, pu = pG.next(), pU.next()
                                for k in range(8):
                                    pe.op(lambda e: e.matmul(pg[:, 0:ntk], wgt[:, k, f * 128:(f + 1) * 128], h2t[:, k, sb0 * 128:sb0 * 128 + ntk],
                                                             start=(k == 0), stop=(k == 7)), reads=[wgt, h2t], writes=[pg], acc=True)
                                for k in range(8):
                                    pe.op(lambda e: e.matmul(pu[:, 0:ntk], wut[:, k, f * 128:(f + 1) * 128], h2t[:, k, sb0 * 128:sb0 * 128 + ntk],
                                                             start=(k == 0), stop=(k == 7)), reads=[wut, h2t], writes=[pu], acc=True)
                                sgt = sg.next()
                                act.op(lambda e: e.activation(out=sgt[:, 0:ntk], in_=pg[:, 0:ntk], func=AF.Silu), reads=[pg], writes=[sgt])
                                dve.op(lambda e: e.tensor_tensor(out=at[:, f, 0:ntk], in0=sgt[:, 0:ntk], in1=pu[:, 0:ntk], op=ALU.mult),
                                       reads=[sgt, pu], writes=[at])
                            for t in range(nsub):
                                ti = sb0 + t
                                for half in range(2):
                                    py = pY.next()
                                    for f in range(4):
                                        pe.op(lambda e: e.matmul(py[:], at[:, f, t * 128:(t + 1) * 128], wdt[:, f, half * 512:(half + 1) * 512],
                                                                 start=(f == 0), stop=(f == 3)), reads=[at, wdt], writes=[py], acc=True)
                                    ya = yacc[:, ti, half * 512:(half + 1) * 512]
                                    gcol = gate_all[:, gl[ti], e_:e_ + 1]
                                    if e_ == 0:
                                        dve.op(lambda e: e.tensor_scalar(out=ya, in0=py[:], scalar1=gcol, scalar2=None, op0=ALU.mult),
                                               reads=[py, gate_all], writes=[yacc])
                                    else:
                                        dve.op(lambda e: e.scalar_tensor_tensor(out=ya, in0=py[:], scalar=gcol, in1=ya, op0=ALU.mult, op1=ALU.add),
                                               reads=[py, gate_all, yacc], writes=[yacc])
                    for ti, (s, j) in enumerate(blk):
                        g = s * NT + j
                        src = s if j < 32 else 2
                        xt = xin.next()
                        sp.dma(xt[:], X.ap()[g * 128:(g + 1) * 128, :], reads=[tX[g]], writes=[xt])
                        pool.op(lambda e: e.tensor_tensor(out=tA[:], in0=yacc[:, ti, :], in1=bc_g2[src][:], op=ALU.mult),
                                reads=[yacc, bc_g2[src]], writes=[tA])
                        dve.op(lambda e: e.scalar_tensor_tensor(out=tA[:], in0=xt[:], scalar=ALPHA, in1=tA[:], op0=ALU.mult, op1=ALU.add),
                               reads=[xt, tA], writes=[tA])
                        layer_norm(tA, tA, stats, mv, rstd)
                        pool.op(lambda e: e.tensor_tensor(out=tA[:], in0=tA[:], in1=bc_lg[:], op=ALU.mult), reads=[tA, bc_lg], writes=[tA])
                        xo = xo4.next()
                        dve.op(lambda e: e.tensor_tensor(out=xo[:], in0=tA[:], in1=bc_lb[:], op=ALU.add), reads=[tA, bc_lb], writes=[xo])
                        if single == "mid":
                            sp.dma(y_out.ap()[g * 128:(g + 1) * 128, :], xo[:], reads=[xo], writes=[tY])
                        elif l == n_layers - 1:
                            if j < 32:
                                r = (s * 32 + j) * 128
                                sp.dma(y_out.ap()[r:r + 128, :], xo[:], reads=[xo], writes=[tY])
                        else:
                            sp.dma(X.ap()[g * 128:(g + 1) * 128, :], xo[:], reads=[xo], writes=[tX[g]])

        fw.barrier()
    return nc


_CONSTS = None


def _consts():
    global _CONSTS
    if _CONSTS is None:
        C64, S64 = _rope_tables(64)
        C32, S32 = _rope_tables(32)
        kk = np.arange(128)
        wmask = np.zeros((128, 2, 128), np.float32)
        wmask[:, 0, :] = np.where(kk[:, None] >= kk[None, :], 0.0, NEG)
        wmask[:, 1, :] = np.where(kk[:, None] <= kk[None, :], 0.0, NEG)
        _CONSTS = dict(ident=np.eye(128, dtype=np.float32), ropeC64=C64, ropeS64=S64, ropeC32=C32, ropeS32=S32, wmask=wmask)
    return _CONSTS


_PER_LAYER = ("w_ada", "b_ada", "w_in", "gqa_q_gain", "gqa_k_gain", "win_sink", "mla_q_gain",
              "mla_w_qb", "mla_kv_gain", "mla_w_kvb", "w_out", "ln1_g", "ln1_b",
              "router_group_w", "router_group_b", "router_expert_w", "router_expert_b",
              "moe_w_gate", "moe_w_up", "moe_w_down", "ln2_g", "ln2_b")


def _f32(a):
    return np.ascontiguousarray(np.asarray(a, dtype=np.float32))


def make_in_maps(inputs, n_cores=8, layer=None, xcur=None):
    sl = slice(None) if layer is None else slice(layer, layer + 1)
    shared = {k: _f32(inputs[k])[sl] for k in _PER_LAYER}
    shared["nab"] = _na_bias_tables(_f32(inputs["na_bias"]))[sl]
    shared.update(_consts())
    c, c_ctx = _f32(inputs["c"]), _f32(inputs["c_ctx"])
    if xcur is None:
        x, ctx = _f32(inputs["x"]), _f32(inputs["ctx"])
    maps = []
    for i in range(n_cores):
        b0 = NS * i
        if xcur is None:
            xc = np.concatenate([np.concatenate([x[b0 + s], ctx[b0 + s]], 0) for s in range(NS)], 0)
        else:
            xc = xcur[i]
        c3 = np.stack([c[b0], c[b0 + 1], c_ctx], 0)
        c3T = np.ascontiguousarray(c3.reshape(3, 8, 128).transpose(2, 1, 0))
        m = dict(shared)
        m["x_in"] = np.ascontiguousarray(xc)
        m["c3T"] = c3T
        maps.append(m)
    return maps


_PROGS = {}


def _prog(kind):
    if kind not in _PROGS:
        _PROGS[kind] = build(single=kind)
    return _PROGS[kind]


def kernel(**inputs):
    xcur = None
    for l in range(DEPTH):
        kind = "last" if l == DEPTH - 1 else "mid"
        maps = make_in_maps(inputs, 8, layer=l, xcur=xcur)
        res = run_bass_kernel_spmd(_prog(kind), maps, core_ids=list(range(8)))
        xcur = [np.asarray(r["y"]) for r in res.results]
    out = np.concatenate([r.reshape(NS, TL, D) for r in xcur], 0)
    return out.astype(np.float32)
```

```python
import os
import numpy as np
from contextlib import ExitStack
import concourse.bass as bass
import concourse.mybir as mybir
from concourse.bass_utils import run_bass_kernel_spmd

F32 = mybir.dt.float32
BF16 = mybir.dt.bfloat16
AF = mybir.ActivationFunctionType
ALU = mybir.AluOpType
AX = mybir.AxisListType

D = 1024
TL = 4096
CT = 256
TS = TL + CT
NT = TS // 128
NS = 2
NTOK = NS * TS
DEPTH = 4
DPROJ = 2208
NEG = -30000.0
ALPHA = 8.0 ** 0.25
EPS = 1e-6
NVAR = 21


class T:
    __slots__ = ("ap", "name", "w", "r", "psum")

    def __init__(self, ap, name="", psum=False):
        self.ap = ap
        self.name = name
        self.w = {}
        self.r = {}
        self.psum = psum

    def __getitem__(self, idx):
        return self.ap[idx]


def _merge(deps, d, skip=None):
    for s, v in d.items():
        if s is skip:
            continue
        if deps.get(s, 0) < v:
            deps[s] = v


class Eng:
    def __init__(self, fw, eng, name):
        self.fw = fw
        self.eng = eng
        self.name = name
        self.sem = fw.new_sem("s_" + name)
        self.n = 0
        self.waited = {}
        self.ring = None
        self.dma_count = 0

    def need(self, deps):
        for sem, val in deps.items():
            if self.waited.get(sem, 0) < val:
                self.eng.wait_ge(sem, val)
                self.waited[sem] = val

    def op(self, build, reads=(), writes=(), acc=False):
        deps = {}
        skip = self.sem if acc else None
        for t in reads:
            _merge(deps, t.w)
            if t.psum:
                _merge(deps, t.r, self.sem)
        for t in writes:
            _merge(deps, t.w, skip)
            _merge(deps, t.r, skip)
        self.need(deps)
        ins = build(self.eng)
        self.n += 1
        ins.then_inc(self.sem, 1)
        for t in reads:
            t.r[self.sem] = self.n
        for t in writes:
            if acc:
                t.w[self.sem] = self.n
            else:
                t.w = {self.sem: self.n}
                t.r = {}
        return ins

    def dma(self, out, in_, reads=(), writes=(), **kw):
        K = len(self.ring)
        m = self.dma_count
        k, j = m % K, m // K
        deps = {}
        if j > 0:
            deps[self.ring[k]] = 16 * j
        for t in reads:
            _merge(deps, t.w)
        for t in writes:
            _merge(deps, t.w)
            _merge(deps, t.r)
        self.need(deps)
        ins = self.eng.dma_start(out=out, in_=in_, **kw)
        ins.then_inc(self.ring[k], 16)
        self.dma_count += 1
        val = 16 * (j + 1)
        for t in reads:
            t.r[self.ring[k]] = val
        for t in writes:
            t.w = {self.ring[k]: val}
            t.r = {}
        return ins


class FW:
    def __init__(self, nc, stack, ring=16):
        self.nc = nc
        self.stack = stack
        self.gen = 0
        self.pe = Eng(self, nc.tensor, "pe")
        self.act = Eng(self, nc.scalar, "act")
        self.dve = Eng(self, nc.vector, "dve")
        self.pool = Eng(self, nc.gpsimd, "pool")
        self.sp = Eng(self, nc.sync, "sp")
        self.sp.ring = [self.new_sem(f"dq_sp{i}") for i in range(int(os.environ.get('RING_SP', 16)))]
        self.pool.ring = [self.new_sem(f"dq_pl{i}") for i in range(int(os.environ.get('RING_PL', 8)))]
        self.engs = [self.pe, self.act, self.dve, self.pool, self.sp]

    def new_sem(self, name):
        return self.stack.enter_context(self.nc.semaphore(name))

    def barrier(self):
        deps = {}
        for e in self.engs:
            if e.n:
                deps[e.sem] = e.n
            if e.ring is not None and e.dma_count:
                K = len(e.ring)
                for k in range(K):
                    cnt = (e.dma_count - k + K - 1) // K
                    if cnt:
                        deps[e.ring[k]] = 16 * cnt
        for e in self.engs:
            d = {s: v for s, v in deps.items() if s is not e.sem}
            e.need(d)
        self.gen += 1
        for e in self.engs:
            if e.n > int(os.environ.get('REFRESH_MIN', '28000')):
                e.sem = self.new_sem(f"s_{e.name}_g{self.gen}")
                e.n = 0


class Scope:
    _n = 0

    def __init__(self, fw):
        self.fw = fw
        self.st = ExitStack()
        Scope._n += 1
        self.tag = f"_s{Scope._n}"

    def __enter__(self):
        self.st.__enter__()
        return self

    def __exit__(self, *a):
        self.fw.barrier()
        return self.st.__exit__(*a)

    def sb(self, name, shape, dt):
        return T(self.st.enter_context(self.fw.nc.sbuf_tensor(name + self.tag, list(shape), dt)), name)

    def ps(self, name, shape, dt):
        return T(self.st.enter_context(self.fw.nc.psum_tensor(name + self.tag, list(shape), dt)), name, psum=True)


class _SkipBody(Exception):
    pass


class _Skip:
    def __enter__(self):
        return self

    def __exit__(self, et, ev, tb):
        return et is _SkipBody

    def sb(self, *a, **k):
        raise _SkipBody()

    ps = sb


class Rot:
    def __init__(self, tiles):
        self.tiles = tiles
        self.i = 0

    def next(self):
        t = self.tiles[self.i % len(self.tiles)]
        self.i += 1
        return t


def _rope_tables(d):
    h = d // 2
    q = h // 2
    pos = np.arange(TL)
    row = (pos // 64).astype(np.float32)
    col = (pos % 64).astype(np.float32)
    freq = (10000.0 ** (-np.arange(0, h, 2, dtype=np.float32) / h)).astype(np.float32)
    C = np.ones((TL + 128, d), np.float32)
    S = np.zeros((TL + 128, d), np.float32)
    for o, p in ((0, row), (h, col)):
        ang = p[:, None] * freq[None, :]
        c, s = np.cos(ang).astype(np.float32), np.sin(ang).astype(np.float32)
        C[:TL, o:o + q] = c
        C[:TL, o + q:o + h] = c
        S[:TL, o:o + q] = -s
        S[:TL, o + q:o + h] = s
    return C, S


def _na_variants():
    keys = {}
    for i in range(32):
        rows = [2 * i, 2 * i + 1]
        ks = set()
        for r in rows:
            r0 = min(max(r - 4, 0), 56)
            for kr in range(r0, r0 + 8):
                ks.add(kr // 2)
        keys[i] = sorted(ks)
    vid = {}
    nxt = 5
    for i in range(32):
        for j in keys[i]:
            if 2 <= i <= 29:
                vid[(i, j)] = j - i + 2
            else:
                vid[(i, j)] = nxt
                nxt += 1
    assert nxt == NVAR, nxt
    return keys, vid


def _na_bias_tables(na_bias):
    keys, vid = _na_variants()
    L = na_bias.shape[0]
    out = np.full((L, NVAR, 128, 4, 128), NEG, np.float32)
    done = set()
    kk = np.arange(128)
    for (i, j), v in vid.items():
        if v in done:
            continue
        done.add(v)
        q_row = 2 * i + kk // 64
        q_col = kk % 64
        k_row = 2 * j + kk // 64
        k_col = kk % 64
        r0 = np.clip(q_row - 4, 0, 56)
        c0 = np.clip(q_col - 8, 0, 48)
        ok = ((k_row[:, None] >= r0[None, :]) & (k_row[:, None] < r0[None, :] + 8)
              & (k_col[:, None] >= c0[None, :]) & (k_col[:, None] < c0[None, :] + 16))
        dr = np.clip(k_row[:, None] - q_row[None, :] + 7, 0, 14)
        dc = np.clip(k_col[:, None] - q_col[None, :] + 15, 0, 30)
        for l in range(L):
            for h in range(4):
                g = na_bias[l, h][dr, dc]
                out[l, v, :, h, :] = np.where(ok, g, np.float32(NEG))
    return out


def build(n_layers=DEPTH, dbg=(), stop_after=None, stop_layer=0, single=None):
    DD = DEPTH if single is None else 1
    if single is not None:
        n_layers = 1
    nc = bass.Bass("TRN2", target_bir_lowering=False)

    def din(name, shape, dt=F32):
        return nc.dram_tensor(name, list(shape), dt, kind="ExternalInput")

    def dscr(name, shape, dt):
        return nc.dram_tensor(name, list(shape), dt, kind="Internal")

    x_in = din("x_in", [NTOK, D])
    c3T = din("c3T", [128, 8, 3])
    w_ada = din("w_ada", [DD, D, 6 * D])
    b_ada = din("b_ada", [DD, 6 * D])
    w_in = din("w_in", [DD, D, DPROJ])
    nab = din("nab", [DD, NVAR, 128, 4, 128])
    gq_gain = din("gqa_q_gain", [DD, 64])
    gk_gain = din("gqa_k_gain", [DD, 64])
    win_sink = din("win_sink", [DD, 4])
    mq_gain = din("mla_q_gain", [DD, 256])
    w_qb = din("mla_w_qb", [DD, 256, 384])
    mkv_gain = din("mla_kv_gain", [DD, 128])
    w_kvb = din("mla_w_kvb", [DD, 128, 512])
    w_out = din("w_out", [DD, D, D])
    ln1_g = din("ln1_g", [DD, D])
    ln1_b = din("ln1_b", [DD, D])
    rg_w = din("router_group_w", [DD, D, 4])
    rg_b = din("router_group_b", [DD, 4])
    re_w = din("router_expert_w", [DD, D, 16])
    re_b = din("router_expert_b", [DD, 16])
    mw_gate = din("moe_w_gate", [DD, 16, D, 512])
    mw_up = din("moe_w_up", [DD, 16, D, 512])
    mw_down = din("moe_w_down", [DD, 16, 512, D])
    ln2_g = din("ln2_g", [DD, D])
    ln2_b = din("ln2_b", [DD, D])
    ident_in = din("ident", [128, 128])
    ropeC64 = din("ropeC64", [TL + 128, 64])
    ropeS64 = din("ropeS64", [TL + 128, 64])
    ropeC32 = din("ropeC32", [TL + 128, 32])
    ropeS32 = din("ropeS32", [TL + 128, 32])
    wmask = din("wmask", [128, 2, 128])
    y_out = nc.dram_tensor("y", [NTOK if single == "mid" else NS * TL, D], F32, kind="ExternalOutput")

    X = dscr("X", [NTOK, D], F32)
    MOD = dscr("MOD", [DD, 3, 6 * D], F32)
    QT = {"A": dscr("QT_A", [NS, 2, 128, TS], BF16), "B": dscr("QT_B", [NS, 2, 128, TS], BF16),
          "C": dscr("QT_C", [NS, 2, 128, TS], BF16), "D": dscr("QT_D", [NS, 4, 96, TS], BF16)}
    KT = {"A": dscr("KT_A", [NS, 2, 128, TS], BF16), "B": dscr("KT_B", [NS, 1, 128, TS], BF16),
          "C": dscr("KT_C", [NS, 1, 128, TS], BF16), "D": dscr("KT_D", [NS, 4, 96, TS], BF16)}
    VV = {"A": dscr("V_A", [NS, TS, 4 * 65], BF16), "B": dscr("V_B", [NS, TS, 2 * 65], BF16),
          "C": dscr("V_C", [NS, TS, 2 * 65], BF16), "D": dscr("V_D", [NS, TS, 4 * 65], BF16)}
    AO = dscr("AO", [NTOK, D], BF16)
    H2T = dscr("H2T", [128, 8, NTOK], BF16)
    dbg_out = {}
    for nm, shp, dt in dbg:
        dbg_out[nm] = nc.dram_tensor("dbg_" + nm, list(shp), dt, kind="ExternalOutput")

    tX = [T(None, f"X{g}") for g in range(NTOK // 128)]
    tMOD = [T(None, f"MOD{l}") for l in range(DEPTH)]
    tQKV = {m: [T(None, f"qkv{m}{s}") for s in range(NS)] for m in "ABCD"}
    tAO = [T(None, f"AO{g}") for g in range(NTOK // 128)]
    tH2T = [T(None, f"H2T{g}") for g in range(NTOK // 128)]
    tY = T(None, "Y")

    def bcast_ap(handle, offset, n, parts=128):
        return bass.AP(handle, offset, [[0, parts], [1, n]])

    with ExitStack() as gst:
        fw = FW(nc, gst)
        pe, act, dve, pool, sp = fw.pe, fw.act, fw.dve, fw.pool, fw.sp

        def gsb(name, shape, dt):
            return T(gst.enter_context(nc.sbuf_tensor(name, list(shape), dt)), name)

        identf = gsb("identf", [128, 128], F32)
        identb = gsb("identb", [128, 128], BF16)
        epsb = gsb("epsb", [128, 1], F32)
        gate_all = gsb("gate_all", [128, NS * NT, 16], F32)
        sp.dma(identf[:], ident_in.ap()[:, :], writes=[identf])
        dve.op(lambda e: e.tensor_copy(out=identb[:], in_=identf[:]), reads=[identf], writes=[identb])
        dve.op(lambda e: e.memset(epsb[:], EPS), writes=[epsb])

        def layer_norm(src, dst, stats, mv, rstd, reads_extra=()):
            for c in range(2):
                dve.op(lambda e: e.bn_stats(out=stats[:, c, :], in_=src[:, c * 512:(c + 1) * 512]),
                       reads=[src], writes=[stats])
            dve.op(lambda e: e.bn_aggr(out=mv[:], in_=stats[:].rearrange("p a b -> p (a b)")),
                   reads=[stats], writes=[mv])
            act.op(lambda e: e.activation(out=rstd[:], in_=mv[:, 1:2], func=AF.Ln, bias=epsb[:, 0:1]),
                   reads=[mv, epsb], writes=[rstd])
            act.op(lambda e: e.activation(out=rstd[:], in_=rstd[:], func=AF.Exp, scale=-0.5),
                   reads=[rstd], writes=[rstd])
            dve.op(lambda e: e.tensor_scalar(out=dst[:], in0=src[:], scalar1=mv[:, 0:1], scalar2=rstd[:, 0:1],
                                             op0=ALU.subtract, op1=ALU.mult),
                   reads=[src, mv, rstd], writes=[dst])

        def transposes(src, ncols, blk, pst, ident, nparts=128):
            for c in range(ncols // blk):
                pe.op(lambda e: e.transpose(pst[0:blk, c, :], src[:, c * blk:(c + 1) * blk], ident[:]),
                      reads=[src, ident], writes=[pst], acc=True)

        def load_bc(dst, handle, offset, n=D, eng=None):
            (eng or sp).dma(dst[:, 0:n], bcast_ap(handle, offset, n), writes=[dst])

        with Scope(fw) as sc:
            c3 = sc.sb("c3", [128, 8, 3], F32)
            sc3 = sc.sb("sc3", [128, 8, 3], F32)
            bada = sc.sb("bada", [3, 6 * D], F32)
            modsb = sc.sb("modsb", [3, 6 * D], F32)
            wch = Rot([sc.sb(f"wch{i}", [128, 8, 512], F32) for i in range(2)])
            pmod = Rot([sc.ps(f"pmod{i}", [3, 512], F32) for i in range(2)])
            sp.dma(c3[:], c3T.ap()[:, :, :], writes=[c3])
            act.op(lambda e: e.activation(out=sc3[:], in_=c3[:], func=AF.Silu), reads=[c3], writes=[sc3])
            for l in range(n_layers):
                sp.dma(bada[:], bcast_ap(b_ada, l * 6 * D, 6 * D, parts=3), writes=[bada])
                for cc in range(12):
                    wt = wch.next()
                    sp.dma(wt[:], w_ada.ap()[l, :, cc * 512:(cc + 1) * 512].rearrange("(c p) n -> p c n", p=128),
                           writes=[wt])
                    pm = pmod.next()
                    for k in range(8):
                        pe.op(lambda e: e.matmul(pm[:], sc3[:, k, :], wt[:, k, :], start=(k == 0), stop=(k == 7)),
                              reads=[sc3, wt], writes=[pm], acc=True)
                    dve.op(lambda e: e.tensor_tensor(out=modsb[:, cc * 512:(cc + 1) * 512], in0=pm[:],
                                                     in1=bada[:, cc * 512:(cc + 1) * 512], op=ALU.add),
                           reads=[pm, bada], writes=[modsb])
                for o in (1, 4):
                    dve.op(lambda e: e.tensor_scalar_add(out=modsb[:, o * D:(o + 1) * D], in0=modsb[:, o * D:(o + 1) * D],
                                                         scalar1=1.0), reads=[modsb], writes=[modsb])
                sp.dma(MOD.ap()[l, :, :], modsb[:], reads=[modsb], writes=[tMOD[l]])

        if "MOD" in dbg_out:
            sp.dma(dbg_out["MOD"].ap()[:, :, :], MOD.ap()[:, :, :], reads=tMOD[:n_layers], writes=[tY])
            fw.barrier()
        if stop_after == "p0":
            n_layers = 0

        def mod_off(l, src, which):
            return (l * 3 + src) * 6 * D + which * D

        keysA, vidA = _na_variants()

        for l in range(n_layers):
            last = (l == DEPTH - 1) if single is None else (single == "last")
            with_ctx = not last
            if stop_after == "lstart" and l == stop_layer:
                break

            for p1_round in range(2 if (l > 0 and os.environ.get("P1_WARM", "0") == "1") else 1):
                with Scope(fw) as sc:
                    winb = sc.sb("winb", [128, 8, DPROJ], BF16)
                    wqbb = sc.sb("wqbb", [128, 2, 384], BF16)
                    wkvbb = sc.sb("wkvbb", [128, 512], BF16)
                    pool.dma(winb[:], w_in.ap()[l].rearrange("(c p) n -> p c n", p=128), writes=[winb])
                    pool.dma(wqbb[:], w_qb.ap()[l].rearrange("(c p) n -> p c n", p=128), writes=[wqbb])
                    pool.dma(wkvbb[:], w_kvb.ap()[l], writes=[wkvbb])
                    bc_sc = [sc.sb(f"bc_sc{i}", [128, D], F32) for i in range(3)]
                    bc_sh = [sc.sb(f"bc_sh{i}", [128, D], F32) for i in range(3)]
                    for src in range(3):
                        sp.dma(bc_sh[src][:], bcast_ap(MOD, mod_off(l, src, 0), D), reads=[tMOD[l]], writes=[bc_sh[src]])
                        sp.dma(bc_sc[src][:], bcast_ap(MOD, mod_off(l, src, 1), D), reads=[tMOD[l]], writes=[bc_sc[src]])
                    gainB = sc.sb("gainB", [128, 6, 64], F32)
                    for hh in range(4):
                        sp.dma(gainB[:, hh, :], bcast_ap(gq_gain, l * 64, 64), writes=[gainB])
                    for hh in range(2):
                        sp.dma(gainB[:, 4 + hh, :], bcast_ap(gk_gain, l * 64, 64), writes=[gainB])
                    gainQ = sc.sb("gainQ", [128, 256], F32)
                    gainKV = sc.sb("gainKV", [128, 128], F32)
                    sp.dma(gainQ[:], bcast_ap(mq_gain, l * 256, 256), writes=[gainQ])
                    sp.dma(gainKV[:], bcast_ap(mkv_gain, l * 128, 128), writes=[gainKV])

                    xin = Rot([sc.sb(f"xin{i}", [128, D], F32) for i in range(2)])
                    rC64 = Rot([sc.sb(f"rC64_{i}", [128, 64], F32) for i in range(2)])
                    rS64 = Rot([sc.sb(f"rS64_{i}", [128, 64], F32) for i in range(2)])
                    rC32 = Rot([sc.sb(f"rC32_{i}", [128, 32], F32) for i in range(2)])
                    rS32 = Rot([sc.sb(f"rS32_{i}", [128, 32], F32) for i in range(2)])
                    stats = sc.sb("stats", [128, 2, 6], F32)
                    mv = sc.sb("mv", [128, 2], F32)
                    rstd = sc.sb("rstd", [128, 1], F32)
                    tmpA = sc.sb("tmpA", [128, D], F32)
                    hb = sc.sb("hb", [128, D], BF16)
                    hT = Rot([sc.sb(f"hT{i}", [128, 8, 128], BF16) for i in range(2)])
                    sq = sc.sb("sq", [128, 384], F32)
                    ss6 = sc.sb("ss6", [128, 6], F32)
                    t1 = sc.sb("t1", [128, 6, 64], F32)
                    t2 = sc.sb("t2", [128, 6, 64], F32)
                    t3 = sc.sb("t3", [128, 6, 64], F32)
                    ssD = sc.sb("ssD", [128, 2], F32)
                    junk = sc.sb("junk", [128, 256], F32)
                    cqkv = sc.sb("cqkv", [128, 384], BF16)
                    cT = sc.sb("cT", [128, 3, 128], BF16)
                    u1 = sc.sb("u1", [128, 4, 32], F32)
                    u2 = sc.sb("u2", [128, 4, 32], F32)
                    u3 = sc.sb("u3", [128, 4, 32], F32)
                    kr1 = sc.sb("kr1", [128, 32], F32)
                    kr2 = sc.sb("kr2", [128, 32], F32)
                    qkA = Rot([sc.sb(f"qkA{i}", [128, 512], BF16) for i in range(2)])
                    qkB = Rot([sc.sb(f"qkB{i}", [128, 384], BF16) for i in range(2)])
                    qkC = Rot([sc.sb(f"qkC{i}", [128, 384], BF16) for i in range(2)])
                    qdb = Rot([sc.sb(f"qdb{i}", [128, 4, 96], BF16) for i in range(2)])
                    kdb = Rot([sc.sb(f"kdb{i}", [128, 4, 96], BF16) for i in range(2)])
                    stgA = Rot([sc.sb(f"stgA{i}", [128, 4, 128], BF16) for i in range(2)])
                    stgB = Rot([sc.sb(f"stgB{i}", [128, 3, 128], BF16) for i in range(2)])
                    stgC = Rot([sc.sb(f"stgC{i}", [128, 3, 128], BF16) for i in range(2)])
                    stgD = Rot([sc.sb(f"stgD{i}", [128, 8, 128], BF16) for i in range(2)])
                    vA = [sc.sb(f"vA{i}", [128, 4, 65], BF16) for i in range(2)]
                    vB = [sc.sb(f"vB{i}", [128, 2, 65], BF16) for i in range(2)]
                    vC = [sc.sb(f"vC{i}", [128, 2, 65], BF16) for i in range(2)]
                    vD = [sc.sb(f"vD{i}", [128, 4, 65], BF16) for i in range(2)]
                    for vt in vA + vB + vC + vD:
                        pool.op(lambda e: e.memset(vt[:], 1.0), writes=[vt])
                    vA, vB, vC, vD = Rot(vA), Rot(vB), Rot(vC), Rot(vD)
                    pT = Rot([sc.ps(f"pT{i}", [128, 8, 128], BF16) for i in range(2)])
                    pj = Rot([sc.ps(f"pj{i}", [128, 512], F32) for i in range(4)])
                    pq = Rot([sc.ps(f"pq{i}", [128, 8, 128], BF16) for i in range(2)])

                    x_src = x_in if l == 0 else X
                    order = [(s, j) for s in range(NS) for j in range(NT)]
                    P1S = float(os.environ.get("P1_STEP", "99"))
                    if P1S < 99:
                        order = order[:2]
                    if l > 0 and os.environ.get("P1_TILES"):
                        order = order[:int(os.environ["P1_TILES"])]
                    if l > 0 and os.environ.get("P1_WARM", "0") == "1" and p1_round == 0:
                        order = order[:2]

                    def p1_load(s, j):
                        g = s * NT + j
                        xt = xin.next()
                        rd = [] if l == 0 else [tX[g]]
                        sp.dma(xt[:], x_src.ap()[g * 128:(g + 1) * 128, :], reads=rd, writes=[xt])
                        r0 = j * 128 if j < 32 else TL
                        tabs = []
                        for rot, src_t, n in ((rC64, ropeC64, 64), (rS64, ropeS64, 64), (rC32, ropeC32, 32), (rS32, ropeS32, 32)):
                            tt = rot.next()
                            sp.dma(tt[:], src_t.ap()[r0:r0 + 128, :], writes=[tt])
                            tabs.append(tt)
                        return xt, tabs

                    def rope(dst_views, src, nh, dd, Ct, St, ta, tb, src_t):
                        q = dd // 4
                        Cb = Ct[:].unsqueeze(1).broadcast_to([128, nh, dd])
                        pool_or_dve = dve
                        dve.op(lambda e: e.tensor_tensor(out=ta[:, 0:nh, :], in0=src, in1=Cb, op=ALU.mult),
                               reads=[src_t, Ct], writes=[ta])
                        sv = src.rearrange("p h (r f e) -> p h r f e", r=2, f=2)
                        tv = tb[:, 0:nh, :].rearrange("p h (r f e) -> p h r f e", r=2, f=2)
                        Sv = St[:].rearrange("p (r f e) -> p r f e", r=2, f=2)
                        for f in range(2):
                            Sb = Sv[:, :, f, :].unsqueeze(1).broadcast_to([128, nh, 2, q])
                            dve.op(lambda e: e.tensor_tensor(out=tv[:, :, :, f, :], in0=sv[:, :, :, 1 - f, :], in1=Sb, op=ALU.mult),
                                   reads=[src_t, St], writes=[tb])
                        for (o_ap, a_ap, b_ap) in dst_views(ta, tb):
                            dve.op(lambda e: e.tensor_tensor(out=o_ap[0], in0=a_ap, in1=b_ap, op=ALU.add),
                                   reads=[ta, tb], writes=[o_ap[1]])

                    nxt = p1_load(*order[0])
                    for idx, (s, j) in enumerate(order):
                        g = s * NT + j
                        tok0 = j * 128
                        xt, (C64, S64, C32, S32) = nxt
                        if idx + 1 < len(order):
                            nxt = p1_load(*order[idx + 1])
                        src = s if j < 32 else 2
                        if l == 0:
                            sp.dma(X.ap()[g * 128:(g + 1) * 128, :], xt[:], reads=[xt], writes=[tX[g]])
                        if P1S < 1:
                            continue
                        layer_norm(xt, tmpA, stats, mv, rstd)
                        pool.op(lambda e: e.tensor_tensor(out=tmpA[:], in0=tmpA[:], in1=bc_sc[src][:], op=ALU.mult),
                                reads=[tmpA, bc_sc[src]], writes=[tmpA])
                        dve.op(lambda e: e.tensor_tensor(out=hb[:], in0=tmpA[:], in1=bc_sh[src][:], op=ALU.add),
                               reads=[tmpA, bc_sh[src]], writes=[hb])
                        pt = pT.next()
                        transposes(hb, D, 128, pt, identb)
                        ht = hT.next()
                        act.op(lambda e: e.copy(out=ht[:], in_=pt[:]), reads=[pt], writes=[ht])

                        def proj(c0, n):
                            p = pj.next()
                            for k in range(8):
                                pe.op(lambda e: e.matmul(p[:, 0:n], ht[:, k, :], winb[:, k, c0:c0 + n], start=(k == 0), stop=(k == 7)),
                                      reads=[ht, winb], writes=[p], acc=True)
                            return p

                        if P1S < 2:
                            continue
                        pa0 = proj(0, 512)
                        pa1 = proj(512, 256)
                        qa = qkA.next()
                        act.op(lambda e: e.activation(out=qa[:, 0:256], in_=pa0[:, 0:256], func=AF.Copy, scale=0.125),
                               reads=[pa0], writes=[qa])
                        act.op(lambda e: e.copy(out=qa[:, 256:512], in_=pa0[:, 256:512]), reads=[pa0], writes=[qa])
                        va = vA.next()
                        dve.op(lambda e: e.tensor_copy(out=va[:, :, 0:64], in_=pa1[:, 0:256].rearrange("p (h d) -> p h d", h=4)),
                               reads=[pa1], writes=[va])
                        pqa = pq.next()
                        transposes(qa, 512, 128, pqa, identb)
                        sa = stgA.next()
                        act.op(lambda e: e.copy(out=sa[:], in_=pqa[:, 0:4, :]), reads=[pqa], writes=[sa])
                        sp.dma(QT["A"].ap()[s, :, :, tok0:tok0 + 128].rearrange("b p t -> p b t"), sa[:, 0:2, :],
                               reads=[sa], writes=[tQKV["A"][s]])
                        sp.dma(KT["A"].ap()[s, :, :, tok0:tok0 + 128].rearrange("b p t -> p b t"), sa[:, 2:4, :],
                               reads=[sa], writes=[tQKV["A"][s]])
                        sp.dma(VV["A"].ap()[s, tok0:tok0 + 128, :], va[:].rearrange("p h d -> p (h d)"),
                               reads=[va], writes=[tQKV["A"][s]])

                        if P1S < 2.05:
                            continue
                        for m in ("B", "C"):
                            pb = proj(768 if m == "B" else 1280, 512)
                            qk_view = pb[:, 0:384].rearrange("p (h d) -> p h d", h=6)
                            if m == "B":
                                act.op(lambda e: e.activation(out=sq[:], in_=pb[:, 0:384], func=AF.Square), reads=[pb], writes=[sq])
                                dve.op(lambda e: e.tensor_reduce(out=ss6[:], in_=sq[:].rearrange("p (h d) -> p h d", h=6),
                                                                 axis=AX.X, op=ALU.add), reads=[sq], writes=[ss6])
                                act.op(lambda e: e.activation(out=ss6[:], in_=ss6[:], func=AF.Ln, bias=epsb[:, 0:1], scale=1.0 / 64),
                                       reads=[ss6, epsb], writes=[ss6])
                                act.op(lambda e: e.activation(out=ss6[:], in_=ss6[:], func=AF.Exp, scale=-0.5), reads=[ss6], writes=[ss6])
                                if P1S < 2.2:
                                    break
                                dve.op(lambda e: e.tensor_tensor(out=t3[:], in0=qk_view, in1=ss6[:].unsqueeze(2).broadcast_to([128, 6, 64]),
                                                                 op=ALU.mult), reads=[pb, ss6], writes=[t3])
                                dve.op(lambda e: e.tensor_tensor(out=t3[:], in0=t3[:], in1=gainB[:], op=ALU.mult),
                                       reads=[t3, gainB], writes=[t3])
                                r_src, r_t = t3[:], t3
                            else:
                                act.op(lambda e: e.copy(out=t3[:], in_=qk_view), reads=[pb], writes=[t3])
                                r_src, r_t = t3[:], t3
                            if P1S < 2.3:
                                break
                            qb_ = (qkB if m == "B" else qkC).next()

                            def views(ta, tb, qb_=qb_):
                                oq = qb_[:, 0:256].rearrange("p (gi kv d) -> p kv gi d", gi=2, kv=2)
                                aq = ta[:, 0:4, :].rearrange("p (kv gi) d -> p kv gi d", kv=2)
                                bq = tb[:, 0:4, :].rearrange("p (kv gi) d -> p kv gi d", kv=2)
                                ok = qb_[:, 256:384].rearrange("p (h d) -> p h d", h=2)
                                return [((oq, qb_), aq, bq), ((ok, qb_), ta[:, 4:6, :], tb[:, 4:6, :])]

                            rope(views, r_src, 6, 64, C64, S64, t1, t2, r_t)
                            if P1S < 2.4:
                                break
                            vb = (vB if m == "B" else vC).next()
                            act.op(lambda e: e.copy(out=vb[:, :, 0:64], in_=pb[:, 384:512].rearrange("p (h d) -> p h d", h=2)),
                                   reads=[pb], writes=[vb])
                            pqb = pq.next()
                            transposes(qb_, 384, 128, pqb, identb)
                            if P1S < 2.5:
                                break
                            sb_ = (stgB if m == "B" else stgC).next()
                            act.op(lambda e: e.copy(out=sb_[:], in_=pqb[:, 0:3, :]), reads=[pqb], writes=[sb_])
                            if P1S >= 2.6:
                                sp.dma(QT[m].ap()[s, :, :, tok0:tok0 + 128].rearrange("b p t -> p b t"), sb_[:, 0:2, :],
                                       reads=[sb_], writes=[tQKV[m][s]])
                            if P1S >= 2.7:
                                sp.dma(KT[m].ap()[s, 0, :, tok0:tok0 + 128], sb_[:, 2, :], reads=[sb_], writes=[tQKV[m][s]])
                            if P1S >= 2.8:
                                sp.dma(VV[m].ap()[s, tok0:tok0 + 128, :], vb[:].rearrange("p h d -> p (h d)"),
                                       reads=[vb], writes=[tQKV[m][s]])
                            if P1S < 2.9:
                                break

                        if P1S < 4:
                            continue
                        pd = proj(1792, 416)
                        act.op(lambda e: e.activation(out=junk[:, 0:256], in_=pd[:, 0:256], func=AF.Square, accum_out=ssD[:, 0:1]),
                               reads=[pd], writes=[junk, ssD])
                        act.op(lambda e: e.activation(out=junk[:, 0:128], in_=pd[:, 256:384], func=AF.Square, accum_out=ssD[:, 1:2]),
                               reads=[pd], writes=[junk, ssD])
                        act.op(lambda e: e.activation(out=ssD[:, 0:1], in_=ssD[:, 0:1], func=AF.Ln, bias=epsb[:, 0:1], scale=1.0 / 256),
                               reads=[ssD, epsb], writes=[ssD])
                        act.op(lambda e: e.activation(out=ssD[:, 1:2], in_=ssD[:, 1:2], func=AF.Ln, bias=epsb[:, 0:1], scale=1.0 / 128),
                               reads=[ssD, epsb], writes=[ssD])
                        act.op(lambda e: e.activation(out=ssD[:], in_=ssD[:], func=AF.Exp, scale=-0.5), reads=[ssD], writes=[ssD])
                        dve.op(lambda e: e.scalar_tensor_tensor(out=cqkv[:, 0:256], in0=pd[:, 0:256], scalar=ssD[:, 0:1], in1=gainQ[:],
                                                                op0=ALU.mult, op1=ALU.mult), reads=[pd, ssD, gainQ], writes=[cqkv])
                        dve.op(lambda e: e.scalar_tensor_tensor(out=cqkv[:, 256:384], in0=pd[:, 256:384], scalar=ssD[:, 1:2], in1=gainKV[:],
                                                                op0=ALU.mult, op1=ALU.mult), reads=[pd, ssD, gainKV], writes=[cqkv])
                        pqc = pq.next()
                        transposes(cqkv, 384, 128, pqc, identb)
                        act.op(lambda e: e.copy(out=cT[:], in_=pqc[:, 0:3, :]), reads=[pqc], writes=[cT])
                        pqd = pj.next()
                        for k in range(2):
                            pe.op(lambda e: e.matmul(pqd[:, 0:384], cT[:, k, :], wqbb[:, k, :], start=(k == 0), stop=(k == 1)),
                                  reads=[cT, wqbb], writes=[pqd], acc=True)
                        pkv = pj.next()
                        pe.op(lambda e: e.matmul(pkv[:, 0:512], cT[:, 2, :], wkvbb[:], start=True, stop=True),
                              reads=[cT, wkvbb], writes=[pkv], acc=True)
                        qd = qdb.next()
                        kd = kdb.next()
                        qv = pqd[:, 0:384].rearrange("p (h d) -> p h d", h=4)
                        kvv = pkv[:, 0:512].rearrange("p (h d) -> p h d", h=4)
                        act.op(lambda e: e.copy(out=qd[:, :, 0:64], in_=qv[:, :, 0:64]), reads=[pqd], writes=[qd])

                        def views_q(ta, tb, qd=qd):
                            return [((qd[:, :, 64:96], qd), ta[:, 0:4, :], tb[:, 0:4, :])]

                        act.op(lambda e: e.copy(out=u3[:], in_=qv[:, :, 64:96]), reads=[pqd], writes=[u3])
                        rope(views_q, u3[:], 4, 32, C32, S32, u1, u2, u3)
                        act.op(lambda e: e.copy(out=kd[:, :, 0:64], in_=kvv[:, :, 0:64]), reads=[pkv], writes=[kd])
                        act.op(lambda e: e.copy(out=kr1[:], in_=pd[:, 384:416]), reads=[pd], writes=[kr1])
                        kr_src = kr1[:].unsqueeze(1)

                        def views_k(ta, tb, kd=kd):
                            return [((kd[:, :, 64:96], kd), ta[:, 0:1, :].broadcast_to([128, 4, 32]), tb[:, 0:1, :].broadcast_to([128, 4, 32]))]

                        rope(views_k, kr_src, 1, 32, C32, S32, u1, u2, kr1)
                        vd = vD.next()
                        dve.op(lambda e: e.tensor_copy(out=vd[:, :, 0:64], in_=kvv[:, :, 64:128]), reads=[pkv], writes=[vd])
                        pqe = pq.next()
                        for h in range(4):
                            pe.op(lambda e: e.transpose(pqe[0:96, h, :], qd[:, h, :], identb[:]), reads=[qd, identb], writes=[pqe], acc=True)
                            pe.op(lambda e: e.transpose(pqe[0:96, 4 + h, :], kd[:, h, :], identb[:]), reads=[kd, identb], writes=[pqe], acc=True)
                        sd = stgD.next()
                        act.op(lambda e: e.copy(out=sd[0:96, :, :], in_=pqe[0:96, :, :]), reads=[pqe], writes=[sd])
                        sp.dma(QT["D"].ap()[s, :, :, tok0:tok0 + 128].rearrange("b p t -> p b t"), sd[0:96, 0:4, :],
                               reads=[sd], writes=[tQKV["D"][s]])
                        sp.dma(KT["D"].ap()[s, :, :, tok0:tok0 + 128].rearrange("b p t -> p b t"), sd[0:96, 4:8, :],
                               reads=[sd], writes=[tQKV["D"][s]])
                        sp.dma(VV["D"].ap()[s, tok0:tok0 + 128, :], vd[:].rearrange("p h d -> p (h d)"),
                               reads=[vd], writes=[tQKV["D"][s]])

            if stop_after == "p1" and l == stop_layer:
                break
            if os.environ.get("SKIP_REST") == "1":
                continue
            SKP = os.environ.get("SKIP_PH", "").split(",")
            with (Scope(fw) if "2" not in SKP else _Skip()) as sc:
                ktb = sc.sb("ktb", [128, 4, TS], BF16)
                vtb = sc.sb("vtb", [128, NT, 260], BF16)
                qtb = Rot([sc.sb(f"qtb{i}", [128, 4, 512], BF16) for i in range(2)])
                ptb = Rot([sc.sb(f"ptb{i}", [128, 512], BF16) for i in range(4)])
                ptw = Rot([sc.sb(f"ptw{i}", [128, 7, 128], BF16) for i in range(3)])
                ssb = Rot([sc.sb(f"ssb{i}", [128, 5, 128], F32) for i in range(2)])
                aos = Rot([sc.sb(f"aos{i}", [128, 4, 256], BF16) for i in range(2)])
                rec = Rot([sc.sb(f"rec{i}", [128, 4, 1], F32) for i in range(4)])
                biasI = sc.sb("biasI", [128, 5, 4, 128], F32)
                biasE = Rot([sc.sb(f"biasE{i}", [128, 4, 4, 128], F32) for i in range(2)])
                wm = sc.sb("wm", [128, 2, 128], F32)
                esink = sc.sb("esink", [128, 4], F32)
                pS = Rot([sc.ps(f"pS{i}", [128, 512], F32) for i in range(4)])
                pO = Rot([sc.ps(f"pO{i}", [128, 512], F32) for i in range(4)])
                sp.dma(wm[:], wmask.ap()[:, :, :], writes=[wm])
                sp.dma(esink[:], bcast_ap(win_sink, l * 4, 4), writes=[esink])
                act.op(lambda e: e.activation(out=esink[:], in_=esink[:], func=AF.Exp), reads=[esink], writes=[esink])
                sp.dma(biasI[:], nab.ap()[l, 0:5].rearrange("v k h q -> k v h q"), writes=[biasI])

                MIX = {"A": dict(nb=2, rows=128, nvh=4, col=0, scale=1.0),
                       "B": dict(nb=1, rows=128, nvh=2, col=256, scale=0.125),
                       "C": dict(nb=1, rows=128, nvh=2, col=512, scale=0.125),
                       "D": dict(nb=4, rows=96, nvh=4, col=768, scale=96.0 ** -0.5)}

                def heads_of(m):
                    if m == "A":
                        return [(c, c, r * 64, 64, 2 * c + r, 2 * c + r) for c in range(2) for r in range(2)]
                    if m in ("B", "C"):
                        return [(c, 0, r * 64, 64, r, c + 2 * r) for c in range(2) for r in range(2)]
                    return [(h, h, 0, 96, h, h) for h in range(4)]

                def finish_head(pos, ao, oh, sink_h=None):
                    for t, po in enumerate(pos):
                        rc = rec.next()
                        if sink_h is not None:
                            dve.op(lambda e: e.tensor_scalar(out=rc[:, 0, :], in0=po[:, 64:65], scalar1=esink[:, sink_h:sink_h + 1],
                                                             scalar2=None, op0=ALU.add), reads=[po, esink], writes=[rc])
                            dve.op(lambda e: e.reciprocal(out=rc[:, 0, :], in_=rc[:, 0, :]), reads=[rc], writes=[rc])
                        else:
                            dve.op(lambda e: e.reciprocal(out=rc[:, 0, :], in_=po[:, 64:65]), reads=[po], writes=[rc])
                        dve.op(lambda e: e.tensor_scalar(out=ao[:, t, oh * 64:(oh + 1) * 64], in0=po[:, 0:64], scalar1=rc[:, 0, :],
                                                         scalar2=None, op0=ALU.mult), reads=[po, rc], writes=[ao])

                def dense_block(m, s, q0, nq, key_tiles):
                    cfg = MIX[m]
                    nq_t = nq // 128
                    qt = qtb.next()
                    R = cfg["rows"]
                    sp.dma(qt[0:R, 0:(2 if m != "D" else 4), 0:nq],
                           QT[m].ap()[s, :, :, q0:q0 + nq].rearrange("b p t -> p b t"), reads=[tQKV[m][s]], writes=[qt])
                    ao = aos.next()
                    for (qb_i, kb_i, r0, nr, vh, oh) in heads_of(m):
                        pos = [pO.next() for _ in range(nq_t)]
                        for ki, kt in enumerate(key_tiles):
                            ps_ = pS.next()
                            pe.op(lambda e: e.matmul(ps_[:, 0:nq], ktb[r0:r0 + nr, kb_i, kt * 128:(kt + 1) * 128],
                                                     qt[r0:r0 + nr, qb_i, 0:nq], start=True, stop=True),
                                  reads=[ktb, qt], writes=[ps_], acc=True)
                            pt_ = ptb.next()
                            act.op(lambda e: e.activation(out=pt_[:, 0:nq], in_=ps_[:, 0:nq], func=AF.Exp, scale=cfg["scale"]),
                                   reads=[ps_], writes=[pt_])
                            for t in range(nq_t):
                                pe.op(lambda e: e.matmul(pos[t][:, 0:65], pt_[:, t * 128:(t + 1) * 128], vtb[:, kt, vh * 65:(vh + 1) * 65],
                                                         start=(ki == 0), stop=(ki == len(key_tiles) - 1)),
                                      reads=[pt_, vtb], writes=[pos[t]], acc=True)
                        finish_head(pos, ao, oh, sink_h=(oh if m == "C" else None))
                    g0 = s * TS + q0
                    sp.dma(AO.ap()[g0:g0 + nq, cfg["col"]:cfg["col"] + 256].rearrange("(t p) c -> p t c", p=128),
                           ao[:, 0:nq_t, :], reads=[ao], writes=[tAO[g0 // 128 + t] for t in range(nq_t)])

                def load_kv(m, s):
                    cfg = MIX[m]
                    R, nb = cfg["rows"], cfg["nb"]
                    sp.dma(ktb[0:R, 0:nb, :], KT[m].ap()[s].rearrange("b p t -> p b t"), reads=[tQKV[m][s]], writes=[ktb])
                    w = cfg["nvh"] * 65
                    sp.dma(vtb[:, :, 0:w], VV[m].ap()[s].rearrange("(t p) c -> p t c", p=128), reads=[tQKV[m][s]], writes=[vtb])

                def local_tile_A(s, i):
                    klist = keysA[i]
                    nl = len(klist)
                    qt = qtb.next()
                    sp.dma(qt[:, 0:2, 0:128], QT["A"].ap()[s, :, :, i * 128:(i + 1) * 128].rearrange("b p t -> p b t"),
                           reads=[tQKV["A"][s]], writes=[qt])
                    if 2 <= i <= 29:
                        bt, bsl = biasI, [vidA[(i, j)] for j in klist]
                    else:
                        bt = biasE.next()
                        v0 = vidA[(i, klist[0])]
                        sp.dma(bt[:, 0:nl], nab.ap()[l, v0:v0 + nl].rearrange("v k h q -> k v h q"), writes=[bt])
                        bsl = list(range(nl))
                    ao = aos.next()
                    for (qb_i, kb_i, r0, nr, vh, oh) in heads_of("A"):
                        psx, psy = pS.next(), pS.next()
                        slots = [(psx, k_) for k_ in range(min(nl, 4))] + ([(psy, 0)] if nl == 5 else [])
                        cslots = [(psy, 1), (psy, 2)]
                        for (p_, sl), kt in zip(slots + cslots, klist + [32, 33]):
                            pe.op(lambda e: e.matmul(p_[:, sl * 128:(sl + 1) * 128], ktb[r0:r0 + nr, kb_i, kt * 128:(kt + 1) * 128],
                                                     qt[r0:r0 + nr, qb_i, 0:128], start=True, stop=True),
                                  reads=[ktb, qt], writes=[p_], acc=True)
                        sb_ = ssb.next()
                        n1 = min(nl, 4)
                        assert bsl[:n1] == list(range(bsl[0], bsl[0] + n1))
                        dve.op(lambda e: e.tensor_tensor(out=sb_[:, 0:n1, :], in0=psx[:, 0:n1 * 128].rearrange("p (k q) -> p k q", k=n1),
                                                         in1=bt[:, bsl[0]:bsl[0] + n1, oh, :], op=ALU.add),
                               reads=[psx, bt], writes=[sb_])
                        if nl == 5:
                            dve.op(lambda e: e.tensor_tensor(out=sb_[:, 4, :], in0=psy[:, 0:128], in1=bt[:, bsl[4], oh, :], op=ALU.add),
                                   reads=[psy, bt], writes=[sb_])
                        pw = ptw.next()
                        act.op(lambda e: e.activation(out=pw[:, 0:nl, :], in_=sb_[:, 0:nl, :], func=AF.Exp), reads=[sb_], writes=[pw])
                        act.op(lambda e: e.activation(out=pw[:, 5:7, :], in_=psy[:, 128:384].rearrange("p (k q) -> p k q", k=2), func=AF.Exp),
                               reads=[psy], writes=[pw])
                        po = pO.next()
                        plist = [(k_, kt) for k_, kt in enumerate(klist)] + [(5, 32), (6, 33)]
                        for n_, (k_, kt) in enumerate(plist):
                            pe.op(lambda e: e.matmul(po[:, 0:65], pw[:, k_, :], vtb[:, kt, vh * 65:(vh + 1) * 65],
                                                     start=(n_ == 0), stop=(n_ == len(plist) - 1)),
                                  reads=[pw, vtb], writes=[po], acc=True)
                        finish_head([po], ao, oh)
                    g0 = s * TS + i * 128
                    sp.dma(AO.ap()[g0:g0 + 128, 0:256], ao[:, 0, :], reads=[ao], writes=[tAO[g0 // 128]])

                def local_tile_C(s, i):
                    qt = qtb.next()
                    sp.dma(qt[:, 0:2, 0:128], QT["C"].ap()[s, :, :, i * 128:(i + 1) * 128].rearrange("b p t -> p b t"),
                           reads=[tQKV["C"][s]], writes=[qt])
                    ao = aos.next()
                    loc = [(0, i - 1)] * (i > 0) + [(1, i)] + [(2, i + 1)] * (i < 31)
                    for (qb_i, kb_i, r0, nr, vh, oh) in heads_of("C"):
                        psx, psy = pS.next(), pS.next()
                        for (p_, sl, kt) in [(psx, sl, kt) for sl, kt in loc] + [(psy, 0, 32), (psy, 1, 33)]:
                            pe.op(lambda e: e.matmul(p_[:, sl * 128:(sl + 1) * 128], ktb[r0:r0 + nr, kb_i, kt * 128:(kt + 1) * 128],
                                                     qt[r0:r0 + nr, qb_i, 0:128], start=True, stop=True),
                                  reads=[ktb, qt], writes=[p_], acc=True)
                        sb_ = ssb.next()
                        for sl, kt in loc:
                            if sl == 1:
                                continue
                            mi = 0 if sl == 0 else 1
                            dve.op(lambda e: e.tensor_tensor(out=sb_[:, sl, :], in0=psx[:, sl * 128:(sl + 1) * 128], in1=wm[:, mi, :], op=ALU.add),
                                   reads=[psx, wm], writes=[sb_])
                        pw = ptw.next()
                        for sl, kt in loc:
                            if sl == 1:
                                act.op(lambda e: e.activation(out=pw[:, 1, :], in_=psx[:, 128:256], func=AF.Exp, scale=0.125),
                                       reads=[psx], writes=[pw])
                            else:
                                act.op(lambda e: e.activation(out=pw[:, sl, :], in_=sb_[:, sl, :], func=AF.Exp, scale=0.125),
                                       reads=[sb_], writes=[pw])
                        act.op(lambda e: e.activation(out=pw[:, 3:5, :], in_=psy[:, 0:256].rearrange("p (k q) -> p k q", k=2), func=AF.Exp, scale=0.125),
                               reads=[psy], writes=[pw])
                        po = pO.next()
                        plist = [(sl, kt) for sl, kt in loc] + [(3, 32), (4, 33)]
                        for n_, (k_, kt) in enumerate(plist):
                            pe.op(lambda e: e.matmul(po[:, 0:65], pw[:, k_, :], vtb[:, kt, vh * 65:(vh + 1) * 65],
                                                     start=(n_ == 0), stop=(n_ == len(plist) - 1)),
                                  reads=[pw, vtb], writes=[po], acc=True)
                        finish_head([po], ao, oh, sink_h=oh)
                    g0 = s * TS + i * 128
                    sp.dma(AO.ap()[g0:g0 + 128, 512:768], ao[:, 0, :], reads=[ao], writes=[tAO[g0 // 128]])

                for s in range(NS):
                    for m in ("A", "C", "B", "D"):
                        load_kv(m, s)
                        if m == "A":
                            for i in range(32):
                                local_tile_A(s, i)
                        elif m == "C":
                            for i in range(32):
                                local_tile_C(s, i)
                        else:
                            for qb in range(8):
                                dense_block(m, s, qb * 512, 512, list(range(NT)))
                        if with_ctx:
                            dense_block(m, s, TL, CT, [32, 33])

            if stop_after == "p2" and l == stop_layer:
                break
            if "AO" in dbg_out and l == 0:
                for g8 in range(0, NTOK // 128, 4):
                    sp.dma(dbg_out["AO"].ap()[g8 * 128:(g8 + 4) * 128, :], AO.ap()[g8 * 128:(g8 + 4) * 128, :],
                           reads=[tAO[g8 + t] for t in range(4)], writes=[tY])
                fw.barrier()
            tiles3 = [(s, j) for s in range(NS) for j in range(NT if with_ctx else 32)]
            with (Scope(fw) if "3" not in SKP else _Skip()) as sc:
                woutb = sc.sb("woutb", [128, 8, D], BF16)
                pool.dma(woutb[:], w_out.ap()[l].rearrange("(c p) n -> p c n", p=128), writes=[woutb])
                wr = sc.sb("wr", [128, 8, 20], F32)
                sp.dma(wr[:, :, 0:4], rg_w.ap()[l].rearrange("(c p) n -> p c n", p=128), writes=[wr])
                sp.dma(wr[:, :, 4:20], re_w.ap()[l].rearrange("(c p) n -> p c n", p=128), writes=[wr])
                rb = sc.sb("rb", [128, 20], F32)
                sp.dma(rb[:, 0:4], bcast_ap(rg_b, l * 4, 4), writes=[rb])
                sp.dma(rb[:, 4:20], bcast_ap(re_b, l * 16, 16), writes=[rb])
                bc_g1 = [sc.sb(f"bc_g1{i}", [128, D], F32) for i in range(3)]
                bc_sc2 = [sc.sb(f"bc_sc2{i}", [128, D], F32) for i in range(3)]
                bc_sh2 = [sc.sb(f"bc_sh2{i}", [128, D], F32) for i in range(3)]
                for src in range(3):
                    sp.dma(bc_g1[src][:], bcast_ap(MOD, mod_off(l, src, 2), D), reads=[tMOD[l]], writes=[bc_g1[src]])
                    sp.dma(bc_sh2[src][:], bcast_ap(MOD, mod_off(l, src, 3), D), reads=[tMOD[l]], writes=[bc_sh2[src]])
                    sp.dma(bc_sc2[src][:], bcast_ap(MOD, mod_off(l, src, 4), D), reads=[tMOD[l]], writes=[bc_sc2[src]])
                bc_lg = sc.sb("bc_lg", [128, D], F32)
                bc_lb = sc.sb("bc_lb", [128, D], F32)
                sp.dma(bc_lg[:], bcast_ap(ln1_g, l * D, D), writes=[bc_lg])
                sp.dma(bc_lb[:], bcast_ap(ln1_b, l * D, D), writes=[bc_lb])
                aoin = Rot([sc.sb(f"aoin{i}", [128, D], BF16) for i in range(2)])
                xin = Rot([sc.sb(f"xin3_{i}", [128, D], F32) for i in range(2)])
                aoT = sc.sb("aoT", [128, 8, 128], BF16)
                tA = sc.sb("tA3", [128, D], F32)
                tB = sc.sb("tB3", [128, D], F32)
                x1 = Rot([sc.sb(f"x1_{i}", [128, D], F32) for i in range(2)])
                h2 = sc.sb("h2", [128, D], F32)
                h2b = sc.sb("h2b", [128, D], BF16)
                h2Ts = Rot([sc.sb(f"h2Ts{i}", [128, 8, 128], BF16) for i in range(2)])
                h2Tf = sc.sb("h2Tf", [128, 8, 128], F32)
                stats = sc.sb("stats3", [128, 2, 6], F32)
                mv = sc.sb("mv3", [128, 2], F32)
                rstd = sc.sb("rstd3", [128, 1], F32)
                lg = sc.sb("lg", [128, 20], F32)
                r1 = sc.sb("r1", [128, 16], F32)
                r2 = sc.sb("r2", [128, 16], F32)
                oh1 = sc.sb("oh1", [128, 16], F32)
                oh2 = sc.sb("oh2", [128, 16], F32)
                sm = sc.sb("sm", [128, 8], F32)
                pT = Rot([sc.ps(f"pT3_{i}", [128, 8, 128], BF16) for i in range(2)])
                pM = sc.ps("pM", [128, D], F32)
                pF = [sc.ps(f"pF{i}", [128, 4, 128], F32) for i in range(2)]
                pL = sc.ps("pL", [128, 32], F32)

                def p3_load(s, j):
                    g = s * NT + j
                    a = aoin.next()
                    sp.dma(a[:], AO.ap()[g * 128:(g + 1) * 128, :], reads=[tAO[g]], writes=[a])
                    xt = xin.next()
                    sp.dma(xt[:], X.ap()[g * 128:(g + 1) * 128, :], reads=[tX[g]], writes=[xt])
                    return a, xt

                nxt = p3_load(*tiles3[0])
                for idx, (s, j) in enumerate(tiles3):
                    g = s * NT + j
                    src = s if j < 32 else 2
                    a, xt = nxt
                    if idx + 1 < len(tiles3):
                        nxt = p3_load(*tiles3[idx + 1])
                    pt = pT.next()
                    transposes(a, D, 128, pt, identb)
                    act.op(lambda e: e.copy(out=aoT[:], in_=pt[:]), reads=[pt], writes=[aoT])
                    for half in range(2):
                        for k in range(8):
                            pe.op(lambda e: e.matmul(pM[:, half * 512:(half + 1) * 512], aoT[:, k, :], woutb[:, k, half * 512:(half + 1) * 512],
                                                     start=(k == 0), stop=(k == 7)), reads=[aoT, woutb], writes=[pM], acc=True)
                    dve.op(lambda e: e.tensor_tensor(out=tA[:], in0=pM[:], in1=bc_g1[src][:], op=ALU.mult),
                           reads=[pM, bc_g1[src]], writes=[tA])
                    dve.op(lambda e: e.scalar_tensor_tensor(out=tA[:], in0=xt[:], scalar=ALPHA, in1=tA[:], op0=ALU.mult, op1=ALU.add),
                            reads=[xt, tA], writes=[tA])
                    layer_norm(tA, tB, stats, mv, rstd)
                    pool.op(lambda e: e.tensor_tensor(out=tB[:], in0=tB[:], in1=bc_lg[:], op=ALU.mult), reads=[tB, bc_lg], writes=[tB])
                    xo = x1.next()
                    dve.op(lambda e: e.tensor_tensor(out=xo[:], in0=tB[:], in1=bc_lb[:], op=ALU.add), reads=[tB, bc_lb], writes=[xo])
                    sp.dma(X.ap()[g * 128:(g + 1) * 128, :], xo[:], reads=[xo], writes=[tX[g]])
                    if ("x1", ) and "x1" in dbg_out and j < 32:
                        sp.dma(dbg_out["x1"].ap()[(s * 32 + j) * 128:(s * 32 + j + 1) * 128, :], xo[:], reads=[xo], writes=[tY])
                    layer_norm(xo, tA, stats, mv, rstd)
                    pool.op(lambda e: e.tensor_tensor(out=tA[:], in0=tA[:], in1=bc_sc2[src][:], op=ALU.mult),
                            reads=[tA, bc_sc2[src]], writes=[tA])
                    dve.op(lambda e: e.tensor_tensor(out=h2[:], in0=tA[:], in1=bc_sh2[src][:], op=ALU.add),
                           reads=[tA, bc_sh2[src]], writes=[h2])
                    act.op(lambda e: e.copy(out=h2b[:], in_=h2[:]), reads=[h2], writes=[h2b])
                    pt = pT.next()
                    transposes(h2b, D, 128, pt, identb)
                    hs = h2Ts.next()
                    act.op(lambda e: e.copy(out=hs[:], in_=pt[:]), reads=[pt], writes=[hs])
                    sp.dma(H2T.ap()[:, :, g * 128:(g + 1) * 128], hs[:], reads=[hs], writes=[tH2T[g]])
                    for c in range(8):
                        pe.op(lambda e: e.transpose(pF[c // 4][:, c % 4, :], h2[:, c * 128:(c + 1) * 128], identf[:]),
                              reads=[h2, identf], writes=[pF[c // 4]], acc=True)
                    for hf in range(2):
                        dve.op(lambda e: e.tensor_copy(out=h2Tf[:, hf * 4:(hf + 1) * 4, :], in_=pF[hf][:]), reads=[pF[hf]], writes=[h2Tf])
                    for k in range(8):
                        pe.op(lambda e: e.matmul(pL[:, 0:20], h2Tf[:, k, :], wr[:, k, :], start=(k == 0), stop=(k == 7)),
                              reads=[h2Tf, wr], writes=[pL], acc=True)
                    dve.op(lambda e: e.tensor_tensor(out=lg[:], in0=pL[:, 0:20], in1=rb[:], op=ALU.add), reads=[pL, rb], writes=[lg])
                    dve.op(lambda e: e.tensor_reduce(out=sm[:, 0:1], in_=lg[:, 0:4], axis=AX.X, op=ALU.max), reads=[lg], writes=[sm])
                    dve.op(lambda e: e.tensor_scalar(out=r1[:, 0:4], in0=lg[:, 0:4], scalar1=sm[:, 0:1], scalar2=None, op0=ALU.subtract),
                           reads=[lg, sm], writes=[r1])
                    act.op(lambda e: e.activation(out=r2[:, 0:4], in_=r1[:, 0:4], func=AF.Exp, accum_out=sm[:, 1:2]),
                           reads=[r1], writes=[r2, sm])
                    dve.op(lambda e: e.reciprocal(out=sm[:, 2:3], in_=sm[:, 1:2]), reads=[sm], writes=[sm])
                    dve.op(lambda e: e.tensor_scalar(out=r1[:, 4:8], in0=r1[:, 0:4], scalar1=0.0, scalar2=-1.0e9, op0=ALU.is_lt, op1=ALU.mult),
                           reads=[r1], writes=[r1])
                    dve.op(lambda e: e.tensor_tensor(out=r2[:].rearrange("p (g e) -> p g e", g=4), in0=lg[:, 4:20].rearrange("p (g e) -> p g e", g=4),
                                                     in1=r1[:, 4:8].unsqueeze(2).broadcast_to([128, 4, 4]), op=ALU.add),
                           reads=[lg, r1], writes=[r2])
                    dve.op(lambda e: e.tensor_reduce(out=sm[:, 3:4], in_=r2[:], axis=AX.X, op=ALU.max), reads=[r2], writes=[sm])
                    dve.op(lambda e: e.tensor_scalar(out=oh1[:], in0=r2[:], scalar1=sm[:, 3:4], scalar2=None, op0=ALU.is_equal),
                           reads=[r2, sm], writes=[oh1])
                    dve.op(lambda e: e.scalar_tensor_tensor(out=r1[:], in0=oh1[:], scalar=-1.0e9, in1=r2[:], op0=ALU.mult, op1=ALU.add),
                           reads=[oh1, r2], writes=[r1])
                    dve.op(lambda e: e.tensor_reduce(out=sm[:, 4:5], in_=r1[:], axis=AX.X, op=ALU.max), reads=[r1], writes=[sm])
                    dve.op(lambda e: e.tensor_scalar(out=oh2[:], in0=r1[:], scalar1=sm[:, 4:5], scalar2=None, op0=ALU.is_equal),
                           reads=[r1, sm], writes=[oh2])
                    dve.op(lambda e: e.tensor_tensor(out=sm[:, 5:6], in0=sm[:, 4:5], in1=sm[:, 3:4], op=ALU.subtract), reads=[sm], writes=[sm])
                    act.op(lambda e: e.activation(out=sm[:, 5:6], in_=sm[:, 5:6], func=AF.Exp), reads=[sm], writes=[sm])
                    dve.op(lambda e: e.tensor_scalar_add(out=sm[:, 5:6], in0=sm[:, 5:6], scalar1=1.0), reads=[sm], writes=[sm])
                    dve.op(lambda e: e.reciprocal(out=sm[:, 5:6], in_=sm[:, 5:6]), reads=[sm], writes=[sm])
                    dve.op(lambda e: e.tensor_scalar(out=sm[:, 6:7], in0=sm[:, 5:6], scalar1=-1.0, scalar2=1.0, op0=ALU.mult, op1=ALU.add),
                           reads=[sm], writes=[sm])
                    dve.op(lambda e: e.tensor_scalar(out=sm[:, 5:7], in0=sm[:, 5:7], scalar1=sm[:, 2:3], scalar2=None, op0=ALU.mult),
                           reads=[sm], writes=[sm])
                    dve.op(lambda e: e.tensor_scalar(out=oh1[:], in0=oh1[:], scalar1=sm[:, 5:6], scalar2=None, op0=ALU.mult),
                           reads=[oh1, sm], writes=[oh1])
                    dve.op(lambda e: e.scalar_tensor_tensor(out=gate_all[:, g, :], in0=oh2[:], scalar=sm[:, 6:7], in1=oh1[:], op0=ALU.mult, op1=ALU.add),
                           reads=[oh2, sm, oh1], writes=[gate_all])

            if stop_after == "p3" and l == stop_layer:
                break
            blocks = []
            for b in range(4):
                s, j0 = b // 2, (b % 2) * 16
                blocks.append([(s, j0 + t) for t in range(16)])
            if with_ctx:
                blocks.append([(0, 32), (0, 33), (1, 32), (1, 33)])
            with (Scope(fw) if "4" not in SKP else _Skip()) as sc:
                h2t = sc.sb("h2t", [128, 8, 2048], BF16)
                yacc = sc.sb("yacc", [128, 16, D], F32)
                wg = Rot([sc.sb(f"wg{i}", [128, 8, 512], BF16) for i in range(2)])
                wu = Rot([sc.sb(f"wu{i}", [128, 8, 512], BF16) for i in range(2)])
                wd = Rot([sc.sb(f"wd{i}", [128, 4, D], BF16) for i in range(2)])
                sg = Rot([sc.sb(f"sg{i}", [128, 512], BF16) for i in range(2)])
                am = Rot([sc.sb(f"am{i}", [128, 4, 512], BF16) for i in range(2)])
                bc_g2 = [sc.sb(f"bc_g2{i}", [128, D], F32) for i in range(3)]
                for src in range(3):
                    sp.dma(bc_g2[src][:], bcast_ap(MOD, mod_off(l, src, 5), D), reads=[tMOD[l]], writes=[bc_g2[src]])
                bc_lg = sc.sb("bc_lg2", [128, D], F32)
                bc_lb = sc.sb("bc_lb2", [128, D], F32)
                sp.dma(bc_lg[:], bcast_ap(ln2_g, l * D, D), writes=[bc_lg])
                sp.dma(bc_lb[:], bcast_ap(ln2_b, l * D, D), writes=[bc_lb])
                xin = Rot([sc.sb(f"xin4_{i}", [128, D], F32) for i in range(1)])
                tA = sc.sb("tA4", [128, D], F32)
                xo4 = Rot([sc.sb(f"xo4_{i}", [128, D], F32) for i in range(1)])
                stats = sc.sb("stats4", [128, 2, 6], F32)
                mv = sc.sb("mv4", [128, 2], F32)
                rstd = sc.sb("rstd4", [128, 1], F32)
                pG = Rot([sc.ps(f"pG{i}", [128, 512], F32) for i in range(2)])
                pU = Rot([sc.ps(f"pU{i}", [128, 512], F32) for i in range(2)])
                pY = Rot([sc.ps(f"pY{i}", [128, 512], F32) for i in range(4)])

                def load_w(e):
                    a, b_, c_ = wg.next(), wu.next(), wd.next()
                    pool.dma(a[:], mw_gate.ap()[l, e].rearrange("(c p) n -> p c n", p=128), writes=[a])
                    pool.dma(b_[:], mw_up.ap()[l, e].rearrange("(c p) n -> p c n", p=128), writes=[b_])
                    pool.dma(c_[:], mw_down.ap()[l, e].rearrange("(c p) n -> p c n", p=128), writes=[c_])
                    return a, b_, c_

                for blk in blocks:
                    nt_b = len(blk)
                    runs = []
                    for (s, j) in blk:
                        g = s * NT + j
                        if runs and runs[-1][0] + runs[-1][1] == g:
                            runs[-1][1] += 1
                        else:
                            runs.append([g, 1])
                    off = 0
                    for g0, n in runs:
                        sp.dma(h2t[:, :, off * 128:(off + n) * 128], H2T.ap()[:, :, g0 * 128:(g0 + n) * 128],
                               reads=[tH2T[g] for g in range(g0, g0 + n)], writes=[h2t])
                        off += n
                    gl = [s * NT + j for (s, j) in blk]
                    wnext = load_w(0)
                    for e_ in range(16):
                        wgt, wut, wdt = wnext
                        if e_ + 1 < 16:
                            wnext = load_w(e_ + 1)
                        for sb0 in range(0, nt_b, 4):
                            nsub = min(4, nt_b - sb0)
                            ntk = nsub * 128
                            at = am.next()
                            for f in range(4):
                                pg, pu = pG.next(), pU.next()
                                for k in range(8):
                                    pe.op(lambda e: e.matmul(pg[:, 0:ntk], wgt[:, k, f * 128:(f + 1) * 128], h2t[:, k, sb0 * 128:sb0 * 128 + ntk],
                                                             start=(k == 0), stop=(k == 7)), reads=[wgt, h2t], writes=[pg], acc=True)
                                for k in range(8):
                                    pe.op(lambda e: e.matmul(pu[:, 0:ntk], wut[:, k, f * 128:(f + 1) * 128], h2t[:, k, sb0 * 128:sb0 * 128 + ntk],
                                                             start=(k == 0), stop=(k == 7)), reads=[wut, h2t], writes=[pu], acc=True)
                                sgt = sg.next()
                                act.op(lambda e: e.activation(out=sgt[:, 0:ntk], in_=pg[:, 0:ntk], func=AF.Silu), reads=[pg], writes=[sgt])
                                dve.op(lambda e: e.tensor_tensor(out=at[:, f, 0:ntk], in0=sgt[:, 0:ntk], in1=pu[:, 0:ntk], op=ALU.mult),
                                       reads=[sgt, pu], writes=[at])
                            for t in range(nsub):
                                ti = sb0 + t
                                for half in range(2):
                                    py = pY.next()
                                    for f in range(4):
                                        pe.op(lambda e: e.matmul(py[:], at[:, f, t * 128:(t + 1) * 128], wdt[:, f, half * 512:(half + 1) * 512],
                                                                 start=(f == 0), stop=(f == 3)), reads=[at, wdt], writes=[py], acc=True)
                                    ya = yacc[:, ti, half * 512:(half + 1) * 512]
                                    gcol = gate_all[:, gl[ti], e_:e_ + 1]
                                    if e_ == 0:
                                        dve.op(lambda e: e.tensor_scalar(out=ya, in0=py[:], scalar1=gcol, scalar2=None, op0=ALU.mult),
                                               reads=[py, gate_all], writes=[yacc])
                                    else:
                                        dve.op(lambda e: e.scalar_tensor_tensor(out=ya, in0=py[:], scalar=gcol, in1=ya, op0=ALU.mult, op1=ALU.add),
                                               reads=[py, gate_all, yacc], writes=[yacc])
                    for ti, (s, j) in enumerate(blk):
                        g = s * NT + j
                        src = s if j < 32 else 2
                        xt = xin.next()
                        sp.dma(xt[:], X.ap()[g * 128:(g + 1) * 128, :], reads=[tX[g]], writes=[xt])
                        pool.op(lambda e: e.tensor_tensor(out=tA[:], in0=yacc[:, ti, :], in1=bc_g2[src][:], op=ALU.mult),
                                reads=[yacc, bc_g2[src]], writes=[tA])
                        dve.op(lambda e: e.scalar_tensor_tensor(out=tA[:], in0=xt[:], scalar=ALPHA, in1=tA[:], op0=ALU.mult, op1=ALU.add),
                               reads=[xt, tA], writes=[tA])
                        layer_norm(tA, tA, stats, mv, rstd)
                        pool.op(lambda e: e.tensor_tensor(out=tA[:], in0=tA[:], in1=bc_lg[:], op=ALU.mult), reads=[tA, bc_lg], writes=[tA])
                        xo = xo4.next()
                        dve.op(lambda e: e.tensor_tensor(out=xo[:], in0=tA[:], in1=bc_lb[:], op=ALU.add), reads=[tA, bc_lb], writes=[xo])
                        if single == "mid":
                            sp.dma(y_out.ap()[g * 128:(g + 1) * 128, :], xo[:], reads=[xo], writes=[tY])
                        elif l == n_layers - 1:
                            if j < 32:
                                r = (s * 32 + j) * 128
                                sp.dma(y_out.ap()[r:r + 128, :], xo[:], reads=[xo], writes=[tY])
                        else:
                            sp.dma(X.ap()[g * 128:(g + 1) * 128, :], xo[:], reads=[xo], writes=[tX[g]])

        fw.barrier()
    return nc


_CONSTS = None


def _consts():
    global _CONSTS
    if _CONSTS is None:
        C64, S64 = _rope_tables(64)
        C32, S32 = _rope_tables(32)
        kk = np.arange(128)
        wmask = np.zeros((128, 2, 128), np.float32)
        wmask[:, 0, :] = np.where(kk[:, None] >= kk[None, :], 0.0, NEG)
        wmask[:, 1, :] = np.where(kk[:, None] <= kk[None, :], 0.0, NEG)
        _CONSTS = dict(ident=np.eye(128, dtype=np.float32), ropeC64=C64, ropeS64=S64, ropeC32=C32, ropeS32=S32, wmask=wmask)
    return _CONSTS


_PER_LAYER = ("w_ada", "b_ada", "w_in", "gqa_q_gain", "gqa_k_gain", "win_sink", "mla_q_gain",
              "mla_w_qb", "mla_kv_gain", "mla_w_kvb", "w_out", "ln1_g", "ln1_b",
              "router_group_w", "router_group_b", "router_expert_w", "router_expert_b",
              "moe_w_gate", "moe_w_up", "moe_w_down", "ln2_g", "ln2_b")


def _f32(a):
    return np.ascontiguousarray(np.asarray(a, dtype=np.float32))


def make_in_maps(inputs, n_cores=8, layer=None, xcur=None):
    sl = slice(None) if layer is None else slice(layer, layer + 1)
    shared = {k: _f32(inputs[k])[sl] for k in _PER_LAYER}
    shared["nab"] = _na_bias_tables(_f32(inputs["na_bias"]))[sl]
    shared.update(_consts())
    c, c_ctx = _f32(inputs["c"]), _f32(inputs["c_ctx"])
    if xcur is None:
        x, ctx = _f32(inputs["x"]), _f32(inputs["ctx"])
    maps = []
    for i in range(n_cores):
        b0 = NS * i
        if xcur is None:
            xc = np.concatenate([np.concatenate([x[b0 + s], ctx[b0 + s]], 0) for s in range(NS)], 0)
        else:
            xc = xcur[i]
        c3 = np.stack([c[b0], c[b0 + 1], c_ctx], 0)
        c3T = np.ascontiguousarray(c3.reshape(3, 8, 128).transpose(2, 1, 0))
        m = dict(shared)
        m["x_in"] = np.ascontiguousarray(xc)
        m["c3T"] = c3T
        maps.append(m)
    return maps


_PROGS = {}


def _prog(kind):
    if kind not in _PROGS:
        _PROGS[kind] = build(single=kind)
    return _PROGS[kind]


def kernel_unfused(**inputs):
    xcur = None
    for l in range(DEPTH):
        kind = "last" if l == DEPTH - 1 else "mid"
        maps = make_in_maps(inputs, 8, layer=l, xcur=xcur)
        res = run_bass_kernel_spmd(_prog(kind), maps, core_ids=list(range(8)))
        xcur = [np.asarray(r["y"]) for r in res.results]
    out = np.concatenate([r.reshape(NS, TL, D) for r in xcur], 0)
    return out.astype(np.float32)


def kernel(**inputs):
    maps = make_in_maps(inputs, 8)
    res = run_bass_kernel_spmd(_prog(None), maps, core_ids=list(range(8)))
    out = np.concatenate([np.asarray(r["y"]).reshape(NS, TL, D) for r in res.results], 0)
    return out.astype(np.float32)
```

```python
import os
import numpy as np
from contextlib import ExitStack
import concourse.bass as bass
import concourse.mybir as mybir
from concourse.bass_utils import run_bass_kernel_spmd

F32 = mybir.dt.float32
BF16 = mybir.dt.bfloat16
AF = mybir.ActivationFunctionType
ALU = mybir.AluOpType
AX = mybir.AxisListType

D = 1024
TL = 4096
CT = 256
TS = TL + CT
NT = TS // 128
NS = 2
NTOK = NS * TS
DEPTH = 4
DPROJ = 2208
NEG = -30000.0
ALPHA = 8.0 ** 0.25
EPS = 1e-6
NVAR = 21


class T:
    __slots__ = ("ap", "name", "w", "r", "psum")

    def __init__(self, ap, name="", psum=False):
        self.ap = ap
        self.name = name
        self.w = {}
        self.r = {}
        self.psum = psum

    def __getitem__(self, idx):
        return self.ap[idx]


def _merge(deps, d, skip=None):
    for s, v in d.items():
        if s is skip:
            continue
        if deps.get(s, 0) < v:
            deps[s] = v


class Eng:
    def __init__(self, fw, eng, name):
        self.fw = fw
        self.eng = eng
        self.name = name
        self.sem = fw.new_sem("s_" + name)
        self.n = 0
        self.waited = {}
        self.ring = None
        self.dma_count = 0

    def need(self, deps):
        for sem, val in deps.items():
            if self.waited.get(sem, 0) < val:
                self.eng.wait_ge(sem, val)
                self.waited[sem] = val

    def op(self, build, reads=(), writes=(), acc=False):
        deps = {}
        skip = self.sem if acc else None
        for t in reads:
            _merge(deps, t.w)
            if t.psum:
                _merge(deps, t.r, self.sem)
        for t in writes:
            _merge(deps, t.w, skip)
            _merge(deps, t.r, skip)
        self.need(deps)
        ins = build(self.eng)
        self.n += 1
        ins.then_inc(self.sem, 1)
        for t in reads:
            t.r[self.sem] = self.n
        for t in writes:
            if acc:
                t.w[self.sem] = self.n
            else:
                t.w = {self.sem: self.n}
                t.r = {}
        return ins

    def dma(self, out, in_, reads=(), writes=(), **kw):
        K = len(self.ring)
        m = self.dma_count
        k, j = m % K, m // K
        deps = {}
        if j > 0:
            deps[self.ring[k]] = 16 * j
        for t in reads:
            _merge(deps, t.w)
        for t in writes:
            _merge(deps, t.w)
            _merge(deps, t.r)
        self.need(deps)
        ins = self.eng.dma_start(out=out, in_=in_, **kw)
        ins.then_inc(self.ring[k], 16)
        self.dma_count += 1
        val = 16 * (j + 1)
        for t in reads:
            t.r[self.ring[k]] = val
        for t in writes:
            t.w = {self.ring[k]: val}
            t.r = {}
        return ins


class FW:
    def __init__(self, nc, stack, ring=16):
        self.nc = nc
        self.stack = stack
        self.gen = 0
        self.pe = Eng(self, nc.tensor, "pe")
        self.act = Eng(self, nc.scalar, "act")
        self.dve = Eng(self, nc.vector, "dve")
        self.pool = Eng(self, nc.gpsimd, "pool")
        self.sp = Eng(self, nc.sync, "sp")
        self.sp.ring = [self.new_sem(f"dq_sp{i}") for i in range(int(os.environ.get('RING_SP', 16)))]
        self.pool.ring = [self.new_sem(f"dq_pl{i}") for i in range(int(os.environ.get('RING_PL', 8)))]
        self.engs = [self.pe, self.act, self.dve, self.pool, self.sp]

    def new_sem(self, name):
        return self.stack.enter_context(self.nc.semaphore(name))

    def barrier(self):
        deps = {}
        for e in self.engs:
            if e.n:
                deps[e.sem] = e.n
            if e.ring is not None and e.dma_count:
                K = len(e.ring)
                for k in range(K):
                    cnt = (e.dma_count - k + K - 1) // K
                    if cnt:
                        deps[e.ring[k]] = 16 * cnt
        for e in self.engs:
            d = {s: v for s, v in deps.items() if s is not e.sem}
            e.need(d)
        self.gen += 1
        for e in self.engs:
            if e.n > int(os.environ.get('REFRESH_MIN', '28000')):
                e.sem = self.new_sem(f"s_{e.name}_g{self.gen}")
                e.n = 0


class Scope:
    _n = 0

    def __init__(self, fw):
        self.fw = fw
        self.st = ExitStack()
        Scope._n += 1
        self.tag = f"_s{Scope._n}"

    def __enter__(self):
        self.st.__enter__()
        return self

    def __exit__(self, *a):
        self.fw.barrier()
        return self.st.__exit__(*a)

    def sb(self, name, shape, dt):
        return T(self.st.enter_context(self.fw.nc.sbuf_tensor(name + self.tag, list(shape), dt)), name)

    def ps(self, name, shape, dt):
        return T(self.st.enter_context(self.fw.nc.psum_tensor(name + self.tag, list(shape), dt)), name, psum=True)


class _SkipBody(Exception):
    pass


class _Skip:
    def __enter__(self):
        return self

    def __exit__(self, et, ev, tb):
        return et is _SkipBody

    def sb(self, *a, **k):
        raise _SkipBody()

    ps = sb


class Rot:
    def __init__(self, tiles):
        self.tiles = tiles
        self.i = 0

    def next(self):
        t = self.tiles[self.i % len(self.tiles)]
        self.i += 1
        return t


def _rope_tables(d):
    h = d // 2
    q = h // 2
    pos = np.arange(TL)
    row = (pos // 64).astype(np.float32)
    col = (pos % 64).astype(np.float32)
    freq = (10000.0 ** (-np.arange(0, h, 2, dtype=np.float32) / h)).astype(np.float32)
    C = np.ones((TL + 128, d), np.float32)
    S = np.zeros((TL + 128, d), np.float32)
    for o, p in ((0, row), (h, col)):
        ang = p[:, None] * freq[None, :]
        c, s = np.cos(ang).astype(np.float32), np.sin(ang).astype(np.float32)
        C[:TL, o:o + q] = c
        C[:TL, o + q:o + h] = c
        S[:TL, o:o + q] = -s
        S[:TL, o + q:o + h] = s
    return C, S


def _na_variants():
    keys = {}
    for i in range(32):
        rows = [2 * i, 2 * i + 1]
        ks = set()
        for r in rows:
            r0 = min(max(r - 4, 0), 56)
            for kr in range(r0, r0 + 8):
                ks.add(kr // 2)
        keys[i] = sorted(ks)
    vid = {}
    nxt = 5
    for i in range(32):
        for j in keys[i]:
            if 2 <= i <= 29:
                vid[(i, j)] = j - i + 2
            else:
                vid[(i, j)] = nxt
                nxt += 1
    assert nxt == NVAR, nxt
    return keys, vid


def _na_bias_tables(na_bias):
    keys, vid = _na_variants()
    L = na_bias.shape[0]
    out = np.full((L, NVAR, 128, 4, 128), NEG, np.float32)
    done = set()
    kk = np.arange(128)
    for (i, j), v in vid.items():
        if v in done:
            continue
        done.add(v)
        q_row = 2 * i + kk // 64
        q_col = kk % 64
        k_row = 2 * j + kk // 64
        k_col = kk % 64
        r0 = np.clip(q_row - 4, 0, 56)
        c0 = np.clip(q_col - 8, 0, 48)
        ok = ((k_row[:, None] >= r0[None, :]) & (k_row[:, None] < r0[None, :] + 8)
              & (k_col[:, None] >= c0[None, :]) & (k_col[:, None] < c0[None, :] + 16))
        dr = np.clip(k_row[:, None] - q_row[None, :] + 7, 0, 14)
        dc = np.clip(k_col[:, None] - q_col[None, :] + 15, 0, 30)
        for l in range(L):
            for h in range(4):
                g = na_bias[l, h][dr, dc]
                out[l, v, :, h, :] = np.where(ok, g, np.float32(NEG))
    return out


def build(n_layers=DEPTH, dbg=(), stop_after=None, stop_layer=0, single=None):
    DD = DEPTH if single is None else 1
    if single is not None:
        n_layers = 1
    nc = bass.Bass("TRN2", target_bir_lowering=False)

    def din(name, shape, dt=F32):
        return nc.dram_tensor(name, list(shape), dt, kind="ExternalInput")

    def dscr(name, shape, dt):
        return nc.dram_tensor(name, list(shape), dt, kind="Internal")

    x_in = din("x_in", [NTOK, D])
    c3T = din("c3T", [128, 8, 3])
    w_ada = din("w_ada", [DD, D, 6 * D])
    b_ada = din("b_ada", [DD, 6 * D])
    w_in = din("w_in", [DD, D, DPROJ])
    nab = din("nab", [DD, NVAR, 128, 4, 128])
    gq_gain = din("gqa_q_gain", [DD, 64])
    gk_gain = din("gqa_k_gain", [DD, 64])
    win_sink = din("win_sink", [DD, 4])
    mq_gain = din("mla_q_gain", [DD, 256])
    w_qb = din("mla_w_qb", [DD, 256, 384])
    mkv_gain = din("mla_kv_gain", [DD, 128])
    w_kvb = din("mla_w_kvb", [DD, 128, 512])
    w_out = din("w_out", [DD, D, D])
    ln1_g = din("ln1_g", [DD, D])
    ln1_b = din("ln1_b", [DD, D])
    rg_w = din("router_group_w", [DD, D, 4])
    rg_b = din("router_group_b", [DD, 4])
    re_w = din("router_expert_w", [DD, D, 16])
    re_b = din("router_expert_b", [DD, 16])
    mw_gate = din("moe_w_gate", [DD, 16, D, 512])
    mw_up = din("moe_w_up", [DD, 16, D, 512])
    mw_down = din("moe_w_down", [DD, 16, 512, D])
    ln2_g = din("ln2_g", [DD, D])
    ln2_b = din("ln2_b", [DD, D])
    ident_in = din("ident", [128, 128])
    ropeC64 = din("ropeC64", [TL + 128, 64])
    ropeS64 = din("ropeS64", [TL + 128, 64])
    ropeC32 = din("ropeC32", [TL + 128, 32])
    ropeS32 = din("ropeS32", [TL + 128, 32])
    wmask = din("wmask", [128, 2, 128])
    y_out = nc.dram_tensor("y", [NTOK if single == "mid" else NS * TL, D], F32, kind="ExternalOutput")

    X = dscr("X", [NTOK, D], F32)
    MOD = dscr("MOD", [DD, 3, 6 * D], F32)
    QT = {"A": dscr("QT_A", [NS, 2, 128, TS], BF16), "B": dscr("QT_B", [NS, 2, 128, TS], BF16),
          "C": dscr("QT_C", [NS, 2, 128, TS], BF16), "D": dscr("QT_D", [NS, 4, 96, TS], BF16)}
    KT = {"A": dscr("KT_A", [NS, 2, 128, TS], BF16), "B": dscr("KT_B", [NS, 1, 128, TS], BF16),
          "C": dscr("KT_C", [NS, 1, 128, TS], BF16), "D": dscr("KT_D", [NS, 4, 96, TS], BF16)}
    VV = {"A": dscr("V_A", [NS, TS, 4 * 65], BF16), "B": dscr("V_B", [NS, TS, 2 * 65], BF16),
          "C": dscr("V_C", [NS, TS, 2 * 65], BF16), "D": dscr("V_D", [NS, TS, 4 * 65], BF16)}
    AO = dscr("AO", [NTOK, D], BF16)
    H2T = dscr("H2T", [128, 8, NTOK], BF16)
    dbg_out = {}
    for nm, shp, dt in dbg:
        dbg_out[nm] = nc.dram_tensor("dbg_" + nm, list(shp), dt, kind="ExternalOutput")

    tX = [T(None, f"X{g}") for g in range(NTOK // 128)]
    tMOD = [T(None, f"MOD{l}") for l in range(DEPTH)]
    tQKV = {m: [T(None, f"qkv{m}{s}") for s in range(NS)] for m in "ABCD"}
    tAO = [T(None, f"AO{g}") for g in range(NTOK // 128)]
    tH2T = [T(None, f"H2T{g}") for g in range(NTOK // 128)]
    tY = T(None, "Y")

    def bcast_ap(handle, offset, n, parts=128):
        return bass.AP(handle, offset, [[0, parts], [1, n]])

    with ExitStack() as gst:
        fw = FW(nc, gst)
        pe, act, dve, pool, sp = fw.pe, fw.act, fw.dve, fw.pool, fw.sp

        def gsb(name, shape, dt):
            return T(gst.enter_context(nc.sbuf_tensor(name, list(shape), dt)), name)

        identf = gsb("identf", [128, 128], F32)
        identb = gsb("identb", [128, 128], BF16)
        epsb = gsb("epsb", [128, 1], F32)
        gate_all = gsb("gate_all", [128, NS * NT, 16], F32)
        sp.dma(identf[:], ident_in.ap()[:, :], writes=[identf])
        dve.op(lambda e: e.tensor_copy(out=identb[:], in_=identf[:]), reads=[identf], writes=[identb])
        dve.op(lambda e: e.memset(epsb[:], EPS), writes=[epsb])

        def layer_norm(src, dst, stats, mv, rstd, reads_extra=()):
            for c in range(2):
                dve.op(lambda e: e.bn_stats(out=stats[:, c, :], in_=src[:, c * 512:(c + 1) * 512]),
                       reads=[src], writes=[stats])
            dve.op(lambda e: e.bn_aggr(out=mv[:], in_=stats[:].rearrange("p a b -> p (a b)")),
                   reads=[stats], writes=[mv])
            act.op(lambda e: e.activation(out=rstd[:], in_=mv[:, 1:2], func=AF.Ln, bias=epsb[:, 0:1]),
                   reads=[mv, epsb], writes=[rstd])
            act.op(lambda e: e.activation(out=rstd[:], in_=rstd[:], func=AF.Exp, scale=-0.5),
                   reads=[rstd], writes=[rstd])
            dve.op(lambda e: e.tensor_scalar(out=dst[:], in0=src[:], scalar1=mv[:, 0:1], scalar2=rstd[:, 0:1],
                                             op0=ALU.subtract, op1=ALU.mult),
                   reads=[src, mv, rstd], writes=[dst])

        def transposes(src, ncols, blk, pst, ident, nparts=128):
            for c in range(ncols // blk):
                pe.op(lambda e: e.transpose(pst[0:blk, c, :], src[:, c * blk:(c + 1) * blk], ident[:]),
                      reads=[src, ident], writes=[pst], acc=True)

        def load_bc(dst, handle, offset, n=D, eng=None):
            (eng or sp).dma(dst[:, 0:n], bcast_ap(handle, offset, n), writes=[dst])

        with Scope(fw) as sc:
            c3 = sc.sb("c3", [128, 8, 3], F32)
            sc3 = sc.sb("sc3", [128, 8, 3], F32)
            bada = sc.sb("bada", [3, 6 * D], F32)
            modsb = sc.sb("modsb", [3, 6 * D], F32)
            wch = Rot([sc.sb(f"wch{i}", [128, 8, 512], F32) for i in range(2)])
            pmod = Rot([sc.ps(f"pmod{i}", [3, 512], F32) for i in range(2)])
            sp.dma(c3[:], c3T.ap()[:, :, :], writes=[c3])
            act.op(lambda e: e.activation(out=sc3[:], in_=c3[:], func=AF.Silu), reads=[c3], writes=[sc3])
            for l in range(n_layers):
                sp.dma(bada[:], bcast_ap(b_ada, l * 6 * D, 6 * D, parts=3), writes=[bada])
                for cc in range(12):
                    wt = wch.next()
                    sp.dma(wt[:], w_ada.ap()[l, :, cc * 512:(cc + 1) * 512].rearrange("(c p) n -> p c n", p=128),
                           writes=[wt])
                    pm = pmod.next()
                    for k in range(8):
                        pe.op(lambda e: e.matmul(pm[:], sc3[:, k, :], wt[:, k, :], start=(k == 0), stop=(k == 7)),
                              reads=[sc3, wt], writes=[pm], acc=True)
                    dve.op(lambda e: e.tensor_tensor(out=modsb[:, cc * 512:(cc + 1) * 512], in0=pm[:],
                                                     in1=bada[:, cc * 512:(cc + 1) * 512], op=ALU.add),
                           reads=[pm, bada], writes=[modsb])
                for o in (1, 4):
                    dve.op(lambda e: e.tensor_scalar_add(out=modsb[:, o * D:(o + 1) * D], in0=modsb[:, o * D:(o + 1) * D],
                                                         scalar1=1.0), reads=[modsb], writes=[modsb])
                sp.dma(MOD.ap()[l, :, :], modsb[:], reads=[modsb], writes=[tMOD[l]])

        if "MOD" in dbg_out:
            sp.dma(dbg_out["MOD"].ap()[:, :, :], MOD.ap()[:, :, :], reads=tMOD[:n_layers], writes=[tY])
            fw.barrier()
        if stop_after == "p0":
            n_layers = 0

        def mod_off(l, src, which):
            return (l * 3 + src) * 6 * D + which * D

        keysA, vidA = _na_variants()

        for l in range(n_layers):
            last = (l == DEPTH - 1) if single is None else (single == "last")
            with_ctx = not last
            if stop_after == "lstart" and l == stop_layer:
                break

            for p1_round in range(2 if (l > 0 and os.environ.get("P1_WARM", "0") == "1") else 1):
                with Scope(fw) as sc:
                    winb = sc.sb("winb", [128, 8, DPROJ], BF16)
                    wqbb = sc.sb("wqbb", [128, 2, 384], BF16)
                    wkvbb = sc.sb("wkvbb", [128, 512], BF16)
                    pool.dma(winb[:], w_in.ap()[l].rearrange("(c p) n -> p c n", p=128), writes=[winb])
                    pool.dma(wqbb[:], w_qb.ap()[l].rearrange("(c p) n -> p c n", p=128), writes=[wqbb])
                    pool.dma(wkvbb[:], w_kvb.ap()[l], writes=[wkvbb])
                    bc_sc = [sc.sb(f"bc_sc{i}", [128, D], F32) for i in range(3)]
                    bc_sh = [sc.sb(f"bc_sh{i}", [128, D], F32) for i in range(3)]
                    for src in range(3):
                        sp.dma(bc_sh[src][:], bcast_ap(MOD, mod_off(l, src, 0), D), reads=[tMOD[l]], writes=[bc_sh[src]])
                        sp.dma(bc_sc[src][:], bcast_ap(MOD, mod_off(l, src, 1), D), reads=[tMOD[l]], writes=[bc_sc[src]])
                    gainB = sc.sb("gainB", [128, 6, 64], F32)
                    for hh in range(4):
                        sp.dma(gainB[:, hh, :], bcast_ap(gq_gain, l * 64, 64), writes=[gainB])
                    for hh in range(2):
                        sp.dma(gainB[:, 4 + hh, :], bcast_ap(gk_gain, l * 64, 64), writes=[gainB])
                    gainQ = sc.sb("gainQ", [128, 256], F32)
                    gainKV = sc.sb("gainKV", [128, 128], F32)
                    sp.dma(gainQ[:], bcast_ap(mq_gain, l * 256, 256), writes=[gainQ])
                    sp.dma(gainKV[:], bcast_ap(mkv_gain, l * 128, 128), writes=[gainKV])

                    xin = Rot([sc.sb(f"xin{i}", [128, D], F32) for i in range(2)])
                    rC64 = Rot([sc.sb(f"rC64_{i}", [128, 64], F32) for i in range(2)])
                    rS64 = Rot([sc.sb(f"rS64_{i}", [128, 64], F32) for i in range(2)])
                    rC32 = Rot([sc.sb(f"rC32_{i}", [128, 32], F32) for i in range(2)])
                    rS32 = Rot([sc.sb(f"rS32_{i}", [128, 32], F32) for i in range(2)])
                    stats = sc.sb("stats", [128, 2, 6], F32)
                    mv = sc.sb("mv", [128, 2], F32)
                    rstd = sc.sb("rstd", [128, 1], F32)
                    tmpA = sc.sb("tmpA", [128, D], F32)
                    hb = sc.sb("hb", [128, D], BF16)
                    hT = Rot([sc.sb(f"hT{i}", [128, 8, 128], BF16) for i in range(2)])
                    sq = sc.sb("sq", [128, 384], F32)
                    ss6 = sc.sb("ss6", [128, 6], F32)
                    t1 = sc.sb("t1", [128, 6, 64], F32)
                    t2 = sc.sb("t2", [128, 6, 64], F32)
                    t3 = sc.sb("t3", [128, 6, 64], F32)
                    ssD = sc.sb("ssD", [128, 2], F32)
                    junk = sc.sb("junk", [128, 256], F32)
                    cqkv = sc.sb("cqkv", [128, 384], BF16)
                    cT = sc.sb("cT", [128, 3, 128], BF16)
                    u1 = sc.sb("u1", [128, 4, 32], F32)
                    u2 = sc.sb("u2", [128, 4, 32], F32)
                    u3 = sc.sb("u3", [128, 4, 32], F32)
                    kr1 = sc.sb("kr1", [128, 32], F32)
                    kr2 = sc.sb("kr2", [128, 32], F32)
                    qkA = Rot([sc.sb(f"qkA{i}", [128, 512], BF16) for i in range(2)])
                    qkB = Rot([sc.sb(f"qkB{i}", [128, 384], BF16) for i in range(2)])
                    qkC = Rot([sc.sb(f"qkC{i}", [128, 384], BF16) for i in range(2)])
                    qdb = Rot([sc.sb(f"qdb{i}", [128, 4, 96], BF16) for i in range(2)])
                    kdb = Rot([sc.sb(f"kdb{i}", [128, 4, 96], BF16) for i in range(2)])
                    stgA = Rot([sc.sb(f"stgA{i}", [128, 4, 128], BF16) for i in range(2)])
                    stgB = Rot([sc.sb(f"stgB{i}", [128, 3, 128], BF16) for i in range(2)])
                    stgC = Rot([sc.sb(f"stgC{i}", [128, 3, 128], BF16) for i in range(2)])
                    stgD = Rot([sc.sb(f"stgD{i}", [128, 8, 128], BF16) for i in range(2)])
                    vA = [sc.sb(f"vA{i}", [128, 4, 65], BF16) for i in range(2)]
                    vB = [sc.sb(f"vB{i}", [128, 2, 65], BF16) for i in range(2)]
                    vC = [sc.sb(f"vC{i}", [128, 2, 65], BF16) for i in range(2)]
                    vD = [sc.sb(f"vD{i}", [128, 4, 65], BF16) for i in range(2)]
                    for vt in vA + vB + vC + vD:
                        pool.op(lambda e: e.memset(vt[:], 1.0), writes=[vt])
                    vA, vB, vC, vD = Rot(vA), Rot(vB), Rot(vC), Rot(vD)
                    pT = Rot([sc.ps(f"pT{i}", [128, 8, 128], BF16) for i in range(2)])
                    pj = Rot([sc.ps(f"pj{i}", [128, 512], F32) for i in range(4)])
                    pq = Rot([sc.ps(f"pq{i}", [128, 8, 128], BF16) for i in range(2)])

                    x_src = x_in if l == 0 else X
                    order = [(s, j) for s in range(NS) for j in range(NT)]
                    P1S = float(os.environ.get("P1_STEP", "99"))
                    if P1S < 99:
                        order = order[:2]
                    if l > 0 and os.environ.get("P1_TILES"):
                        order = order[:int(os.environ["P1_TILES"])]
                    if l > 0 and os.environ.get("P1_WARM", "0") == "1" and p1_round == 0:
                        order = order[:2]

                    def p1_load(s, j):
                        g = s * NT + j
                        xt = xin.next()
                        rd = [] if l == 0 else [tX[g]]
                        sp.dma(xt[:], x_src.ap()[g * 128:(g + 1) * 128, :], reads=rd, writes=[xt])
                        r0 = j * 128 if j < 32 else TL
                        tabs = []
                        for rot, src_t, n in ((rC64, ropeC64, 64), (rS64, ropeS64, 64), (rC32, ropeC32, 32), (rS32, ropeS32, 32)):
                            tt = rot.next()
                            sp.dma(tt[:], src_t.ap()[r0:r0 + 128, :], writes=[tt])
                            tabs.append(tt)
                        return xt, tabs

                    def rope(dst_views, src, nh, dd, Ct, St, ta, tb, src_t):
                        q = dd // 4
                        Cb = Ct[:].unsqueeze(1).broadcast_to([128, nh, dd])
                        pool_or_dve = dve
                        dve.op(lambda e: e.tensor_tensor(out=ta[:, 0:nh, :], in0=src, in1=Cb, op=ALU.mult),
                               reads=[src_t, Ct], writes=[ta])
                        sv = src.rearrange("p h (r f e) -> p h r f e", r=2, f=2)
                        tv = tb[:, 0:nh, :].rearrange("p h (r f e) -> p h r f e", r=2, f=2)
                        Sv = St[:].rearrange("p (r f e) -> p r f e", r=2, f=2)
                        for f in range(2):
                            Sb = Sv[:, :, f, :].unsqueeze(1).broadcast_to([128, nh, 2, q])
                            dve.op(lambda e: e.tensor_tensor(out=tv[:, :, :, f, :], in0=sv[:, :, :, 1 - f, :], in1=Sb, op=ALU.mult),
                                   reads=[src_t, St], writes=[tb])
                        for (o_ap, a_ap, b_ap) in dst_views(ta, tb):
                            dve.op(lambda e: e.tensor_tensor(out=o_ap[0], in0=a_ap, in1=b_ap, op=ALU.add),
                                   reads=[ta, tb], writes=[o_ap[1]])

                    nxt = p1_load(*order[0])
                    for idx, (s, j) in enumerate(order):
                        g = s * NT + j
                        tok0 = j * 128
                        xt, (C64, S64, C32, S32) = nxt
                        if idx + 1 < len(order):
                            nxt = p1_load(*order[idx + 1])
                        src = s if j < 32 else 2
                        if l == 0:
                            sp.dma(X.ap()[g * 128:(g + 1) * 128, :], xt[:], reads=[xt], writes=[tX[g]])
                        if P1S < 1:
                            continue
                        layer_norm(xt, tmpA, stats, mv, rstd)
                        pool.op(lambda e: e.tensor_tensor(out=tmpA[:], in0=tmpA[:], in1=bc_sc[src][:], op=ALU.mult),
                                reads=[tmpA, bc_sc[src]], writes=[tmpA])
                        dve.op(lambda e: e.tensor_tensor(out=hb[:], in0=tmpA[:], in1=bc_sh[src][:], op=ALU.add),
                               reads=[tmpA, bc_sh[src]], writes=[hb])
                        pt = pT.next()
                        transposes(hb, D, 128, pt, identb)
                        ht = hT.next()
                        act.op(lambda e: e.copy(out=ht[:], in_=pt[:]), reads=[pt], writes=[ht])

                        def proj(c0, n):
                            p = pj.next()
                            for k in range(8):
                                pe.op(lambda e: e.matmul(p[:, 0:n], ht[:, k, :], winb[:, k, c0:c0 + n], start=(k == 0), stop=(k == 7)),
                                      reads=[ht, winb], writes=[p], acc=True)
                            return p

                        if P1S < 2:
                            continue
                        pa0 = proj(0, 512)
                        pa1 = proj(512, 256)
                        qa = qkA.next()
                        act.op(lambda e: e.activation(out=qa[:, 0:256], in_=pa0[:, 0:256], func=AF.Copy, scale=0.125),
                               reads=[pa0], writes=[qa])
                        act.op(lambda e: e.copy(out=qa[:, 256:512], in_=pa0[:, 256:512]), reads=[pa0], writes=[qa])
                        va = vA.next()
                        dve.op(lambda e: e.tensor_copy(out=va[:, :, 0:64], in_=pa1[:, 0:256].rearrange("p (h d) -> p h d", h=4)),
                               reads=[pa1], writes=[va])
                        pqa = pq.next()
                        transposes(qa, 512, 128, pqa, identb)
                        sa = stgA.next()
                        act.op(lambda e: e.copy(out=sa[:], in_=pqa[:, 0:4, :]), reads=[pqa], writes=[sa])
                        sp.dma(QT["A"].ap()[s, :, :, tok0:tok0 + 128].rearrange("b p t -> p b t"), sa[:, 0:2, :],
                               reads=[sa], writes=[tQKV["A"][s]])
                        sp.dma(KT["A"].ap()[s, :, :, tok0:tok0 + 128].rearrange("b p t -> p b t"), sa[:, 2:4, :],
                               reads=[sa], writes=[tQKV["A"][s]])
                        sp.dma(VV["A"].ap()[s, tok0:tok0 + 128, :], va[:].rearrange("p h d -> p (h d)"),
                               reads=[va], writes=[tQKV["A"][s]])

                        if P1S < 2.05:
                            continue
                        for m in ("B", "C"):
                            pb = proj(768 if m == "B" else 1280, 512)
                            qk_view = pb[:, 0:384].rearrange("p (h d) -> p h d", h=6)
                            if m == "B":
                                act.op(lambda e: e.activation(out=sq[:], in_=pb[:, 0:384], func=AF.Square), reads=[pb], writes=[sq])
                                dve.op(lambda e: e.tensor_reduce(out=ss6[:], in_=sq[:].rearrange("p (h d) -> p h d", h=6),
                                                                 axis=AX.X, op=ALU.add), reads=[sq], writes=[ss6])
                                act.op(lambda e: e.activation(out=ss6[:], in_=ss6[:], func=AF.Ln, bias=epsb[:, 0:1], scale=1.0 / 64),
                                       reads=[ss6, epsb], writes=[ss6])
                                act.op(lambda e: e.activation(out=ss6[:], in_=ss6[:], func=AF.Exp, scale=-0.5), reads=[ss6], writes=[ss6])
                                if P1S < 2.2:
                                    break
                                dve.op(lambda e: e.tensor_tensor(out=t3[:], in0=qk_view, in1=ss6[:].unsqueeze(2).broadcast_to([128, 6, 64]),
                                                                 op=ALU.mult), reads=[pb, ss6], writes=[t3])
                                dve.op(lambda e: e.tensor_tensor(out=t3[:], in0=t3[:], in1=gainB[:], op=ALU.mult),
                                       reads=[t3, gainB], writes=[t3])
                                r_src, r_t = t3[:], t3
                            else:
                                act.op(lambda e: e.copy(out=t3[:], in_=qk_view), reads=[pb], writes=[t3])
                                r_src, r_t = t3[:], t3
                            if P1S < 2.3:
                                break
                            qb_ = (qkB if m == "B" else qkC).next()

                            def views(ta, tb, qb_=qb_):
                                oq = qb_[:, 0:256].rearrange("p (gi kv d) -> p kv gi d", gi=2, kv=2)
                                aq = ta[:, 0:4, :].rearrange("p (kv gi) d -> p kv gi d", kv=2)
                                bq = tb[:, 0:4, :].rearrange("p (kv gi) d -> p kv gi d", kv=2)
                                ok = qb_[:, 256:384].rearrange("p (h d) -> p h d", h=2)
                                return [((oq, qb_), aq, bq), ((ok, qb_), ta[:, 4:6, :], tb[:, 4:6, :])]

                            rope(views, r_src, 6, 64, C64, S64, t1, t2, r_t)
                            if P1S < 2.4:
                                break
                            vb = (vB if m == "B" else vC).next()
                            act.op(lambda e: e.copy(out=vb[:, :, 0:64], in_=pb[:, 384:512].rearrange("p (h d) -> p h d", h=2)),
                                   reads=[pb], writes=[vb])
                            pqb = pq.next()
                            transposes(qb_, 384, 128, pqb, identb)
                            if P1S < 2.5:
                                break
                            sb_ = (stgB if m == "B" else stgC).next()
                            act.op(lambda e: e.copy(out=sb_[:], in_=pqb[:, 0:3, :]), reads=[pqb], writes=[sb_])
                            if P1S >= 2.6:
                                sp.dma(QT[m].ap()[s, :, :, tok0:tok0 + 128].rearrange("b p t -> p b t"), sb_[:, 0:2, :],
                                       reads=[sb_], writes=[tQKV[m][s]])
                            if P1S >= 2.7:
                                sp.dma(KT[m].ap()[s, 0, :, tok0:tok0 + 128], sb_[:, 2, :], reads=[sb_], writes=[tQKV[m][s]])
                            if P1S >= 2.8:
                                sp.dma(VV[m].ap()[s, tok0:tok0 + 128, :], vb[:].rearrange("p h d -> p (h d)"),
                                       reads=[vb], writes=[tQKV[m][s]])
                            if P1S < 2.9:
                                break

                        if P1S < 4:
                            continue
                        pd = proj(1792, 416)
                        act.op(lambda e: e.activation(out=junk[:, 0:256], in_=pd[:, 0:256], func=AF.Square, accum_out=ssD[:, 0:1]),
                               reads=[pd], writes=[junk, ssD])
                        act.op(lambda e: e.activation(out=junk[:, 0:128], in_=pd[:, 256:384], func=AF.Square, accum_out=ssD[:, 1:2]),
                               reads=[pd], writes=[junk, ssD])
                        act.op(lambda e: e.activation(out=ssD[:, 0:1], in_=ssD[:, 0:1], func=AF.Ln, bias=epsb[:, 0:1], scale=1.0 / 256),
                               reads=[ssD, epsb], writes=[ssD])
                        act.op(lambda e: e.activation(out=ssD[:, 1:2], in_=ssD[:, 1:2], func=AF.Ln, bias=epsb[:, 0:1], scale=1.0 / 128),
                               reads=[ssD, epsb], writes=[ssD])
                        act.op(lambda e: e.activation(out=ssD[:], in_=ssD[:], func=AF.Exp, scale=-0.5), reads=[ssD], writes=[ssD])
                        dve.op(lambda e: e.scalar_tensor_tensor(out=cqkv[:, 0:256], in0=pd[:, 0:256], scalar=ssD[:, 0:1], in1=gainQ[:],
                                                                op0=ALU.mult, op1=ALU.mult), reads=[pd, ssD, gainQ], writes=[cqkv])
                        dve.op(lambda e: e.scalar_tensor_tensor(out=cqkv[:, 256:384], in0=pd[:, 256:384], scalar=ssD[:, 1:2], in1=gainKV[:],
                                                                op0=ALU.mult, op1=ALU.mult), reads=[pd, ssD, gainKV], writes=[cqkv])
                        pqc = pq.next()
                        transposes(cqkv, 384, 128, pqc, identb)
                        act.op(lambda e: e.copy(out=cT[:], in_=pqc[:, 0:3, :]), reads=[pqc], writes=[cT])
                        pqd = pj.next()
                        for k in range(2):
                            pe.op(lambda e: e.matmul(pqd[:, 0:384], cT[:, k, :], wqbb[:, k, :], start=(k == 0), stop=(k == 1)),
                                  reads=[cT, wqbb], writes=[pqd], acc=True)
                        pkv = pj.next()
                        pe.op(lambda e: e.matmul(pkv[:, 0:512], cT[:, 2, :], wkvbb[:], start=True, stop=True),
                              reads=[cT, wkvbb], writes=[pkv], acc=True)
                        qd = qdb.next()
                        kd = kdb.next()
                        qv = pqd[:, 0:384].rearrange("p (h d) -> p h d", h=4)
                        kvv = pkv[:, 0:512].rearrange("p (h d) -> p h d", h=4)
                        act.op(lambda e: e.copy(out=qd[:, :, 0:64], in_=qv[:, :, 0:64]), reads=[pqd], writes=[qd])

                        def views_q(ta, tb, qd=qd):
                            return [((qd[:, :, 64:96], qd), ta[:, 0:4, :], tb[:, 0:4, :])]

                        act.op(lambda e: e.copy(out=u3[:], in_=qv[:, :, 64:96]), reads=[pqd], writes=[u3])
                        rope(views_q, u3[:], 4, 32, C32, S32, u1, u2, u3)
                        act.op(lambda e: e.copy(out=kd[:, :, 0:64], in_=kvv[:, :, 0:64]), reads=[pkv], writes=[kd])
                        act.op(lambda e: e.copy(out=kr1[:], in_=pd[:, 384:416]), reads=[pd], writes=[kr1])
                        kr_src = kr1[:].unsqueeze(1)

                        def views_k(ta, tb, kd=kd):
                            return [((kd[:, :, 64:96], kd), ta[:, 0:1, :].broadcast_to([128, 4, 32]), tb[:, 0:1, :].broadcast_to([128, 4, 32]))]

                        rope(views_k, kr_src, 1, 32, C32, S32, u1, u2, kr1)
                        vd = vD.next()
                        dve.op(lambda e: e.tensor_copy(out=vd[:, :, 0:64], in_=kvv[:, :, 64:128]), reads=[pkv], writes=[vd])
                        pqe = pq.next()
                        for h in range(4):
                            pe.op(lambda e: e.transpose(pqe[0:96, h, :], qd[:, h, :], identb[:]), reads=[qd, identb], writes=[pqe], acc=True)
                            pe.op(lambda e: e.transpose(pqe[0:96, 4 + h, :], kd[:, h, :], identb[:]), reads=[kd, identb], writes=[pqe], acc=True)
                        sd = stgD.next()
                        act.op(lambda e: e.copy(out=sd[0:96, :, :], in_=pqe[0:96, :, :]), reads=[pqe], writes=[sd])
                        sp.dma(QT["D"].ap()[s, :, :, tok0:tok0 + 128].rearrange("b p t -> p b t"), sd[0:96, 0:4, :],
                               reads=[sd], writes=[tQKV["D"][s]])
                        sp.dma(KT["D"].ap()[s, :, :, tok0:tok0 + 128].rearrange("b p t -> p b t"), sd[0:96, 4:8, :],
                               reads=[sd], writes=[tQKV["D"][s]])
                        sp.dma(VV["D"].ap()[s, tok0:tok0 + 128, :], vd[:].rearrange("p h d -> p (h d)"),
                               reads=[vd], writes=[tQKV["D"][s]])

            if stop_after == "p1" and l == stop_layer:
                break
            if os.environ.get("SKIP_REST") == "1":
                continue
            SKP = os.environ.get("SKIP_PH", "").split(",")
            with (Scope(fw) if "2" not in SKP else _Skip()) as sc:
                ktb = sc.sb("ktb", [128, 4, TS], BF16)
                vtb = sc.sb("vtb", [128, NT, 260], BF16)
                qtb = Rot([sc.sb(f"qtb{i}", [128, 4, 512], BF16) for i in range(2)])
                ptb = Rot([sc.sb(f"ptb{i}", [128, 512], BF16) for i in range(4)])
                ptw = Rot([sc.sb(f"ptw{i}", [128, 7, 128], BF16) for i in range(3)])
                ssb = Rot([sc.sb(f"ssb{i}", [128, 5, 128], F32) for i in range(2)])
                aos = Rot([sc.sb(f"aos{i}", [128, 4, 256], BF16) for i in range(2)])
                rec = Rot([sc.sb(f"rec{i}", [128, 4, 1], F32) for i in range(4)])
                biasI = sc.sb("biasI", [128, 5, 4, 128], F32)
                biasE = Rot([sc.sb(f"biasE{i}", [128, 4, 4, 128], F32) for i in range(2)])
                wm = sc.sb("wm", [128, 2, 128], F32)
                esink = sc.sb("esink", [128, 4], F32)
                pS = Rot([sc.ps(f"pS{i}", [128, 512], F32) for i in range(4)])
                pO = Rot([sc.ps(f"pO{i}", [128, 512], F32) for i in range(4)])
                sp.dma(wm[:], wmask.ap()[:, :, :], writes=[wm])
                sp.dma(esink[:], bcast_ap(win_sink, l * 4, 4), writes=[esink])
                act.op(lambda e: e.activation(out=esink[:], in_=esink[:], func=AF.Exp), reads=[esink], writes=[esink])
                sp.dma(biasI[:], nab.ap()[l, 0:5].rearrange("v k h q -> k v h q"), writes=[biasI])

                MIX = {"A": dict(nb=2, rows=128, nvh=4, col=0, scale=1.0),
                       "B": dict(nb=1, rows=128, nvh=2, col=256, scale=0.125),
                       "C": dict(nb=1, rows=128, nvh=2, col=512, scale=0.125),
                       "D": dict(nb=4, rows=96, nvh=4, col=768, scale=96.0 ** -0.5)}

                def heads_of(m):
                    if m == "A":
                        return [(c, c, r * 64, 64, 2 * c + r, 2 * c + r) for c in range(2) for r in range(2)]
                    if m in ("B", "C"):
                        return [(c, 0, r * 64, 64, r, c + 2 * r) for c in range(2) for r in range(2)]
                    return [(h, h, 0, 96, h, h) for h in range(4)]

                def finish_head(pos, ao, oh, sink_h=None):
                    for t, po in enumerate(pos):
                        rc = rec.next()
                        if sink_h is not None:
                            dve.op(lambda e: e.tensor_scalar(out=rc[:, 0, :], in0=po[:, 64:65], scalar1=esink[:, sink_h:sink_h + 1],
                                                             scalar2=None, op0=ALU.add), reads=[po, esink], writes=[rc])
                            dve.op(lambda e: e.reciprocal(out=rc[:, 0, :], in_=rc[:, 0, :]), reads=[rc], writes=[rc])
                        else:
                            dve.op(lambda e: e.reciprocal(out=rc[:, 0, :], in_=po[:, 64:65]), reads=[po], writes=[rc])
                        dve.op(lambda e: e.tensor_scalar(out=ao[:, t, oh * 64:(oh + 1) * 64], in0=po[:, 0:64], scalar1=rc[:, 0, :],
                                                         scalar2=None, op0=ALU.mult), reads=[po, rc], writes=[ao])

                def dense_block(m, s, q0, nq, key_tiles):
                    cfg = MIX[m]
                    nq_t = nq // 128
                    qt = qtb.next()
                    R = cfg["rows"]
                    sp.dma(qt[0:R, 0:(2 if m != "D" else 4), 0:nq],
                           QT[m].ap()[s, :, :, q0:q0 + nq].rearrange("b p t -> p b t"), reads=[tQKV[m][s]], writes=[qt])
                    ao = aos.next()
                    nk = len(key_tiles)
                    items = [(hd, ki, kt) for hd in heads_of(m) for ki, kt in enumerate(key_tiles)]
                    cur = {}

                    def stage_s(it):
                        (qb_i, kb_i, r0, nr, vh, oh), ki, kt = it
                        ps_ = pS.next()
                        pe.op(lambda e: e.matmul(ps_[:, 0:nq], ktb[r0:r0 + nr, kb_i, kt * 128:(kt + 1) * 128],
                                                 qt[r0:r0 + nr, qb_i, 0:nq], start=True, stop=True),
                              reads=[ktb, qt], writes=[ps_], acc=True)
                        pt_ = ptb.next()
                        act.op(lambda e: e.activation(out=pt_[:, 0:nq], in_=ps_[:, 0:nq], func=AF.Exp, scale=cfg["scale"]),
                               reads=[ps_], writes=[pt_])
                        return pt_

                    def stage_pv(it, pt_):
                        (qb_i, kb_i, r0, nr, vh, oh), ki, kt = it
                        if ki == 0:
                            cur["pos"] = [pO.next() for _ in range(nq_t)]
                        pos = cur["pos"]
                        for t in range(nq_t):
                            pe.op(lambda e: e.matmul(pos[t][:, 0:65], pt_[:, t * 128:(t + 1) * 128], vtb[:, kt, vh * 65:(vh + 1) * 65],
                                                     start=(ki == 0), stop=(ki == nk - 1)),
                                  reads=[pt_, vtb], writes=[pos[t]], acc=True)
                        if ki == nk - 1:
                            finish_head(pos, ao, oh, sink_h=(oh if m == "C" else None))

                    pend = []
                    for it in items:
                        pend.append((it, stage_s(it)))
                        if len(pend) > 2:
                            stage_pv(*pend.pop(0))
                    while pend:
                        stage_pv(*pend.pop(0))
                    g0 = s * TS + q0
                    sp.dma(AO.ap()[g0:g0 + nq, cfg["col"]:cfg["col"] + 256].rearrange("(t p) c -> p t c", p=128),
                           ao[:, 0:nq_t, :], reads=[ao], writes=[tAO[g0 // 128 + t] for t in range(nq_t)])

                pend_l = []

                def push_l(fn):
                    pend_l.append(fn)
                    if len(pend_l) > 1:
                        pend_l.pop(0)()

                def flush_l():
                    while pend_l:
                        pend_l.pop(0)()

                def load_kv(m, s):
                    flush_l()
                    cfg = MIX[m]
                    R, nb = cfg["rows"], cfg["nb"]
                    sp.dma(ktb[0:R, 0:nb, :], KT[m].ap()[s].rearrange("b p t -> p b t"), reads=[tQKV[m][s]], writes=[ktb])
                    w = cfg["nvh"] * 65
                    sp.dma(vtb[:, :, 0:w], VV[m].ap()[s].rearrange("(t p) c -> p t c", p=128), reads=[tQKV[m][s]], writes=[vtb])

                def local_tile_A(s, i):
                    klist = keysA[i]
                    nl = len(klist)
                    qt = qtb.next()
                    sp.dma(qt[:, 0:2, 0:128], QT["A"].ap()[s, :, :, i * 128:(i + 1) * 128].rearrange("b p t -> p b t"),
                           reads=[tQKV["A"][s]], writes=[qt])
                    if 2 <= i <= 29:
                        bt, bsl = biasI, [vidA[(i, j)] for j in klist]
                    else:
                        bt = biasE.next()
                        v0 = vidA[(i, klist[0])]
                        sp.dma(bt[:, 0:nl], nab.ap()[l, v0:v0 + nl].rearrange("v k h q -> k v h q"), writes=[bt])
                        bsl = list(range(nl))
                    ao = aos.next()
                    for (qb_i, kb_i, r0, nr, vh, oh) in heads_of("A"):
                        psx, psy = pS.next(), pS.next()
                        slots = [(psx, k_) for k_ in range(min(nl, 4))] + ([(psy, 0)] if nl == 5 else [])
                        cslots = [(psy, 1), (psy, 2)]
                        for (p_, sl), kt in zip(slots + cslots, klist + [32, 33]):
                            pe.op(lambda e: e.matmul(p_[:, sl * 128:(sl + 1) * 128], ktb[r0:r0 + nr, kb_i, kt * 128:(kt + 1) * 128],
                                                     qt[r0:r0 + nr, qb_i, 0:128], start=True, stop=True),
                                  reads=[ktb, qt], writes=[p_], acc=True)
                        sb_ = ssb.next()
                        n1 = min(nl, 4)
                        assert bsl[:n1] == list(range(bsl[0], bsl[0] + n1))
                        dve.op(lambda e: e.tensor_tensor(out=sb_[:, 0:n1, :], in0=psx[:, 0:n1 * 128].rearrange("p (k q) -> p k q", k=n1),
                                                         in1=bt[:, bsl[0]:bsl[0] + n1, oh, :], op=ALU.add),
                               reads=[psx, bt], writes=[sb_])
                        if nl == 5:
                            dve.op(lambda e: e.tensor_tensor(out=sb_[:, 4, :], in0=psy[:, 0:128], in1=bt[:, bsl[4], oh, :], op=ALU.add),
                                   reads=[psy, bt], writes=[sb_])
                        pw = ptw.next()
                        act.op(lambda e: e.activation(out=pw[:, 0:nl, :], in_=sb_[:, 0:nl, :], func=AF.Exp), reads=[sb_], writes=[pw])
                        act.op(lambda e: e.activation(out=pw[:, 5:7, :], in_=psy[:, 128:384].rearrange("p (k q) -> p k q", k=2), func=AF.Exp),
                               reads=[psy], writes=[pw])
                        plist = [(k_, kt) for k_, kt in enumerate(klist)] + [(5, 32), (6, 33)]

                        def stage2(pw=pw, vh=vh, oh=oh, plist=plist, ao=ao, lastp=(oh == 3)):
                            po = pO.next()
                            for n_, (k_, kt) in enumerate(plist):
                                pe.op(lambda e: e.matmul(po[:, 0:65], pw[:, k_, :], vtb[:, kt, vh * 65:(vh + 1) * 65],
                                                         start=(n_ == 0), stop=(n_ == len(plist) - 1)),
                                      reads=[pw, vtb], writes=[po], acc=True)
                            finish_head([po], ao, oh)
                            if lastp:
                                g0 = s * TS + i * 128
                                sp.dma(AO.ap()[g0:g0 + 128, 0:256], ao[:, 0, :], reads=[ao], writes=[tAO[g0 // 128]])

                        push_l(stage2)

                def local_tile_C(s, i):
                    qt = qtb.next()
                    sp.dma(qt[:, 0:2, 0:128], QT["C"].ap()[s, :, :, i * 128:(i + 1) * 128].rearrange("b p t -> p b t"),
                           reads=[tQKV["C"][s]], writes=[qt])
                    ao = aos.next()
                    loc = [(0, i - 1)] * (i > 0) + [(1, i)] + [(2, i + 1)] * (i < 31)
                    for (qb_i, kb_i, r0, nr, vh, oh) in heads_of("C"):
                        psx, psy = pS.next(), pS.next()
                        for (p_, sl, kt) in [(psx, sl, kt) for sl, kt in loc] + [(psy, 0, 32), (psy, 1, 33)]:
                            pe.op(lambda e: e.matmul(p_[:, sl * 128:(sl + 1) * 128], ktb[r0:r0 + nr, kb_i, kt * 128:(kt + 1) * 128],
                                                     qt[r0:r0 + nr, qb_i, 0:128], start=True, stop=True),
                                  reads=[ktb, qt], writes=[p_], acc=True)
                        sb_ = ssb.next()
                        for sl, kt in loc:
                            if sl == 1:
                                continue
                            mi = 0 if sl == 0 else 1
                            dve.op(lambda e: e.tensor_tensor(out=sb_[:, sl, :], in0=psx[:, sl * 128:(sl + 1) * 128], in1=wm[:, mi, :], op=ALU.add),
                                   reads=[psx, wm], writes=[sb_])
                        pw = ptw.next()
                        for sl, kt in loc:
                            if sl == 1:
                                act.op(lambda e: e.activation(out=pw[:, 1, :], in_=psx[:, 128:256], func=AF.Exp, scale=0.125),
                                       reads=[psx], writes=[pw])
                            else:
                                act.op(lambda e: e.activation(out=pw[:, sl, :], in_=sb_[:, sl, :], func=AF.Exp, scale=0.125),
                                       reads=[sb_], writes=[pw])
                        act.op(lambda e: e.activation(out=pw[:, 3:5, :], in_=psy[:, 0:256].rearrange("p (k q) -> p k q", k=2), func=AF.Exp, scale=0.125),
                               reads=[psy], writes=[pw])
                        plist = [(sl, kt) for sl, kt in loc] + [(3, 32), (4, 33)]

                        def stage2(pw=pw, vh=vh, oh=oh, plist=plist, ao=ao, lastp=(oh == 3)):
                            po = pO.next()
                            for n_, (k_, kt) in enumerate(plist):
                                pe.op(lambda e: e.matmul(po[:, 0:65], pw[:, k_, :], vtb[:, kt, vh * 65:(vh + 1) * 65],
                                                         start=(n_ == 0), stop=(n_ == len(plist) - 1)),
                                      reads=[pw, vtb], writes=[po], acc=True)
                            finish_head([po], ao, oh, sink_h=oh)
                            if lastp:
                                g0 = s * TS + i * 128
                                sp.dma(AO.ap()[g0:g0 + 128, 512:768], ao[:, 0, :], reads=[ao], writes=[tAO[g0 // 128]])

                        push_l(stage2)

                for s in range(NS):
                    for m in ("A", "C", "B", "D"):
                        if m in os.environ.get("SKIP_MIX", ""):
                            continue
                        load_kv(m, s)
                        if m == "A":
                            for i in range(32):
                                local_tile_A(s, i)
                        elif m == "C":
                            for i in range(32):
                                local_tile_C(s, i)
                        else:
                            for qb in range(8):
                                dense_block(m, s, qb * 512, 512, list(range(NT)))
                        flush_l()
                        if with_ctx:
                            dense_block(m, s, TL, CT, [32, 33])

            if stop_after == "p2" and l == stop_layer:
                break
            if "AO" in dbg_out and l == 0:
                for g8 in range(0, NTOK // 128, 4):
                    sp.dma(dbg_out["AO"].ap()[g8 * 128:(g8 + 4) * 128, :], AO.ap()[g8 * 128:(g8 + 4) * 128, :],
                           reads=[tAO[g8 + t] for t in range(4)], writes=[tY])
                fw.barrier()
            tiles3 = [(s, j) for s in range(NS) for j in range(NT if with_ctx else 32)]
            with (Scope(fw) if "3" not in SKP else _Skip()) as sc:
                woutb = sc.sb("woutb", [128, 8, D], BF16)
                pool.dma(woutb[:], w_out.ap()[l].rearrange("(c p) n -> p c n", p=128), writes=[woutb])
                wr = sc.sb("wr", [128, 8, 20], F32)
                sp.dma(wr[:, :, 0:4], rg_w.ap()[l].rearrange("(c p) n -> p c n", p=128), writes=[wr])
                sp.dma(wr[:, :, 4:20], re_w.ap()[l].rearrange("(c p) n -> p c n", p=128), writes=[wr])
                rb = sc.sb("rb", [128, 20], F32)
                sp.dma(rb[:, 0:4], bcast_ap(rg_b, l * 4, 4), writes=[rb])
                sp.dma(rb[:, 4:20], bcast_ap(re_b, l * 16, 16), writes=[rb])
                bc_g1 = [sc.sb(f"bc_g1{i}", [128, D], F32) for i in range(3)]
                bc_sc2 = [sc.sb(f"bc_sc2{i}", [128, D], F32) for i in range(3)]
                bc_sh2 = [sc.sb(f"bc_sh2{i}", [128, D], F32) for i in range(3)]
                for src in range(3):
                    sp.dma(bc_g1[src][:], bcast_ap(MOD, mod_off(l, src, 2), D), reads=[tMOD[l]], writes=[bc_g1[src]])
                    sp.dma(bc_sh2[src][:], bcast_ap(MOD, mod_off(l, src, 3), D), reads=[tMOD[l]], writes=[bc_sh2[src]])
                    sp.dma(bc_sc2[src][:], bcast_ap(MOD, mod_off(l, src, 4), D), reads=[tMOD[l]], writes=[bc_sc2[src]])
                bc_lg = sc.sb("bc_lg", [128, D], F32)
                bc_lb = sc.sb("bc_lb", [128, D], F32)
                sp.dma(bc_lg[:], bcast_ap(ln1_g, l * D, D), writes=[bc_lg])
                sp.dma(bc_lb[:], bcast_ap(ln1_b, l * D, D), writes=[bc_lb])
                aoin = Rot([sc.sb(f"aoin{i}", [128, D], BF16) for i in range(2)])
                xin = Rot([sc.sb(f"xin3_{i}", [128, D], F32) for i in range(2)])
                aoT = sc.sb("aoT", [128, 8, 128], BF16)
                tA = sc.sb("tA3", [128, D], F32)
                tB = sc.sb("tB3", [128, D], F32)
                x1 = Rot([sc.sb(f"x1_{i}", [128, D], F32) for i in range(2)])
                h2 = sc.sb("h2", [128, D], F32)
                h2b = sc.sb("h2b", [128, D], BF16)
                h2Ts = Rot([sc.sb(f"h2Ts{i}", [128, 8, 128], BF16) for i in range(2)])
                h2Tf = sc.sb("h2Tf", [128, 8, 128], F32)
                stats = sc.sb("stats3", [128, 2, 6], F32)
                mv = sc.sb("mv3", [128, 2], F32)
                rstd = sc.sb("rstd3", [128, 1], F32)
                lg = sc.sb("lg", [128, 20], F32)
                r1 = sc.sb("r1", [128, 16], F32)
                r2 = sc.sb("r2", [128, 16], F32)
                oh1 = sc.sb("oh1", [128, 16], F32)
                oh2 = sc.sb("oh2", [128, 16], F32)
                sm = sc.sb("sm", [128, 8], F32)
                pT = Rot([sc.ps(f"pT3_{i}", [128, 8, 128], BF16) for i in range(2)])
                pM = sc.ps("pM", [128, D], F32)
                pF = [sc.ps(f"pF{i}", [128, 4, 128], F32) for i in range(2)]
                pL = sc.ps("pL", [128, 32], F32)

                def p3_load(s, j):
                    g = s * NT + j
                    a = aoin.next()
                    sp.dma(a[:], AO.ap()[g * 128:(g + 1) * 128, :], reads=[tAO[g]], writes=[a])
                    xt = xin.next()
                    sp.dma(xt[:], X.ap()[g * 128:(g + 1) * 128, :], reads=[tX[g]], writes=[xt])
                    return a, xt

                nxt = p3_load(*tiles3[0])
                for idx, (s, j) in enumerate(tiles3):
                    g = s * NT + j
                    src = s if j < 32 else 2
                    a, xt = nxt
                    if idx + 1 < len(tiles3):
                        nxt = p3_load(*tiles3[idx + 1])
                    pt = pT.next()
                    transposes(a, D, 128, pt, identb)
                    act.op(lambda e: e.copy(out=aoT[:], in_=pt[:]), reads=[pt], writes=[aoT])
                    for half in range(2):
                        for k in range(8):
                            pe.op(lambda e: e.matmul(pM[:, half * 512:(half + 1) * 512], aoT[:, k, :], woutb[:, k, half * 512:(half + 1) * 512],
                                                     start=(k == 0), stop=(k == 7)), reads=[aoT, woutb], writes=[pM], acc=True)
                    dve.op(lambda e: e.tensor_tensor(out=tA[:], in0=pM[:], in1=bc_g1[src][:], op=ALU.mult),
                           reads=[pM, bc_g1[src]], writes=[tA])
                    dve.op(lambda e: e.scalar_tensor_tensor(out=tA[:], in0=xt[:], scalar=ALPHA, in1=tA[:], op0=ALU.mult, op1=ALU.add),
                            reads=[xt, tA], writes=[tA])
                    layer_norm(tA, tB, stats, mv, rstd)
                    pool.op(lambda e: e.tensor_tensor(out=tB[:], in0=tB[:], in1=bc_lg[:], op=ALU.mult), reads=[tB, bc_lg], writes=[tB])
                    xo = x1.next()
                    dve.op(lambda e: e.tensor_tensor(out=xo[:], in0=tB[:], in1=bc_lb[:], op=ALU.add), reads=[tB, bc_lb], writes=[xo])
                    sp.dma(X.ap()[g * 128:(g + 1) * 128, :], xo[:], reads=[xo], writes=[tX[g]])
                    if ("x1", ) and "x1" in dbg_out and j < 32:
                        sp.dma(dbg_out["x1"].ap()[(s * 32 + j) * 128:(s * 32 + j + 1) * 128, :], xo[:], reads=[xo], writes=[tY])
                    layer_norm(xo, tA, stats, mv, rstd)
                    pool.op(lambda e: e.tensor_tensor(out=tA[:], in0=tA[:], in1=bc_sc2[src][:], op=ALU.mult),
                            reads=[tA, bc_sc2[src]], writes=[tA])
                    dve.op(lambda e: e.tensor_tensor(out=h2[:], in0=tA[:], in1=bc_sh2[src][:], op=ALU.add),
                           reads=[tA, bc_sh2[src]], writes=[h2])
                    act.op(lambda e: e.copy(out=h2b[:], in_=h2[:]), reads=[h2], writes=[h2b])
                    pt = pT.next()
                    transposes(h2b, D, 128, pt, identb)
                    hs = h2Ts.next()
                    act.op(lambda e: e.copy(out=hs[:], in_=pt[:]), reads=[pt], writes=[hs])
                    sp.dma(H2T.ap()[:, :, g * 128:(g + 1) * 128], hs[:], reads=[hs], writes=[tH2T[g]])
                    for c in range(8):
                        pe.op(lambda e: e.transpose(pF[c // 4][:, c % 4, :], h2[:, c * 128:(c + 1) * 128], identf[:]),
                              reads=[h2, identf], writes=[pF[c // 4]], acc=True)
                    for hf in range(2):
                        dve.op(lambda e: e.tensor_copy(out=h2Tf[:, hf * 4:(hf + 1) * 4, :], in_=pF[hf][:]), reads=[pF[hf]], writes=[h2Tf])
                    for k in range(8):
                        pe.op(lambda e: e.matmul(pL[:, 0:20], h2Tf[:, k, :], wr[:, k, :], start=(k == 0), stop=(k == 7)),
                              reads=[h2Tf, wr], writes=[pL], acc=True)
                    dve.op(lambda e: e.tensor_tensor(out=lg[:], in0=pL[:, 0:20], in1=rb[:], op=ALU.add), reads=[pL, rb], writes=[lg])
                    dve.op(lambda e: e.tensor_reduce(out=sm[:, 0:1], in_=lg[:, 0:4], axis=AX.X, op=ALU.max), reads=[lg], writes=[sm])
                    dve.op(lambda e: e.tensor_scalar(out=r1[:, 0:4], in0=lg[:, 0:4], scalar1=sm[:, 0:1], scalar2=None, op0=ALU.subtract),
                           reads=[lg, sm], writes=[r1])
                    act.op(lambda e: e.activation(out=r2[:, 0:4], in_=r1[:, 0:4], func=AF.Exp, accum_out=sm[:, 1:2]),
                           reads=[r1], writes=[r2, sm])
                    dve.op(lambda e: e.reciprocal(out=sm[:, 2:3], in_=sm[:, 1:2]), reads=[sm], writes=[sm])
                    dve.op(lambda e: e.tensor_scalar(out=r1[:, 4:8], in0=r1[:, 0:4], scalar1=0.0, scalar2=-1.0e9, op0=ALU.is_lt, op1=ALU.mult),
                           reads=[r1], writes=[r1])
                    dve.op(lambda e: e.tensor_tensor(out=r2[:].rearrange("p (g e) -> p g e", g=4), in0=lg[:, 4:20].rearrange("p (g e) -> p g e", g=4),
                                                     in1=r1[:, 4:8].unsqueeze(2).broadcast_to([128, 4, 4]), op=ALU.add),
                           reads=[lg, r1], writes=[r2])
                    dve.op(lambda e: e.tensor_reduce(out=sm[:, 3:4], in_=r2[:], axis=AX.X, op=ALU.max), reads=[r2], writes=[sm])
                    dve.op(lambda e: e.tensor_scalar(out=oh1[:], in0=r2[:], scalar1=sm[:, 3:4], scalar2=None, op0=ALU.is_equal),
                           reads=[r2, sm], writes=[oh1])
                    dve.op(lambda e: e.scalar_tensor_tensor(out=r1[:], in0=oh1[:], scalar=-1.0e9, in1=r2[:], op0=ALU.mult, op1=ALU.add),
                           reads=[oh1, r2], writes=[r1])
                    dve.op(lambda e: e.tensor_reduce(out=sm[:, 4:5], in_=r1[:], axis=AX.X, op=ALU.max), reads=[r1], writes=[sm])
                    dve.op(lambda e: e.tensor_scalar(out=oh2[:], in0=r1[:], scalar1=sm[:, 4:5], scalar2=None, op0=ALU.is_equal),
                           reads=[r1, sm], writes=[oh2])
                    dve.op(lambda e: e.tensor_tensor(out=sm[:, 5:6], in0=sm[:, 4:5], in1=sm[:, 3:4], op=ALU.subtract), reads=[sm], writes=[sm])
                    act.op(lambda e: e.activation(out=sm[:, 5:6], in_=sm[:, 5:6], func=AF.Exp), reads=[sm], writes=[sm])
                    dve.op(lambda e: e.tensor_scalar_add(out=sm[:, 5:6], in0=sm[:, 5:6], scalar1=1.0), reads=[sm], writes=[sm])
                    dve.op(lambda e: e.reciprocal(out=sm[:, 5:6], in_=sm[:, 5:6]), reads=[sm], writes=[sm])
                    dve.op(lambda e: e.tensor_scalar(out=sm[:, 6:7], in0=sm[:, 5:6], scalar1=-1.0, scalar2=1.0, op0=ALU.mult, op1=ALU.add),
                           reads=[sm], writes=[sm])
                    dve.op(lambda e: e.tensor_scalar(out=sm[:, 5:7], in0=sm[:, 5:7], scalar1=sm[:, 2:3], scalar2=None, op0=ALU.mult),
                           reads=[sm], writes=[sm])
                    dve.op(lambda e: e.tensor_scalar(out=oh1[:], in0=oh1[:], scalar1=sm[:, 5:6], scalar2=None, op0=ALU.mult),
                           reads=[oh1, sm], writes=[oh1])
                    dve.op(lambda e: e.scalar_tensor_tensor(out=gate_all[:, g, :], in0=oh2[:], scalar=sm[:, 6:7], in1=oh1[:], op0=ALU.mult, op1=ALU.add),
                           reads=[oh2, sm, oh1], writes=[gate_all])

            if stop_after == "p3" and l == stop_layer:
                break
            blocks = []
            for b in range(4):
                s, j0 = b // 2, (b % 2) * 16
                blocks.append([(s, j0 + t) for t in range(16)])
            if with_ctx:
                blocks.append([(0, 32), (0, 33), (1, 32), (1, 33)])
            with (Scope(fw) if "4" not in SKP else _Skip()) as sc:
                h2t = sc.sb("h2t", [128, 8, 2048], BF16)
                yacc = sc.sb("yacc", [128, 16, D], F32)
                wg = Rot([sc.sb(f"wg{i}", [128, 8, 512], BF16) for i in range(2)])
                wu = Rot([sc.sb(f"wu{i}", [128, 8, 512], BF16) for i in range(2)])
                wd = Rot([sc.sb(f"wd{i}", [128, 4, D], BF16) for i in range(2)])
                sg = Rot([sc.sb(f"sg{i}", [128, 512], BF16) for i in range(2)])
                am = Rot([sc.sb(f"am{i}", [128, 4, 512], BF16) for i in range(2)])
                bc_g2 = [sc.sb(f"bc_g2{i}", [128, D], F32) for i in range(3)]
                for src in range(3):
                    sp.dma(bc_g2[src][:], bcast_ap(MOD, mod_off(l, src, 5), D), reads=[tMOD[l]], writes=[bc_g2[src]])
                bc_lg = sc.sb("bc_lg2", [128, D], F32)
                bc_lb = sc.sb("bc_lb2", [128, D], F32)
                sp.dma(bc_lg[:], bcast_ap(ln2_g, l * D, D), writes=[bc_lg])
                sp.dma(bc_lb[:], bcast_ap(ln2_b, l * D, D), writes=[bc_lb])
                xin = Rot([sc.sb(f"xin4_{i}", [128, D], F32) for i in range(1)])
                tA = sc.sb("tA4", [128, D], F32)
                xo4 = Rot([sc.sb(f"xo4_{i}", [128, D], F32) for i in range(1)])
                stats = sc.sb("stats4", [128, 2, 6], F32)
                mv = sc.sb("mv4", [128, 2], F32)
                rstd = sc.sb("rstd4", [128, 1], F32)
                pG = Rot([sc.ps(f"pG{i}", [128, 512], F32) for i in range(2)])
                pU = Rot([sc.ps(f"pU{i}", [128, 512], F32) for i in range(2)])
                pY = Rot([sc.ps(f"pY{i}", [128, 512], F32) for i in range(4)])

                def load_w(e):
                    a, b_, c_ = wg.next(), wu.next(), wd.next()
                    pool.dma(a[:], mw_gate.ap()[l, e].rearrange("(c p) n -> p c n", p=128), writes=[a])
                    pool.dma(b_[:], mw_up.ap()[l, e].rearrange("(c p) n -> p c n", p=128), writes=[b_])
                    pool.dma(c_[:], mw_down.ap()[l, e].rearrange("(c p) n -> p c n", p=128), writes=[c_])
                    return a, b_, c_

                for blk in blocks:
                    nt_b = len(blk)
                    runs = []
                    for (s, j) in blk:
                        g = s * NT + j
                        if runs and runs[-1][0] + runs[-1][1] == g:
                            runs[-1][1] += 1
                        else:
                            runs.append([g, 1])
                    off = 0
                    for g0, n in runs:
                        sp.dma(h2t[:, :, off * 128:(off + n) * 128], H2T.ap()[:, :, g0 * 128:(g0 + n) * 128],
                               reads=[tH2T[g] for g in range(g0, g0 + n)], writes=[h2t])
                        off += n
                    gl = [s * NT + j for (s, j) in blk]
                    wts = {0: load_w(0), 1: load_w(1)}
                    subs = list(range(0, nt_b, 4))

                    def stage_a(e_, sb0):
                        wgt, wut, _ = wts[e_]
                        nsub = min(4, nt_b - sb0)
                        ntk = nsub * 128
                        at = am.next()
                        for f in range(4):
                            pg, pu = pG.next(), pU.next()
                            for k in range(8):
                                pe.op(lambda e: e.matmul(pg[:, 0:ntk], wgt[:, k, f * 128:(f + 1) * 128], h2t[:, k, sb0 * 128:sb0 * 128 + ntk],
                                                         start=(k == 0), stop=(k == 7)), reads=[wgt, h2t], writes=[pg], acc=True)
                            for k in range(8):
                                pe.op(lambda e: e.matmul(pu[:, 0:ntk], wut[:, k, f * 128:(f + 1) * 128], h2t[:, k, sb0 * 128:sb0 * 128 + ntk],
                                                         start=(k == 0), stop=(k == 7)), reads=[wut, h2t], writes=[pu], acc=True)
                            sgt = sg.next()
                            act.op(lambda e: e.activation(out=sgt[:, 0:ntk], in_=pg[:, 0:ntk], func=AF.Silu), reads=[pg], writes=[sgt])
                            dve.op(lambda e: e.tensor_tensor(out=at[:, f, 0:ntk], in0=sgt[:, 0:ntk], in1=pu[:, 0:ntk], op=ALU.mult),
                                   reads=[sgt, pu], writes=[at])
                        return at

                    def stage_b(e_, sb0, at):
                        wdt = wts[e_][2]
                        nsub = min(4, nt_b - sb0)
                        for t in range(nsub):
                            ti = sb0 + t
                            for half in range(2):
                                py = pY.next()
                                for f in range(4):
                                    pe.op(lambda e: e.matmul(py[:], at[:, f, t * 128:(t + 1) * 128], wdt[:, f, half * 512:(half + 1) * 512],
                                                             start=(f == 0), stop=(f == 3)), reads=[at, wdt], writes=[py], acc=True)
                                ya = yacc[:, ti, half * 512:(half + 1) * 512]
                                gcol = gate_all[:, gl[ti], e_:e_ + 1]
                                if e_ == 0:
                                    dve.op(lambda e: e.tensor_scalar(out=ya, in0=py[:], scalar1=gcol, scalar2=None, op0=ALU.mult),
                                           reads=[py, gate_all], writes=[yacc])
                                else:
                                    dve.op(lambda e: e.scalar_tensor_tensor(out=ya, in0=py[:], scalar=gcol, in1=ya, op0=ALU.mult, op1=ALU.add),
                                           reads=[py, gate_all, yacc], writes=[yacc])
                        if sb0 == subs[-1] and e_ + 2 < 16:
                            wts[e_ + 2] = load_w(e_ + 2)

                    pend = None
                    for e_ in range(16):
                        for sb0 in subs:
                            at = stage_a(e_, sb0)
                            if pend is not None:
                                stage_b(*pend)
                            pend = (e_, sb0, at)
                    stage_b(*pend)
                    for ti, (s, j) in enumerate(blk):
                        g = s * NT + j
                        src = s if j < 32 else 2
                        xt = xin.next()
                        sp.dma(xt[:], X.ap()[g * 128:(g + 1) * 128, :], reads=[tX[g]], writes=[xt])
                        pool.op(lambda e: e.tensor_tensor(out=tA[:], in0=yacc[:, ti, :], in1=bc_g2[src][:], op=ALU.mult),
                                reads=[yacc, bc_g2[src]], writes=[tA])
                        dve.op(lambda e: e.scalar_tensor_tensor(out=tA[:], in0=xt[:], scalar=ALPHA, in1=tA[:], op0=ALU.mult, op1=ALU.add),
                               reads=[xt, tA], writes=[tA])
                        layer_norm(tA, tA, stats, mv, rstd)
                        pool.op(lambda e: e.tensor_tensor(out=tA[:], in0=tA[:], in1=bc_lg[:], op=ALU.mult), reads=[tA, bc_lg], writes=[tA])
                        xo = xo4.next()
                        dve.op(lambda e: e.tensor_tensor(out=xo[:], in0=tA[:], in1=bc_lb[:], op=ALU.add), reads=[tA, bc_lb], writes=[xo])
                        if single == "mid":
                            sp.dma(y_out.ap()[g * 128:(g + 1) * 128, :], xo[:], reads=[xo], writes=[tY])
                        elif l == n_layers - 1:
                            if j < 32:
                                r = (s * 32 + j) * 128
                                sp.dma(y_out.ap()[r:r + 128, :], xo[:], reads=[xo], writes=[tY])
                        else:
                            sp.dma(X.ap()[g * 128:(g + 1) * 128, :], xo[:], reads=[xo], writes=[tX[g]])

        fw.barrier()
    return nc


_CONSTS = None


def _consts():
    global _CONSTS
    if _CONSTS is None:
        C64, S64 = _rope_tables(64)
        C32, S32 = _rope_tables(32)
        kk = np.arange(128)
        wmask = np.zeros((128, 2, 128), np.float32)
        wmask[:, 0, :] = np.where(kk[:, None] >= kk[None, :], 0.0, NEG)
        wmask[:, 1, :] = np.where(kk[:, None] <= kk[None, :], 0.0, NEG)
        _CONSTS = dict(ident=np.eye(128, dtype=np.float32), ropeC64=C64, ropeS64=S64, ropeC32=C32, ropeS32=S32, wmask=wmask)
    return _CONSTS


_PER_LAYER = ("w_ada", "b_ada", "w_in", "gqa_q_gain", "gqa_k_gain", "win_sink", "mla_q_gain",
              "mla_w_qb", "mla_kv_gain", "mla_w_kvb", "w_out", "ln1_g", "ln1_b",
              "router_group_w", "router_group_b", "router_expert_w", "router_expert_b",
              "moe_w_gate", "moe_w_up", "moe_w_down", "ln2_g", "ln2_b")


def _f32(a):
    return np.ascontiguousarray(np.asarray(a, dtype=np.float32))


def make_in_maps(inputs, n_cores=8, layer=None, xcur=None):
    sl = slice(None) if layer is None else slice(layer, layer + 1)
    shared = {k: _f32(inputs[k])[sl] for k in _PER_LAYER}
    shared["nab"] = _na_bias_tables(_f32(inputs["na_bias"]))[sl]
    shared.update(_consts())
    c, c_ctx = _f32(inputs["c"]), _f32(inputs["c_ctx"])
    if xcur is None:
        x, ctx = _f32(inputs["x"]), _f32(inputs["ctx"])
    maps = []
    for i in range(n_cores):
        b0 = NS * i
        if xcur is None:
            xc = np.concatenate([np.concatenate([x[b0 + s], ctx[b0 + s]], 0) for s in range(NS)], 0)
        else:
            xc = xcur[i]
        c3 = np.stack([c[b0], c[b0 + 1], c_ctx], 0)
        c3T = np.ascontiguousarray(c3.reshape(3, 8, 128).transpose(2, 1, 0))
        m = dict(shared)
        m["x_in"] = np.ascontiguousarray(xc)
        m["c3T"] = c3T
        maps.append(m)
    return maps


_PROGS = {}


def _prog(kind):
    if kind not in _PROGS:
        _PROGS[kind] = build(single=kind)
    return _PROGS[kind]


def kernel_unfused(**inputs):
    xcur = None
    for l in range(DEPTH):
        kind = "last" if l == DEPTH - 1 else "mid"
        maps = make_in_maps(inputs, 8, layer=l, xcur=xcur)
        res = run_bass_kernel_spmd(_prog(kind), maps, core_ids=list(range(8)))
        xcur = [np.asarray(r["y"]) for r in res.results]
    out = np.concatenate([r.reshape(NS, TL, D) for r in xcur], 0)
    return out.astype(np.float32)


def kernel(**inputs):
    maps = make_in_maps(inputs, 8)
    res = run_bass_kernel_spmd(_prog(None), maps, core_ids=list(range(8)))
    out = np.concatenate([np.asarray(r["y"]).reshape(NS, TL, D) for r in res.results], 0)
    return out.astype(np.float32)
```

```python
import os
import numpy as np
from contextlib import ExitStack
import concourse.bass as bass
import concourse.mybir as mybir
from concourse.bass_utils import run_bass_kernel_spmd

F32 = mybir.dt.float32
BF16 = mybir.dt.bfloat16
AF = mybir.ActivationFunctionType
ALU = mybir.AluOpType
AX = mybir.AxisListType

D = 1024
TL = 4096
CT = 256
TS = TL + CT
NT = TS // 128
NS = 2
NTOK = NS * TS
DEPTH = 4
DPROJ = 2208
NEG = -30000.0
ALPHA = 8.0 ** 0.25
EPS = 1e-6
NVAR = 21


class T:
    __slots__ = ("ap", "name", "w", "r", "psum")

    def __init__(self, ap, name="", psum=False):
        self.ap = ap
        self.name = name
        self.w = {}
        self.r = {}
        self.psum = psum

    def __getitem__(self, idx):
        return self.ap[idx]


def _merge(deps, d, skip=None):
    for s, v in d.items():
        if s is skip:
            continue
        if deps.get(s, 0) < v:
            deps[s] = v


class Eng:
    def __init__(self, fw, eng, name):
        self.fw = fw
        self.eng = eng
        self.name = name
        self.sem = fw.new_sem("s_" + name)
        self.n = 0
        self.waited = {}
        self.ring = None
        self.dma_count = 0

    def need(self, deps):
        for sem, val in deps.items():
            if self.waited.get(sem, 0) < val:
                self.eng.wait_ge(sem, val)
                self.waited[sem] = val

    def op(self, build, reads=(), writes=(), acc=False):
        deps = {}
        skip = self.sem if acc else None
        for t in reads:
            _merge(deps, t.w)
            if t.psum:
                _merge(deps, t.r, self.sem)
        for t in writes:
            _merge(deps, t.w, skip)
            _merge(deps, t.r, skip)
        self.need(deps)
        ins = build(self.eng)
        self.n += 1
        ins.then_inc(self.sem, 1)
        for t in reads:
            t.r[self.sem] = self.n
        for t in writes:
            if acc:
                t.w[self.sem] = self.n
            else:
                t.w = {self.sem: self.n}
                t.r = {}
        return ins

    def dma(self, out, in_, reads=(), writes=(), **kw):
        K = len(self.ring)
        m = self.dma_count
        k, j = m % K, m // K
        deps = {}
        if j > 0:
            deps[self.ring[k]] = 16 * j
        for t in reads:
            _merge(deps, t.w)
        for t in writes:
            _merge(deps, t.w)
            _merge(deps, t.r)
        self.need(deps)
        ins = self.eng.dma_start(out=out, in_=in_, **kw)
        ins.then_inc(self.ring[k], 16)
        self.dma_count += 1
        val = 16 * (j + 1)
        for t in reads:
            t.r[self.ring[k]] = val
        for t in writes:
            t.w = {self.ring[k]: val}
            t.r = {}
        return ins


class FW:
    def __init__(self, nc, stack, ring=16):
        self.nc = nc
        self.stack = stack
        self.gen = 0
        self.pe = Eng(self, nc.tensor, "pe")
        self.act = Eng(self, nc.scalar, "act")
        self.dve = Eng(self, nc.vector, "dve")
        self.pool = Eng(self, nc.gpsimd, "pool")
        self.sp = Eng(self, nc.sync, "sp")
        self.sp.ring = [self.new_sem(f"dq_sp{i}") for i in range(int(os.environ.get('RING_SP', 16)))]
        self.pool.ring = [self.new_sem(f"dq_pl{i}") for i in range(int(os.environ.get('RING_PL', 8)))]
        self.engs = [self.pe, self.act, self.dve, self.pool, self.sp]

    def new_sem(self, name):
        return self.stack.enter_context(self.nc.semaphore(name))

    def barrier(self):
        deps = {}
        for e in self.engs:
            if e.n:
                deps[e.sem] = e.n
            if e.ring is not None and e.dma_count:
                K = len(e.ring)
                for k in range(K):
                    cnt = (e.dma_count - k + K - 1) // K
                    if cnt:
                        deps[e.ring[k]] = 16 * cnt
        for e in self.engs:
            d = {s: v for s, v in deps.items() if s is not e.sem}
            e.need(d)
        self.gen += 1
        for e in self.engs:
            if e.n > int(os.environ.get('REFRESH_MIN', '28000')):
                e.sem = self.new_sem(f"s_{e.name}_g{self.gen}")
                e.n = 0


class Scope:
    _n = 0

    def __init__(self, fw):
        self.fw = fw
        self.st = ExitStack()
        Scope._n += 1
        self.tag = f"_s{Scope._n}"

    def __enter__(self):
        self.st.__enter__()
        return self

    def __exit__(self, *a):
        self.fw.barrier()
        return self.st.__exit__(*a)

    def sb(self, name, shape, dt):
        return T(self.st.enter_context(self.fw.nc.sbuf_tensor(name + self.tag, list(shape), dt)), name)

    def ps(self, name, shape, dt):
        return T(self.st.enter_context(self.fw.nc.psum_tensor(name + self.tag, list(shape), dt)), name, psum=True)


class _SkipBody(Exception):
    pass


class _Skip:
    def __enter__(self):
        return self

    def __exit__(self, et, ev, tb):
        return et is _SkipBody

    def sb(self, *a, **k):
        raise _SkipBody()

    ps = sb


class Rot:
    def __init__(self, tiles):
        self.tiles = tiles
        self.i = 0

    def next(self):
        t = self.tiles[self.i % len(self.tiles)]
        self.i += 1
        return t


def _rope_tables(d):
    h = d // 2
    q = h // 2
    pos = np.arange(TL)
    row = (pos // 64).astype(np.float32)
    col = (pos % 64).astype(np.float32)
    freq = (10000.0 ** (-np.arange(0, h, 2, dtype=np.float32) / h)).astype(np.float32)
    C = np.ones((TL + 128, d), np.float32)
    S = np.zeros((TL + 128, d), np.float32)
    for o, p in ((0, row), (h, col)):
        ang = p[:, None] * freq[None, :]
        c, s = np.cos(ang).astype(np.float32), np.sin(ang).astype(np.float32)
        C[:TL, o:o + q] = c
        C[:TL, o + q:o + h] = c
        S[:TL, o:o + q] = -s
        S[:TL, o + q:o + h] = s
    return C, S


def _na_variants():
    keys = {}
    for i in range(32):
        rows = [2 * i, 2 * i + 1]
        ks = set()
        for r in rows:
            r0 = min(max(r - 4, 0), 56)
            for kr in range(r0, r0 + 8):
                ks.add(kr // 2)
        keys[i] = sorted(ks)
    vid = {}
    nxt = 5
    for i in range(32):
        for j in keys[i]:
            if 2 <= i <= 29:
                vid[(i, j)] = j - i + 2
            else:
                vid[(i, j)] = nxt
                nxt += 1
    assert nxt == NVAR, nxt
    return keys, vid


def _na_bias_tables(na_bias):
    keys, vid = _na_variants()
    L = na_bias.shape[0]
    out = np.full((L, NVAR, 128, 4, 128), NEG, np.float32)
    done = set()
    kk = np.arange(128)
    for (i, j), v in vid.items():
        if v in done:
            continue
        done.add(v)
        q_row = 2 * i + kk // 64
        q_col = kk % 64
        k_row = 2 * j + kk // 64
        k_col = kk % 64
        r0 = np.clip(q_row - 4, 0, 56)
        c0 = np.clip(q_col - 8, 0, 48)
        ok = ((k_row[:, None] >= r0[None, :]) & (k_row[:, None] < r0[None, :] + 8)
              & (k_col[:, None] >= c0[None, :]) & (k_col[:, None] < c0[None, :] + 16))
        dr = np.clip(k_row[:, None] - q_row[None, :] + 7, 0, 14)
        dc = np.clip(k_col[:, None] - q_col[None, :] + 15, 0, 30)
        for l in range(L):
            for h in range(4):
                g = na_bias[l, h][dr, dc]
                out[l, v, :, h, :] = np.where(ok, g, np.float32(NEG))
    return out


def build(n_layers=DEPTH, dbg=(), stop_after=None, stop_layer=0, single=None):
    DD = DEPTH if single is None else 1
    if single is not None:
        n_layers = 1
    nc = bass.Bass("TRN2", target_bir_lowering=False)

    def din(name, shape, dt=F32):
        return nc.dram_tensor(name, list(shape), dt, kind="ExternalInput")

    def dscr(name, shape, dt):
        return nc.dram_tensor(name, list(shape), dt, kind="Internal")

    x_in = din("x_in", [NTOK, D])
    c3T = din("c3T", [128, 8, 3])
    w_ada = din("w_ada", [DD, D, 6 * D])
    b_ada = din("b_ada", [DD, 6 * D])
    w_in = din("w_in", [DD, D, DPROJ])
    nab = din("nab", [DD, NVAR, 128, 4, 128])
    gq_gain = din("gqa_q_gain", [DD, 64])
    gk_gain = din("gqa_k_gain", [DD, 64])
    win_sink = din("win_sink", [DD, 4])
    mq_gain = din("mla_q_gain", [DD, 256])
    w_qb = din("mla_w_qb", [DD, 256, 384])
    mkv_gain = din("mla_kv_gain", [DD, 128])
    w_kvb = din("mla_w_kvb", [DD, 128, 512])
    w_out = din("w_out", [DD, D, D])
    ln1_g = din("ln1_g", [DD, D])
    ln1_b = din("ln1_b", [DD, D])
    rg_w = din("router_group_w", [DD, D, 4])
    rg_b = din("router_group_b", [DD, 4])
    re_w = din("router_expert_w", [DD, D, 16])
    re_b = din("router_expert_b", [DD, 16])
    mw_gate = din("moe_w_gate", [DD, 16, D, 512])
    mw_up = din("moe_w_up", [DD, 16, D, 512])
    mw_down = din("moe_w_down", [DD, 16, 512, D])
    ln2_g = din("ln2_g", [DD, D])
    ln2_b = din("ln2_b", [DD, D])
    ident_in = din("ident", [128, 128])
    ropeC64 = din("ropeC64", [TL + 128, 64])
    ropeS64 = din("ropeS64", [TL + 128, 64])
    ropeC32 = din("ropeC32", [TL + 128, 32])
    ropeS32 = din("ropeS32", [TL + 128, 32])
    wmask = din("wmask", [128, 2, 128])
    y_out = nc.dram_tensor("y", [NTOK if single == "mid" else NS * TL, D], F32, kind="ExternalOutput")

    X = dscr("X", [NTOK, D], F32)
    MOD = dscr("MOD", [DD, 3, 6 * D], F32)
    QT = {"A": dscr("QT_A", [NS, 2, 128, TS], BF16), "B": dscr("QT_B", [NS, 2, 128, TS], BF16),
          "C": dscr("QT_C", [NS, 2, 128, TS], BF16), "D": dscr("QT_D", [NS, 4, 96, TS], BF16)}
    KT = {"A": dscr("KT_A", [NS, 2, 128, TS], BF16), "B": dscr("KT_B", [NS, 1, 128, TS], BF16),
          "C": dscr("KT_C", [NS, 1, 128, TS], BF16), "D": dscr("KT_D", [NS, 4, 96, TS], BF16)}
    VV = {"A": dscr("V_A", [NS, TS, 4 * 65], BF16), "B": dscr("V_B", [NS, TS, 2 * 65], BF16),
          "C": dscr("V_C", [NS, TS, 2 * 65], BF16), "D": dscr("V_D", [NS, TS, 4 * 65], BF16)}
    AO = dscr("AO", [NTOK, D], BF16)
    H2T = dscr("H2T", [128, 8, NTOK], BF16)
    dbg_out = {}
    for nm, shp, dt in dbg:
        dbg_out[nm] = nc.dram_tensor("dbg_" + nm, list(shp), dt, kind="ExternalOutput")

    tX = [T(None, f"X{g}") for g in range(NTOK // 128)]
    tMOD = [T(None, f"MOD{l}") for l in range(DEPTH)]
    tQKV = {m: [T(None, f"qkv{m}{s}") for s in range(NS)] for m in "ABCD"}
    tAO = [T(None, f"AO{g}") for g in range(NTOK // 128)]
    tH2T = [T(None, f"H2T{g}") for g in range(NTOK // 128)]
    tY = T(None, "Y")

    def bcast_ap(handle, offset, n, parts=128):
        return bass.AP(handle, offset, [[0, parts], [1, n]])

    with ExitStack() as gst:
        fw = FW(nc, gst)
        pe, act, dve, pool, sp = fw.pe, fw.act, fw.dve, fw.pool, fw.sp

        def gsb(name, shape, dt):
            return T(gst.enter_context(nc.sbuf_tensor(name, list(shape), dt)), name)

        identf = gsb("identf", [128, 128], F32)
        identb = gsb("identb", [128, 128], BF16)
        epsb = gsb("epsb", [128, 1], F32)
        gate_all = gsb("gate_all", [128, NS * NT, 16], F32)
        sp.dma(identf[:], ident_in.ap()[:, :], writes=[identf])
        dve.op(lambda e: e.tensor_copy(out=identb[:], in_=identf[:]), reads=[identf], writes=[identb])
        dve.op(lambda e: e.memset(epsb[:], EPS), writes=[epsb])

        def layer_norm(src, dst, stats, mv, rstd, reads_extra=()):
            for c in range(2):
                dve.op(lambda e: e.bn_stats(out=stats[:, c, :], in_=src[:, c * 512:(c + 1) * 512]),
                       reads=[src], writes=[stats])
            dve.op(lambda e: e.bn_aggr(out=mv[:], in_=stats[:].rearrange("p a b -> p (a b)")),
                   reads=[stats], writes=[mv])
            act.op(lambda e: e.activation(out=rstd[:], in_=mv[:, 1:2], func=AF.Ln, bias=epsb[:, 0:1]),
                   reads=[mv, epsb], writes=[rstd])
            act.op(lambda e: e.activation(out=rstd[:], in_=rstd[:], func=AF.Exp, scale=-0.5),
                   reads=[rstd], writes=[rstd])
            dve.op(lambda e: e.tensor_scalar(out=dst[:], in0=src[:], scalar1=mv[:, 0:1], scalar2=rstd[:, 0:1],
                                             op0=ALU.subtract, op1=ALU.mult),
                   reads=[src, mv, rstd], writes=[dst])

        def transposes(src, ncols, blk, pst, ident, nparts=128):
            for c in range(ncols // blk):
                pe.op(lambda e: e.transpose(pst[0:blk, c, :], src[:, c * blk:(c + 1) * blk], ident[:]),
                      reads=[src, ident], writes=[pst], acc=True)

        def load_bc(dst, handle, offset, n=D, eng=None):
            (eng or sp).dma(dst[:, 0:n], bcast_ap(handle, offset, n), writes=[dst])

        with Scope(fw) as sc:
            c3 = sc.sb("c3", [128, 8, 3], F32)
            sc3 = sc.sb("sc3", [128, 8, 3], F32)
            bada = sc.sb("bada", [3, 6 * D], F32)
            modsb = sc.sb("modsb", [3, 6 * D], F32)
            wch = Rot([sc.sb(f"wch{i}", [128, 8, 512], F32) for i in range(2)])
            pmod = Rot([sc.ps(f"pmod{i}", [3, 512], F32) for i in range(2)])
            sp.dma(c3[:], c3T.ap()[:, :, :], writes=[c3])
            act.op(lambda e: e.activation(out=sc3[:], in_=c3[:], func=AF.Silu), reads=[c3], writes=[sc3])
            for l in range(n_layers):
                sp.dma(bada[:], bcast_ap(b_ada, l * 6 * D, 6 * D, parts=3), writes=[bada])
                for cc in range(12):
                    wt = wch.next()
                    sp.dma(wt[:], w_ada.ap()[l, :, cc * 512:(cc + 1) * 512].rearrange("(c p) n -> p c n", p=128),
                           writes=[wt])
                    pm = pmod.next()
                    for k in range(8):
                        pe.op(lambda e: e.matmul(pm[:], sc3[:, k, :], wt[:, k, :], start=(k == 0), stop=(k == 7)),
                              reads=[sc3, wt], writes=[pm], acc=True)
                    dve.op(lambda e: e.tensor_tensor(out=modsb[:, cc * 512:(cc + 1) * 512], in0=pm[:],
                                                     in1=bada[:, cc * 512:(cc + 1) * 512], op=ALU.add),
                           reads=[pm, bada], writes=[modsb])
                for o in (1, 4):
                    dve.op(lambda e: e.tensor_scalar_add(out=modsb[:, o * D:(o + 1) * D], in0=modsb[:, o * D:(o + 1) * D],
                                                         scalar1=1.0), reads=[modsb], writes=[modsb])
                sp.dma(MOD.ap()[l, :, :], modsb[:], reads=[modsb], writes=[tMOD[l]])

        if "MOD" in dbg_out:
            sp.dma(dbg_out["MOD"].ap()[:, :, :], MOD.ap()[:, :, :], reads=tMOD[:n_layers], writes=[tY])
            fw.barrier()
        if stop_after == "p0":
            n_layers = 0

        def mod_off(l, src, which):
            return (l * 3 + src) * 6 * D + which * D

        keysA, vidA = _na_variants()

        for l in range(n_layers):
            last = (l == DEPTH - 1) if single is None else (single == "last")
            with_ctx = not last
            if stop_after == "lstart" and l == stop_layer:
                break

            for p1_round in range(2 if (l > 0 and os.environ.get("P1_WARM", "0") == "1") else 1):
                with Scope(fw) as sc:
                    winb = sc.sb("winb", [128, 8, DPROJ], BF16)
                    wqbb = sc.sb("wqbb", [128, 2, 384], BF16)
                    wkvbb = sc.sb("wkvbb", [128, 512], BF16)
                    pool.dma(winb[:], w_in.ap()[l].rearrange("(c p) n -> p c n", p=128), writes=[winb])
                    pool.dma(wqbb[:], w_qb.ap()[l].rearrange("(c p) n -> p c n", p=128), writes=[wqbb])
                    pool.dma(wkvbb[:], w_kvb.ap()[l], writes=[wkvbb])
                    bc_sc = [sc.sb(f"bc_sc{i}", [128, D], F32) for i in range(3)]
                    bc_sh = [sc.sb(f"bc_sh{i}", [128, D], F32) for i in range(3)]
                    for src in range(3):
                        sp.dma(bc_sh[src][:], bcast_ap(MOD, mod_off(l, src, 0), D), reads=[tMOD[l]], writes=[bc_sh[src]])
                        sp.dma(bc_sc[src][:], bcast_ap(MOD, mod_off(l, src, 1), D), reads=[tMOD[l]], writes=[bc_sc[src]])
                    gainB = sc.sb("gainB", [128, 6, 64], F32)
                    for hh in range(4):
                        sp.dma(gainB[:, hh, :], bcast_ap(gq_gain, l * 64, 64), writes=[gainB])
                    for hh in range(2):
                        sp.dma(gainB[:, 4 + hh, :], bcast_ap(gk_gain, l * 64, 64), writes=[gainB])
                    gainQ = sc.sb("gainQ", [128, 256], F32)
                    gainKV = sc.sb("gainKV", [128, 128], F32)
                    sp.dma(gainQ[:], bcast_ap(mq_gain, l * 256, 256), writes=[gainQ])
                    sp.dma(gainKV[:], bcast_ap(mkv_gain, l * 128, 128), writes=[gainKV])

                    xin = Rot([sc.sb(f"xin{i}", [128, D], F32) for i in range(2)])
                    rC64 = Rot([sc.sb(f"rC64_{i}", [128, 64], F32) for i in range(2)])
                    rS64 = Rot([sc.sb(f"rS64_{i}", [128, 64], F32) for i in range(2)])
                    rC32 = Rot([sc.sb(f"rC32_{i}", [128, 32], F32) for i in range(2)])
                    rS32 = Rot([sc.sb(f"rS32_{i}", [128, 32], F32) for i in range(2)])
                    stats = sc.sb("stats", [128, 2, 6], F32)
                    mv = sc.sb("mv", [128, 2], F32)
                    rstd = sc.sb("rstd", [128, 1], F32)
                    tmpA = sc.sb("tmpA", [128, D], F32)
                    hb = sc.sb("hb", [128, D], BF16)
                    hT = Rot([sc.sb(f"hT{i}", [128, 8, 128], BF16) for i in range(2)])
                    sq = sc.sb("sq", [128, 384], F32)
                    ss6 = sc.sb("ss6", [128, 6], F32)
                    t1 = sc.sb("t1", [128, 6, 64], F32)
                    t2 = sc.sb("t2", [128, 6, 64], F32)
                    t3 = sc.sb("t3", [128, 6, 64], F32)
                    ssD = sc.sb("ssD", [128, 2], F32)
                    junk = sc.sb("junk", [128, 256], F32)
                    cqkv = sc.sb("cqkv", [128, 384], BF16)
                    cT = sc.sb("cT", [128, 3, 128], BF16)
                    u1 = sc.sb("u1", [128, 4, 32], F32)
                    u2 = sc.sb("u2", [128, 4, 32], F32)
                    u3 = sc.sb("u3", [128, 4, 32], F32)
                    kr1 = sc.sb("kr1", [128, 32], F32)
                    kr2 = sc.sb("kr2", [128, 32], F32)
                    qkA = Rot([sc.sb(f"qkA{i}", [128, 512], BF16) for i in range(2)])
                    qkB = Rot([sc.sb(f"qkB{i}", [128, 384], BF16) for i in range(2)])
                    qkC = Rot([sc.sb(f"qkC{i}", [128, 384], BF16) for i in range(2)])
                    qdb = Rot([sc.sb(f"qdb{i}", [128, 4, 96], BF16) for i in range(2)])
                    kdb = Rot([sc.sb(f"kdb{i}", [128, 4, 96], BF16) for i in range(2)])
                    stgA = Rot([sc.sb(f"stgA{i}", [128, 4, 128], BF16) for i in range(2)])
                    stgB = Rot([sc.sb(f"stgB{i}", [128, 3, 128], BF16) for i in range(2)])
                    stgC = Rot([sc.sb(f"stgC{i}", [128, 3, 128], BF16) for i in range(2)])
                    stgD = Rot([sc.sb(f"stgD{i}", [128, 8, 128], BF16) for i in range(2)])
                    vA = [sc.sb(f"vA{i}", [128, 4, 65], BF16) for i in range(2)]
                    vB = [sc.sb(f"vB{i}", [128, 2, 65], BF16) for i in range(2)]
                    vC = [sc.sb(f"vC{i}", [128, 2, 65], BF16) for i in range(2)]
                    vD = [sc.sb(f"vD{i}", [128, 4, 65], BF16) for i in range(2)]
                    for vt in vA + vB + vC + vD:
                        pool.op(lambda e: e.memset(vt[:], 1.0), writes=[vt])
                    vA, vB, vC, vD = Rot(vA), Rot(vB), Rot(vC), Rot(vD)
                    pT = Rot([sc.ps(f"pT{i}", [128, 8, 128], BF16) for i in range(2)])
                    pj = Rot([sc.ps(f"pj{i}", [128, 512], F32) for i in range(4)])
                    pq = Rot([sc.ps(f"pq{i}", [128, 8, 128], BF16) for i in range(2)])

                    x_src = x_in if l == 0 else X
                    order = [(s, j) for s in range(NS) for j in range(NT)]
                    P1S = float(os.environ.get("P1_STEP", "99"))
                    if P1S < 99:
                        order = order[:2]
                    if l > 0 and os.environ.get("P1_TILES"):
                        order = order[:int(os.environ["P1_TILES"])]
                    if l > 0 and os.environ.get("P1_WARM", "0") == "1" and p1_round == 0:
                        order = order[:2]

                    def p1_load(s, j):
                        g = s * NT + j
                        xt = xin.next()
                        rd = [] if l == 0 else [tX[g]]
                        sp.dma(xt[:], x_src.ap()[g * 128:(g + 1) * 128, :], reads=rd, writes=[xt])
                        r0 = j * 128 if j < 32 else TL
                        tabs = []
                        for rot, src_t, n in ((rC64, ropeC64, 64), (rS64, ropeS64, 64), (rC32, ropeC32, 32), (rS32, ropeS32, 32)):
                            tt = rot.next()
                            sp.dma(tt[:], src_t.ap()[r0:r0 + 128, :], writes=[tt])
                            tabs.append(tt)
                        return xt, tabs

                    def rope(dst_views, src, nh, dd, Ct, St, ta, tb, src_t):
                        q = dd // 4
                        Cb = Ct[:].unsqueeze(1).broadcast_to([128, nh, dd])
                        pool_or_dve = dve
                        dve.op(lambda e: e.tensor_tensor(out=ta[:, 0:nh, :], in0=src, in1=Cb, op=ALU.mult),
                               reads=[src_t, Ct], writes=[ta])
                        sv = src.rearrange("p h (r f e) -> p h r f e", r=2, f=2)
                        tv = tb[:, 0:nh, :].rearrange("p h (r f e) -> p h r f e", r=2, f=2)
                        Sv = St[:].rearrange("p (r f e) -> p r f e", r=2, f=2)
                        for f in range(2):
                            Sb = Sv[:, :, f, :].unsqueeze(1).broadcast_to([128, nh, 2, q])
                            dve.op(lambda e: e.tensor_tensor(out=tv[:, :, :, f, :], in0=sv[:, :, :, 1 - f, :], in1=Sb, op=ALU.mult),
                                   reads=[src_t, St], writes=[tb])
                        for (o_ap, a_ap, b_ap) in dst_views(ta, tb):
                            dve.op(lambda e: e.tensor_tensor(out=o_ap[0], in0=a_ap, in1=b_ap, op=ALU.add),
                                   reads=[ta, tb], writes=[o_ap[1]])

                    nxt = p1_load(*order[0])
                    for idx, (s, j) in enumerate(order):
                        g = s * NT + j
                        tok0 = j * 128
                        xt, (C64, S64, C32, S32) = nxt
                        if idx + 1 < len(order):
                            nxt = p1_load(*order[idx + 1])
                        src = s if j < 32 else 2
                        if l == 0:
                            sp.dma(X.ap()[g * 128:(g + 1) * 128, :], xt[:], reads=[xt], writes=[tX[g]])
                        if P1S < 1:
                            continue
                        layer_norm(xt, tmpA, stats, mv, rstd)
                        pool.op(lambda e: e.tensor_tensor(out=tmpA[:], in0=tmpA[:], in1=bc_sc[src][:], op=ALU.mult),
                                reads=[tmpA, bc_sc[src]], writes=[tmpA])
                        dve.op(lambda e: e.tensor_tensor(out=hb[:], in0=tmpA[:], in1=bc_sh[src][:], op=ALU.add),
                               reads=[tmpA, bc_sh[src]], writes=[hb])
                        pt = pT.next()
                        transposes(hb, D, 128, pt, identb)
                        ht = hT.next()
                        act.op(lambda e: e.copy(out=ht[:], in_=pt[:]), reads=[pt], writes=[ht])

                        def proj(c0, n):
                            p = pj.next()
                            for k in range(8):
                                pe.op(lambda e: e.matmul(p[:, 0:n], ht[:, k, :], winb[:, k, c0:c0 + n], start=(k == 0), stop=(k == 7)),
                                      reads=[ht, winb], writes=[p], acc=True)
                            return p

                        if P1S < 2:
                            continue
                        pa0 = proj(0, 512)
                        pa1 = proj(512, 256)
                        qa = qkA.next()
                        act.op(lambda e: e.activation(out=qa[:, 0:256], in_=pa0[:, 0:256], func=AF.Copy, scale=0.125),
                               reads=[pa0], writes=[qa])
                        act.op(lambda e: e.copy(out=qa[:, 256:512], in_=pa0[:, 256:512]), reads=[pa0], writes=[qa])
                        va = vA.next()
                        dve.op(lambda e: e.tensor_copy(out=va[:, :, 0:64], in_=pa1[:, 0:256].rearrange("p (h d) -> p h d", h=4)),
                               reads=[pa1], writes=[va])
                        pqa = pq.next()
                        transposes(qa, 512, 128, pqa, identb)
                        sa = stgA.next()
                        act.op(lambda e: e.copy(out=sa[:], in_=pqa[:, 0:4, :]), reads=[pqa], writes=[sa])
                        sp.dma(QT["A"].ap()[s, :, :, tok0:tok0 + 128].rearrange("b p t -> p b t"), sa[:, 0:2, :],
                               reads=[sa], writes=[tQKV["A"][s]])
                        sp.dma(KT["A"].ap()[s, :, :, tok0:tok0 + 128].rearrange("b p t -> p b t"), sa[:, 2:4, :],
                               reads=[sa], writes=[tQKV["A"][s]])
                        sp.dma(VV["A"].ap()[s, tok0:tok0 + 128, :], va[:].rearrange("p h d -> p (h d)"),
                               reads=[va], writes=[tQKV["A"][s]])

                        if P1S < 2.05:
                            continue
                        for m in ("B", "C"):
                            pb = proj(768 if m == "B" else 1280, 512)
                            qk_view = pb[:, 0:384].rearrange("p (h d) -> p h d", h=6)
                            if m == "B":
                                act.op(lambda e: e.activation(out=sq[:], in_=pb[:, 0:384], func=AF.Square), reads=[pb], writes=[sq])
                                dve.op(lambda e: e.tensor_reduce(out=ss6[:], in_=sq[:].rearrange("p (h d) -> p h d", h=6),
                                                                 axis=AX.X, op=ALU.add), reads=[sq], writes=[ss6])
                                act.op(lambda e: e.activation(out=ss6[:], in_=ss6[:], func=AF.Ln, bias=epsb[:, 0:1], scale=1.0 / 64),
                                       reads=[ss6, epsb], writes=[ss6])
                                act.op(lambda e: e.activation(out=ss6[:], in_=ss6[:], func=AF.Exp, scale=-0.5), reads=[ss6], writes=[ss6])
                                if P1S < 2.2:
                                    break
                                dve.op(lambda e: e.tensor_tensor(out=t3[:], in0=qk_view, in1=ss6[:].unsqueeze(2).broadcast_to([128, 6, 64]),
                                                                 op=ALU.mult), reads=[pb, ss6], writes=[t3])
                                dve.op(lambda e: e.tensor_tensor(out=t3[:], in0=t3[:], in1=gainB[:], op=ALU.mult),
                                       reads=[t3, gainB], writes=[t3])
                                r_src, r_t = t3[:], t3
                            else:
                                act.op(lambda e: e.copy(out=t3[:], in_=qk_view), reads=[pb], writes=[t3])
                                r_src, r_t = t3[:], t3
                            if P1S < 2.3:
                                break
                            qb_ = (qkB if m == "B" else qkC).next()

                            def views(ta, tb, qb_=qb_):
                                oq = qb_[:, 0:256].rearrange("p (gi kv d) -> p kv gi d", gi=2, kv=2)
                                aq = ta[:, 0:4, :].rearrange("p (kv gi) d -> p kv gi d", kv=2)
                                bq = tb[:, 0:4, :].rearrange("p (kv gi) d -> p kv gi d", kv=2)
                                ok = qb_[:, 256:384].rearrange("p (h d) -> p h d", h=2)
                                return [((oq, qb_), aq, bq), ((ok, qb_), ta[:, 4:6, :], tb[:, 4:6, :])]

                            rope(views, r_src, 6, 64, C64, S64, t1, t2, r_t)
                            if P1S < 2.4:
                                break
                            vb = (vB if m == "B" else vC).next()
                            act.op(lambda e: e.copy(out=vb[:, :, 0:64], in_=pb[:, 384:512].rearrange("p (h d) -> p h d", h=2)),
                                   reads=[pb], writes=[vb])
                            pqb = pq.next()
                            transposes(qb_, 384, 128, pqb, identb)
                            if P1S < 2.5:
                                break
                            sb_ = (stgB if m == "B" else stgC).next()
                            act.op(lambda e: e.copy(out=sb_[:], in_=pqb[:, 0:3, :]), reads=[pqb], writes=[sb_])
                            if P1S >= 2.6:
                                sp.dma(QT[m].ap()[s, :, :, tok0:tok0 + 128].rearrange("b p t -> p b t"), sb_[:, 0:2, :],
                                       reads=[sb_], writes=[tQKV[m][s]])
                            if P1S >= 2.7:
                                sp.dma(KT[m].ap()[s, 0, :, tok0:tok0 + 128], sb_[:, 2, :], reads=[sb_], writes=[tQKV[m][s]])
                            if P1S >= 2.8:
                                sp.dma(VV[m].ap()[s, tok0:tok0 + 128, :], vb[:].rearrange("p h d -> p (h d)"),
                                       reads=[vb], writes=[tQKV[m][s]])
                            if P1S < 2.9:
                                break

                        if P1S < 4:
                            continue
                        pd = proj(1792, 416)
                        act.op(lambda e: e.activation(out=junk[:, 0:256], in_=pd[:, 0:256], func=AF.Square, accum_out=ssD[:, 0:1]),
                               reads=[pd], writes=[junk, ssD])
                        act.op(lambda e: e.activation(out=junk[:, 0:128], in_=pd[:, 256:384], func=AF.Square, accum_out=ssD[:, 1:2]),
                               reads=[pd], writes=[junk, ssD])
                        act.op(lambda e: e.activation(out=ssD[:, 0:1], in_=ssD[:, 0:1], func=AF.Ln, bias=epsb[:, 0:1], scale=1.0 / 256),
                               reads=[ssD, epsb], writes=[ssD])
                        act.op(lambda e: e.activation(out=ssD[:, 1:2], in_=ssD[:, 1:2], func=AF.Ln, bias=epsb[:, 0:1], scale=1.0 / 128),
                               reads=[ssD, epsb], writes=[ssD])
                        act.op(lambda e: e.activation(out=ssD[:], in_=ssD[:], func=AF.Exp, scale=-0.5), reads=[ssD], writes=[ssD])
                        dve.op(lambda e: e.scalar_tensor_tensor(out=cqkv[:, 0:256], in0=pd[:, 0:256], scalar=ssD[:, 0:1], in1=gainQ[:],
                                                                op0=ALU.mult, op1=ALU.mult), reads=[pd, ssD, gainQ], writes=[cqkv])
                        dve.op(lambda e: e.scalar_tensor_tensor(out=cqkv[:, 256:384], in0=pd[:, 256:384], scalar=ssD[:, 1:2], in1=gainKV[:],
                                                                op0=ALU.mult, op1=ALU.mult), reads=[pd, ssD, gainKV], writes=[cqkv])
                        pqc = pq.next()
                        transposes(cqkv, 384, 128, pqc, identb)
                        act.op(lambda e: e.copy(out=cT[:], in_=pqc[:, 0:3, :]), reads=[pqc], writes=[cT])
                        pqd = pj.next()
                        for k in range(2):
                            pe.op(lambda e: e.matmul(pqd[:, 0:384], cT[:, k, :], wqbb[:, k, :], start=(k == 0), stop=(k == 1)),
                                  reads=[cT, wqbb], writes=[pqd], acc=True)
                        pkv = pj.next()
                        pe.op(lambda e: e.matmul(pkv[:, 0:512], cT[:, 2, :], wkvbb[:], start=True, stop=True),
                              reads=[cT, wkvbb], writes=[pkv], acc=True)
                        qd = qdb.next()
                        kd = kdb.next()
                        qv = pqd[:, 0:384].rearrange("p (h d) -> p h d", h=4)
                        kvv = pkv[:, 0:512].rearrange("p (h d) -> p h d", h=4)
                        act.op(lambda e: e.copy(out=qd[:, :, 0:64], in_=qv[:, :, 0:64]), reads=[pqd], writes=[qd])

                        def views_q(ta, tb, qd=qd):
                            return [((qd[:, :, 64:96], qd), ta[:, 0:4, :], tb[:, 0:4, :])]

                        act.op(lambda e: e.copy(out=u3[:], in_=qv[:, :, 64:96]), reads=[pqd], writes=[u3])
                        rope(views_q, u3[:], 4, 32, C32, S32, u1, u2, u3)
                        act.op(lambda e: e.copy(out=kd[:, :, 0:64], in_=kvv[:, :, 0:64]), reads=[pkv], writes=[kd])
                        act.op(lambda e: e.copy(out=kr1[:], in_=pd[:, 384:416]), reads=[pd], writes=[kr1])
                        kr_src = kr1[:].unsqueeze(1)

                        def views_k(ta, tb, kd=kd):
                            return [((kd[:, :, 64:96], kd), ta[:, 0:1, :].broadcast_to([128, 4, 32]), tb[:, 0:1, :].broadcast_to([128, 4, 32]))]

                        rope(views_k, kr_src, 1, 32, C32, S32, u1, u2, kr1)
                        vd = vD.next()
                        dve.op(lambda e: e.tensor_copy(out=vd[:, :, 0:64], in_=kvv[:, :, 64:128]), reads=[pkv], writes=[vd])
                        pqe = pq.next()
                        for h in range(4):
                            pe.op(lambda e: e.transpose(pqe[0:96, h, :], qd[:, h, :], identb[:]), reads=[qd, identb], writes=[pqe], acc=True)
                            pe.op(lambda e: e.transpose(pqe[0:96, 4 + h, :], kd[:, h, :], identb[:]), reads=[kd, identb], writes=[pqe], acc=True)
                        sd = stgD.next()
                        act.op(lambda e: e.copy(out=sd[0:96, :, :], in_=pqe[0:96, :, :]), reads=[pqe], writes=[sd])
                        sp.dma(QT["D"].ap()[s, :, :, tok0:tok0 + 128].rearrange("b p t -> p b t"), sd[0:96, 0:4, :],
                               reads=[sd], writes=[tQKV["D"][s]])
                        sp.dma(KT["D"].ap()[s, :, :, tok0:tok0 + 128].rearrange("b p t -> p b t"), sd[0:96, 4:8, :],
                               reads=[sd], writes=[tQKV["D"][s]])
                        sp.dma(VV["D"].ap()[s, tok0:tok0 + 128, :], vd[:].rearrange("p h d -> p (h d)"),
                               reads=[vd], writes=[tQKV["D"][s]])

            if stop_after == "p1" and l == stop_layer:
                break
            if os.environ.get("SKIP_REST") == "1":
                continue
            SKP = os.environ.get("SKIP_PH", "").split(",")
            with (Scope(fw) if "2" not in SKP else _Skip()) as sc:
                ktb = sc.sb("ktb", [128, 4, TS], BF16)
                vtb = sc.sb("vtb", [128, NT, 260], BF16)
                qtb = Rot([sc.sb(f"qtb{i}", [128, 4, 512], BF16) for i in range(2)])
                ptb = Rot([sc.sb(f"ptb{i}", [128, 1024], BF16) for i in range(3)])
                ptw = Rot([sc.sb(f"ptw{i}", [128, 7, 128], BF16) for i in range(3)])
                ssb = Rot([sc.sb(f"ssb{i}", [128, 5, 128], F32) for i in range(2)])
                aos = Rot([sc.sb(f"aos{i}", [128, 4, 256], BF16) for i in range(2)])
                rec = Rot([sc.sb(f"rec{i}", [128, 4, 1], F32) for i in range(4)])
                biasI = sc.sb("biasI", [128, 5, 4, 128], F32)
                biasE = Rot([sc.sb(f"biasE{i}", [128, 4, 4, 128], F32) for i in range(2)])
                wm = sc.sb("wm", [128, 2, 128], F32)
                esink = sc.sb("esink", [128, 4], F32)
                pSD_l = [sc.ps(f"pS{i}", [128, 1024], F32) for i in range(2)]
                pSL_l = [T(d.ap[:, h * 512:(h + 1) * 512], f"pSL{k}{h}", psum=True) for k, d in enumerate(pSD_l) for h in range(2)]
                pSD = Rot(pSD_l)
                pS = Rot(pSL_l)

                def alias_sync(to_dense):
                    for k, dk in enumerate(pSD_l):
                        for lt in pSL_l[2 * k:2 * k + 2]:
                            if to_dense:
                                _merge(dk.w, lt.w)
                                _merge(dk.r, lt.r)
                            else:
                                _merge(lt.w, dk.w)
                                _merge(lt.r, dk.r)
                pO = Rot([sc.ps(f"pO{i}", [128, 512], F32) for i in range(4)])
                sp.dma(wm[:], wmask.ap()[:, :, :], writes=[wm])
                sp.dma(esink[:], bcast_ap(win_sink, l * 4, 4), writes=[esink])
                act.op(lambda e: e.activation(out=esink[:], in_=esink[:], func=AF.Exp), reads=[esink], writes=[esink])
                sp.dma(biasI[:], nab.ap()[l, 0:5].rearrange("v k h q -> k v h q"), writes=[biasI])

                MIX = {"A": dict(nb=2, rows=128, nvh=4, col=0, scale=1.0),
                       "B": dict(nb=1, rows=128, nvh=2, col=256, scale=0.125),
                       "C": dict(nb=1, rows=128, nvh=2, col=512, scale=0.125),
                       "D": dict(nb=4, rows=96, nvh=4, col=768, scale=96.0 ** -0.5)}

                def heads_of(m):
                    if m == "A":
                        return [(c, c, r * 64, 64, 2 * c + r, 2 * c + r) for c in range(2) for r in range(2)]
                    if m in ("B", "C"):
                        return [(c, 0, r * 64, 64, r, c + 2 * r) for c in range(2) for r in range(2)]
                    return [(h, h, 0, 96, h, h) for h in range(4)]

                def finish_head(pos, ao, oh, sink_h=None):
                    for t, po in enumerate(pos):
                        rc = rec.next()
                        if sink_h is not None:
                            dve.op(lambda e: e.tensor_scalar(out=rc[:, 0, :], in0=po[:, 64:65], scalar1=esink[:, sink_h:sink_h + 1],
                                                             scalar2=None, op0=ALU.add), reads=[po, esink], writes=[rc])
                            dve.op(lambda e: e.reciprocal(out=rc[:, 0, :], in_=rc[:, 0, :]), reads=[rc], writes=[rc])
                        else:
                            dve.op(lambda e: e.reciprocal(out=rc[:, 0, :], in_=po[:, 64:65]), reads=[po], writes=[rc])
                        dve.op(lambda e: e.tensor_scalar(out=ao[:, t, oh * 64:(oh + 1) * 64], in0=po[:, 0:64], scalar1=rc[:, 0, :],
                                                         scalar2=None, op0=ALU.mult), reads=[po, rc], writes=[ao])

                def dense_block(m, s, q0, nq, key_tiles):
                    cfg = MIX[m]
                    nq_t = nq // 128
                    qt = qtb.next()
                    R = cfg["rows"]
                    sp.dma(qt[0:R, 0:(2 if m != "D" else 4), 0:nq],
                           QT[m].ap()[s, :, :, q0:q0 + nq].rearrange("b p t -> p b t"), reads=[tQKV[m][s]], writes=[qt])
                    ao = aos.next()
                    nk = len(key_tiles)
                    assert nk % 2 == 0
                    alias_sync(True)
                    items = [(hd, 2 * p, key_tiles[2 * p], key_tiles[2 * p + 1]) for hd in heads_of(m) for p in range(nk // 2)]
                    cur = {}

                    def stage_s(it):
                        (qb_i, kb_i, r0, nr, vh, oh), ki, kt0, kt1 = it
                        ps_ = pSD.next()
                        for u, kt in enumerate((kt0, kt1)):
                            pe.op(lambda e: e.matmul(ps_[:, u * 512:u * 512 + nq], ktb[r0:r0 + nr, kb_i, kt * 128:(kt + 1) * 128],
                                                     qt[r0:r0 + nr, qb_i, 0:nq], start=True, stop=True),
                                  reads=[ktb, qt], writes=[ps_], acc=True)
                        pt_ = ptb.next()
                        act.op(lambda e: e.activation(out=pt_[:, :].rearrange("p (u q) -> p u q", u=2)[:, :, 0:nq],
                                                      in_=ps_[:, :].rearrange("p (u q) -> p u q", u=2)[:, :, 0:nq],
                                                      func=AF.Exp, scale=cfg["scale"]),
                               reads=[ps_], writes=[pt_])
                        return pt_

                    def stage_pv(it, pt_):
                        (qb_i, kb_i, r0, nr, vh, oh), ki, kt0, kt1 = it
                        if ki == 0:
                            cur["pos"] = [pO.next() for _ in range(nq_t)]
                        pos = cur["pos"]
                        for u, kt in enumerate((kt0, kt1)):
                            for t in range(nq_t):
                                pe.op(lambda e: e.matmul(pos[t][:, 0:65], pt_[:, u * 512 + t * 128:u * 512 + (t + 1) * 128],
                                                         vtb[:, kt, vh * 65:(vh + 1) * 65],
                                                         start=(ki + u == 0), stop=(ki + u == nk - 1)),
                                      reads=[pt_, vtb], writes=[pos[t]], acc=True)
                        if ki + 2 == nk:
                            finish_head(pos, ao, oh, sink_h=(oh if m == "C" else None))

                    pend = []
                    for it in items:
                        pend.append((it, stage_s(it)))
                        if len(pend) > 1:
                            stage_pv(*pend.pop(0))
                    while pend:
                        stage_pv(*pend.pop(0))
                    g0 = s * TS + q0
                    sp.dma(AO.ap()[g0:g0 + nq, cfg["col"]:cfg["col"] + 256].rearrange("(t p) c -> p t c", p=128),
                           ao[:, 0:nq_t, :], reads=[ao], writes=[tAO[g0 // 128 + t] for t in range(nq_t)])

                pend_l = []

                def push_l(fn):
                    pend_l.append(fn)
                    if len(pend_l) > 1:
                        pend_l.pop(0)()

                def flush_l():
                    while pend_l:
                        pend_l.pop(0)()

                def load_kv(m, s):
                    flush_l()
                    cfg = MIX[m]
                    R, nb = cfg["rows"], cfg["nb"]
                    sp.dma(ktb[0:R, 0:nb, :], KT[m].ap()[s].rearrange("b p t -> p b t"), reads=[tQKV[m][s]], writes=[ktb])
                    w = cfg["nvh"] * 65
                    sp.dma(vtb[:, :, 0:w], VV[m].ap()[s].rearrange("(t p) c -> p t c", p=128), reads=[tQKV[m][s]], writes=[vtb])

                def local_tile_A(s, i):
                    alias_sync(False)
                    klist = keysA[i]
                    nl = len(klist)
                    qt = qtb.next()
                    sp.dma(qt[:, 0:2, 0:128], QT["A"].ap()[s, :, :, i * 128:(i + 1) * 128].rearrange("b p t -> p b t"),
                           reads=[tQKV["A"][s]], writes=[qt])
                    if 2 <= i <= 29:
                        bt, bsl = biasI, [vidA[(i, j)] for j in klist]
                    else:
                        bt = biasE.next()
                        v0 = vidA[(i, klist[0])]
                        sp.dma(bt[:, 0:nl], nab.ap()[l, v0:v0 + nl].rearrange("v k h q -> k v h q"), writes=[bt])
                        bsl = list(range(nl))
                    ao = aos.next()
                    for (qb_i, kb_i, r0, nr, vh, oh) in heads_of("A"):
                        psx, psy = pS.next(), pS.next()
                        slots = [(psx, k_) for k_ in range(min(nl, 4))] + ([(psy, 0)] if nl == 5 else [])
                        cslots = [(psy, 1), (psy, 2)]
                        for (p_, sl), kt in zip(slots + cslots, klist + [32, 33]):
                            pe.op(lambda e: e.matmul(p_[:, sl * 128:(sl + 1) * 128], ktb[r0:r0 + nr, kb_i, kt * 128:(kt + 1) * 128],
                                                     qt[r0:r0 + nr, qb_i, 0:128], start=True, stop=True),
                                  reads=[ktb, qt], writes=[p_], acc=True)
                        sb_ = ssb.next()
                        n1 = min(nl, 4)
                        assert bsl[:n1] == list(range(bsl[0], bsl[0] + n1))
                        dve.op(lambda e: e.tensor_tensor(out=sb_[:, 0:n1, :], in0=psx[:, 0:n1 * 128].rearrange("p (k q) -> p k q", k=n1),
                                                         in1=bt[:, bsl[0]:bsl[0] + n1, oh, :], op=ALU.add),
                               reads=[psx, bt], writes=[sb_])
                        if nl == 5:
                            dve.op(lambda e: e.tensor_tensor(out=sb_[:, 4, :], in0=psy[:, 0:128], in1=bt[:, bsl[4], oh, :], op=ALU.add),
                                   reads=[psy, bt], writes=[sb_])
                        pw = ptw.next()
                        act.op(lambda e: e.activation(out=pw[:, 0:nl, :], in_=sb_[:, 0:nl, :], func=AF.Exp), reads=[sb_], writes=[pw])
                        act.op(lambda e: e.activation(out=pw[:, 5:7, :], in_=psy[:, 128:384].rearrange("p (k q) -> p k q", k=2), func=AF.Exp),
                               reads=[psy], writes=[pw])
                        plist = [(k_, kt) for k_, kt in enumerate(klist)] + [(5, 32), (6, 33)]

                        def stage2(pw=pw, vh=vh, oh=oh, plist=plist, ao=ao, lastp=(oh == 3)):
                            po = pO.next()
                            for n_, (k_, kt) in enumerate(plist):
                                pe.op(lambda e: e.matmul(po[:, 0:65], pw[:, k_, :], vtb[:, kt, vh * 65:(vh + 1) * 65],
                                                         start=(n_ == 0), stop=(n_ == len(plist) - 1)),
                                      reads=[pw, vtb], writes=[po], acc=True)
                            finish_head([po], ao, oh)
                            if lastp:
                                g0 = s * TS + i * 128
                                sp.dma(AO.ap()[g0:g0 + 128, 0:256], ao[:, 0, :], reads=[ao], writes=[tAO[g0 // 128]])

                        push_l(stage2)

                def local_tile_C(s, i):
                    alias_sync(False)
                    qt = qtb.next()
                    sp.dma(qt[:, 0:2, 0:128], QT["C"].ap()[s, :, :, i * 128:(i + 1) * 128].rearrange("b p t -> p b t"),
                           reads=[tQKV["C"][s]], writes=[qt])
                    ao = aos.next()
                    loc = [(0, i - 1)] * (i > 0) + [(1, i)] + [(2, i + 1)] * (i < 31)
                    for (qb_i, kb_i, r0, nr, vh, oh) in heads_of("C"):
                        psx, psy = pS.next(), pS.next()
                        for (p_, sl, kt) in [(psx, sl, kt) for sl, kt in loc] + [(psy, 0, 32), (psy, 1, 33)]:
                            pe.op(lambda e: e.matmul(p_[:, sl * 128:(sl + 1) * 128], ktb[r0:r0 + nr, kb_i, kt * 128:(kt + 1) * 128],
                                                     qt[r0:r0 + nr, qb_i, 0:128], start=True, stop=True),
                                  reads=[ktb, qt], writes=[p_], acc=True)
                        sb_ = ssb.next()
                        for sl, kt in loc:
                            if sl == 1:
                                continue
                            mi = 0 if sl == 0 else 1
                            dve.op(lambda e: e.tensor_tensor(out=sb_[:, sl, :], in0=psx[:, sl * 128:(sl + 1) * 128], in1=wm[:, mi, :], op=ALU.add),
                                   reads=[psx, wm], writes=[sb_])
                        pw = ptw.next()
                        for sl, kt in loc:
                            if sl == 1:
                                act.op(lambda e: e.activation(out=pw[:, 1, :], in_=psx[:, 128:256], func=AF.Exp, scale=0.125),
                                       reads=[psx], writes=[pw])
                            else:
                                act.op(lambda e: e.activation(out=pw[:, sl, :], in_=sb_[:, sl, :], func=AF.Exp, scale=0.125),
                                       reads=[sb_], writes=[pw])
                        act.op(lambda e: e.activation(out=pw[:, 3:5, :], in_=psy[:, 0:256].rearrange("p (k q) -> p k q", k=2), func=AF.Exp, scale=0.125),
                               reads=[psy], writes=[pw])
                        plist = [(sl, kt) for sl, kt in loc] + [(3, 32), (4, 33)]

                        def stage2(pw=pw, vh=vh, oh=oh, plist=plist, ao=ao, lastp=(oh == 3)):
                            po = pO.next()
                            for n_, (k_, kt) in enumerate(plist):
                                pe.op(lambda e: e.matmul(po[:, 0:65], pw[:, k_, :], vtb[:, kt, vh * 65:(vh + 1) * 65],
                                                         start=(n_ == 0), stop=(n_ == len(plist) - 1)),
                                      reads=[pw, vtb], writes=[po], acc=True)
                            finish_head([po], ao, oh, sink_h=oh)
                            if lastp:
                                g0 = s * TS + i * 128
                                sp.dma(AO.ap()[g0:g0 + 128, 512:768], ao[:, 0, :], reads=[ao], writes=[tAO[g0 // 128]])

                        push_l(stage2)

                for s in range(NS):
                    for m in ("A", "C", "B", "D"):
                        if m in os.environ.get("SKIP_MIX", ""):
                            continue
                        load_kv(m, s)
                        if m == "A":
                            for i in range(32):
                                local_tile_A(s, i)
                        elif m == "C":
                            for i in range(32):
                                local_tile_C(s, i)
                        else:
                            for qb in range(8):
                                dense_block(m, s, qb * 512, 512, list(range(NT)))
                        flush_l()
                        if with_ctx:
                            dense_block(m, s, TL, CT, [32, 33])

            if stop_after == "p2" and l == stop_layer:
                break
            if "AO" in dbg_out and l == 0:
                for g8 in range(0, NTOK // 128, 4):
                    sp.dma(dbg_out["AO"].ap()[g8 * 128:(g8 + 4) * 128, :], AO.ap()[g8 * 128:(g8 + 4) * 128, :],
                           reads=[tAO[g8 + t] for t in range(4)], writes=[tY])
                fw.barrier()
            tiles3 = [(s, j) for s in range(NS) for j in range(NT if with_ctx else 32)]
            with (Scope(fw) if "3" not in SKP else _Skip()) as sc:
                woutb = sc.sb("woutb", [128, 8, D], BF16)
                pool.dma(woutb[:], w_out.ap()[l].rearrange("(c p) n -> p c n", p=128), writes=[woutb])
                wr = sc.sb("wr", [128, 8, 20], F32)
                sp.dma(wr[:, :, 0:4], rg_w.ap()[l].rearrange("(c p) n -> p c n", p=128), writes=[wr])
                sp.dma(wr[:, :, 4:20], re_w.ap()[l].rearrange("(c p) n -> p c n", p=128), writes=[wr])
                rb = sc.sb("rb", [128, 20], F32)
                sp.dma(rb[:, 0:4], bcast_ap(rg_b, l * 4, 4), writes=[rb])
                sp.dma(rb[:, 4:20], bcast_ap(re_b, l * 16, 16), writes=[rb])
                bc_g1 = [sc.sb(f"bc_g1{i}", [128, D], F32) for i in range(3)]
                bc_sc2 = [sc.sb(f"bc_sc2{i}", [128, D], F32) for i in range(3)]
                bc_sh2 = [sc.sb(f"bc_sh2{i}", [128, D], F32) for i in range(3)]
                for src in range(3):
                    sp.dma(bc_g1[src][:], bcast_ap(MOD, mod_off(l, src, 2), D), reads=[tMOD[l]], writes=[bc_g1[src]])
                    sp.dma(bc_sh2[src][:], bcast_ap(MOD, mod_off(l, src, 3), D), reads=[tMOD[l]], writes=[bc_sh2[src]])
                    sp.dma(bc_sc2[src][:], bcast_ap(MOD, mod_off(l, src, 4), D), reads=[tMOD[l]], writes=[bc_sc2[src]])
                bc_lg = sc.sb("bc_lg", [128, D], F32)
                bc_lb = sc.sb("bc_lb", [128, D], F32)
                sp.dma(bc_lg[:], bcast_ap(ln1_g, l * D, D), writes=[bc_lg])
                sp.dma(bc_lb[:], bcast_ap(ln1_b, l * D, D), writes=[bc_lb])
                aoin = Rot([sc.sb(f"aoin{i}", [128, D], BF16) for i in range(4)])
                xin = Rot([sc.sb(f"xin3_{i}", [128, D], F32) for i in range(4)])
                aoT = sc.sb("aoT", [128, 8, 128], BF16)
                tA = sc.sb("tA3", [128, D], F32)
                tB = sc.sb("tB3", [128, D], F32)
                x1 = Rot([sc.sb(f"x1_{i}", [128, D], F32) for i in range(4)])
                h2 = sc.sb("h2", [128, D], F32)
                h2b = sc.sb("h2b", [128, D], BF16)
                h2Ts = Rot([sc.sb(f"h2Ts{i}", [128, 8, 128], BF16) for i in range(4)])
                h2Tf = sc.sb("h2Tf", [128, 8, 128], F32)
                stats = sc.sb("stats3", [128, 2, 6], F32)
                mv = sc.sb("mv3", [128, 2], F32)
                rstd = sc.sb("rstd3", [128, 1], F32)
                lg = sc.sb("lg", [128, 20], F32)
                r1 = sc.sb("r1", [128, 16], F32)
                r2 = sc.sb("r2", [128, 16], F32)
                oh1 = sc.sb("oh1", [128, 16], F32)
                oh2 = sc.sb("oh2", [128, 16], F32)
                sm = sc.sb("sm", [128, 8], F32)
                pT = Rot([sc.ps(f"pT3_{i}", [128, 8, 128], BF16) for i in range(2)])
                pM = sc.ps("pM", [128, D], F32)
                pF = [sc.ps(f"pF{i}", [128, 4, 128], F32) for i in range(2)]
                pL = sc.ps("pL", [128, 32], F32)

                def p3_load(s, j):
                    g = s * NT + j
                    a = aoin.next()
                    sp.dma(a[:], AO.ap()[g * 128:(g + 1) * 128, :], reads=[tAO[g]], writes=[a])
                    xt = xin.next()
                    sp.dma(xt[:], X.ap()[g * 128:(g + 1) * 128, :], reads=[tX[g]], writes=[xt])
                    return a, xt

                names3 = [aoT, tA, tB, h2, h2b, h2Tf, stats, mv, rstd, lg, r1, r2, oh1, oh2, sm]
                shapes3 = [([128, 8, 128], BF16), ([128, D], F32), ([128, D], F32), ([128, D], F32), ([128, D], BF16), ([128, 8, 128], F32),
                           ([128, 2, 6], F32), ([128, 2], F32), ([128, 1], F32), ([128, 20], F32), ([128, 16], F32), ([128, 16], F32),
                           ([128, 16], F32), ([128, 16], F32), ([128, 8], F32)]
                B2 = [names3, [sc.sb(f"s1b_{k}", shp, dt) for k, (shp, dt) in enumerate(shapes3)]]

                def tile_gen(s, j, a, xt, B):
                    aoT, tA, tB, h2, h2b, h2Tf, stats, mv, rstd, lg, r1, r2, oh1, oh2, sm = B
                    g = s * NT + j
                    src = s if j < 32 else 2
                    pt = pT.next()
                    transposes(a, D, 128, pt, identb)
                    act.op(lambda e: e.copy(out=aoT[:], in_=pt[:]), reads=[pt], writes=[aoT])
                    for half in range(2):
                        for k in range(8):
                            pe.op(lambda e: e.matmul(pM[:, half * 512:(half + 1) * 512], aoT[:, k, :], woutb[:, k, half * 512:(half + 1) * 512],
                                                     start=(k == 0), stop=(k == 7)), reads=[aoT, woutb], writes=[pM], acc=True)
                    dve.op(lambda e: e.tensor_tensor(out=tA[:], in0=pM[:], in1=bc_g1[src][:], op=ALU.mult),
                           reads=[pM, bc_g1[src]], writes=[tA])
                    yield
                    dve.op(lambda e: e.scalar_tensor_tensor(out=tA[:], in0=xt[:], scalar=ALPHA, in1=tA[:], op0=ALU.mult, op1=ALU.add),
                            reads=[xt, tA], writes=[tA])
                    layer_norm(tA, tB, stats, mv, rstd)
                    pool.op(lambda e: e.tensor_tensor(out=tB[:], in0=tB[:], in1=bc_lg[:], op=ALU.mult), reads=[tB, bc_lg], writes=[tB])
                    xo = x1.next()
                    dve.op(lambda e: e.tensor_tensor(out=xo[:], in0=tB[:], in1=bc_lb[:], op=ALU.add), reads=[tB, bc_lb], writes=[xo])
                    sp.dma(X.ap()[g * 128:(g + 1) * 128, :], xo[:], reads=[xo], writes=[tX[g]])
                    if ("x1", ) and "x1" in dbg_out and j < 32:
                        sp.dma(dbg_out["x1"].ap()[(s * 32 + j) * 128:(s * 32 + j + 1) * 128, :], xo[:], reads=[xo], writes=[tY])
                    yield
                    layer_norm(xo, tA, stats, mv, rstd)
                    pool.op(lambda e: e.tensor_tensor(out=tA[:], in0=tA[:], in1=bc_sc2[src][:], op=ALU.mult),
                            reads=[tA, bc_sc2[src]], writes=[tA])
                    dve.op(lambda e: e.tensor_tensor(out=h2[:], in0=tA[:], in1=bc_sh2[src][:], op=ALU.add),
                           reads=[tA, bc_sh2[src]], writes=[h2])
                    act.op(lambda e: e.copy(out=h2b[:], in_=h2[:]), reads=[h2], writes=[h2b])
                    yield
                    pt = pT.next()
                    transposes(h2b, D, 128, pt, identb)
                    hs = h2Ts.next()
                    act.op(lambda e: e.copy(out=hs[:], in_=pt[:]), reads=[pt], writes=[hs])
                    sp.dma(H2T.ap()[:, :, g * 128:(g + 1) * 128], hs[:], reads=[hs], writes=[tH2T[g]])
                    yield
                    for c in range(8):
                        pe.op(lambda e: e.transpose(pF[c // 4][:, c % 4, :], h2[:, c * 128:(c + 1) * 128], identf[:]),
                              reads=[h2, identf], writes=[pF[c // 4]], acc=True)
                    for hf in range(2):
                        dve.op(lambda e: e.tensor_copy(out=h2Tf[:, hf * 4:(hf + 1) * 4, :], in_=pF[hf][:]), reads=[pF[hf]], writes=[h2Tf])
                    yield
                    for k in range(8):
                        pe.op(lambda e: e.matmul(pL[:, 0:20], h2Tf[:, k, :], wr[:, k, :], start=(k == 0), stop=(k == 7)),
                              reads=[h2Tf, wr], writes=[pL], acc=True)
                    dve.op(lambda e: e.tensor_tensor(out=lg[:], in0=pL[:, 0:20], in1=rb[:], op=ALU.add), reads=[pL, rb], writes=[lg])
                    yield
                    dve.op(lambda e: e.tensor_reduce(out=sm[:, 0:1], in_=lg[:, 0:4], axis=AX.X, op=ALU.max), reads=[lg], writes=[sm])
                    dve.op(lambda e: e.tensor_scalar(out=r1[:, 0:4], in0=lg[:, 0:4], scalar1=sm[:, 0:1], scalar2=None, op0=ALU.subtract),
                           reads=[lg, sm], writes=[r1])
                    act.op(lambda e: e.activation(out=r2[:, 0:4], in_=r1[:, 0:4], func=AF.Exp, accum_out=sm[:, 1:2]),
                           reads=[r1], writes=[r2, sm])
                    dve.op(lambda e: e.reciprocal(out=sm[:, 2:3], in_=sm[:, 1:2]), reads=[sm], writes=[sm])
                    dve.op(lambda e: e.tensor_scalar(out=r1[:, 4:8], in0=r1[:, 0:4], scalar1=0.0, scalar2=-1.0e9, op0=ALU.is_lt, op1=ALU.mult),
                           reads=[r1], writes=[r1])
                    dve.op(lambda e: e.tensor_tensor(out=r2[:].rearrange("p (g e) -> p g e", g=4), in0=lg[:, 4:20].rearrange("p (g e) -> p g e", g=4),
                                                     in1=r1[:, 4:8].unsqueeze(2).broadcast_to([128, 4, 4]), op=ALU.add),
                           reads=[lg, r1], writes=[r2])
                    dve.op(lambda e: e.tensor_reduce(out=sm[:, 3:4], in_=r2[:], axis=AX.X, op=ALU.max), reads=[r2], writes=[sm])
                    dve.op(lambda e: e.tensor_scalar(out=oh1[:], in0=r2[:], scalar1=sm[:, 3:4], scalar2=None, op0=ALU.is_equal),
                           reads=[r2, sm], writes=[oh1])
                    yield
                    dve.op(lambda e: e.scalar_tensor_tensor(out=r1[:], in0=oh1[:], scalar=-1.0e9, in1=r2[:], op0=ALU.mult, op1=ALU.add),
                           reads=[oh1, r2], writes=[r1])
                    dve.op(lambda e: e.tensor_reduce(out=sm[:, 4:5], in_=r1[:], axis=AX.X, op=ALU.max), reads=[r1], writes=[sm])
                    dve.op(lambda e: e.tensor_scalar(out=oh2[:], in0=r1[:], scalar1=sm[:, 4:5], scalar2=None, op0=ALU.is_equal),
                           reads=[r1, sm], writes=[oh2])
                    dve.op(lambda e: e.tensor_tensor(out=sm[:, 5:6], in0=sm[:, 4:5], in1=sm[:, 3:4], op=ALU.subtract), reads=[sm], writes=[sm])
                    act.op(lambda e: e.activation(out=sm[:, 5:6], in_=sm[:, 5:6], func=AF.Exp), reads=[sm], writes=[sm])
                    dve.op(lambda e: e.tensor_scalar_add(out=sm[:, 5:6], in0=sm[:, 5:6], scalar1=1.0), reads=[sm], writes=[sm])
                    dve.op(lambda e: e.reciprocal(out=sm[:, 5:6], in_=sm[:, 5:6]), reads=[sm], writes=[sm])
                    dve.op(lambda e: e.tensor_scalar(out=sm[:, 6:7], in0=sm[:, 5:6], scalar1=-1.0, scalar2=1.0, op0=ALU.mult, op1=ALU.add),
                           reads=[sm], writes=[sm])
                    dve.op(lambda e: e.tensor_scalar(out=sm[:, 5:7], in0=sm[:, 5:7], scalar1=sm[:, 2:3], scalar2=None, op0=ALU.mult),
                           reads=[sm], writes=[sm])
                    dve.op(lambda e: e.tensor_scalar(out=oh1[:], in0=oh1[:], scalar1=sm[:, 5:6], scalar2=None, op0=ALU.mult),
                           reads=[oh1, sm], writes=[oh1])
                    dve.op(lambda e: e.scalar_tensor_tensor(out=gate_all[:, g, :], in0=oh2[:], scalar=sm[:, 6:7], in1=oh1[:], op0=ALU.mult, op1=ALU.add),
                           reads=[oh2, sm, oh1], writes=[gate_all])

                pairs = [tiles3[i:i + 2] for i in range(0, len(tiles3), 2)]
                nxt = [p3_load(*t) for t in pairs[0]]
                for pi, pr in enumerate(pairs):
                    curl = nxt
                    if pi + 1 < len(pairs):
                        nxt = [p3_load(*t) for t in pairs[pi + 1]]
                    gens = [tile_gen(t[0], t[1], ld[0], ld[1], B2[k]) for k, (t, ld) in enumerate(zip(pr, curl))]
                    while gens:
                        for gq in list(gens):
                            try:
                                next(gq)
                            except StopIteration:
                                gens.remove(gq)

            if stop_after == "p3" and l == stop_layer:
                break
            blocks = []
            for b in range(4):
                s, j0 = b // 2, (b % 2) * 16
                blocks.append([(s, j0 + t) for t in range(16)])
            if with_ctx:
                blocks.append([(0, 32), (0, 33), (1, 32), (1, 33)])
            with (Scope(fw) if "4" not in SKP else _Skip()) as sc:
                h2t = sc.sb("h2t", [128, 8, 2048], BF16)
                yacc = sc.sb("yacc", [128, 16, D], F32)
                wg = Rot([sc.sb(f"wg{i}", [128, 8, 512], BF16) for i in range(2)])
                wu = Rot([sc.sb(f"wu{i}", [128, 8, 512], BF16) for i in range(2)])
                wd = Rot([sc.sb(f"wd{i}", [128, 4, D], BF16) for i in range(2)])
                sg = Rot([sc.sb(f"sg{i}", [128, 512], BF16) for i in range(2)])
                am = Rot([sc.sb(f"am{i}", [128, 4, 512], BF16) for i in range(2)])
                bc_g2 = [sc.sb(f"bc_g2{i}", [128, D], F32) for i in range(3)]
                for src in range(3):
                    sp.dma(bc_g2[src][:], bcast_ap(MOD, mod_off(l, src, 5), D), reads=[tMOD[l]], writes=[bc_g2[src]])
                bc_lg = sc.sb("bc_lg2", [128, D], F32)
                bc_lb = sc.sb("bc_lb2", [128, D], F32)
                sp.dma(bc_lg[:], bcast_ap(ln2_g, l * D, D), writes=[bc_lg])
                sp.dma(bc_lb[:], bcast_ap(ln2_b, l * D, D), writes=[bc_lb])
                xin = Rot([sc.sb(f"xin4_{i}", [128, D], F32) for i in range(1)])
                tA = sc.sb("tA4", [128, D], F32)
                xo4 = Rot([sc.sb(f"xo4_{i}", [128, D], F32) for i in range(1)])
                stats = sc.sb("stats4", [128, 2, 6], F32)
                mv = sc.sb("mv4", [128, 2], F32)
                rstd = sc.sb("rstd4", [128, 1], F32)
                pG = Rot([sc.ps(f"pG{i}", [128, 512], F32) for i in range(2)])
                pU = Rot([sc.ps(f"pU{i}", [128, 512], F32) for i in range(2)])
                pY = Rot([sc.ps(f"pY{i}", [128, 512], F32) for i in range(4)])

                def load_w(e):
                    a, b_, c_ = wg.next(), wu.next(), wd.next()
                    pool.dma(a[:], mw_gate.ap()[l, e].rearrange("(c p) n -> p c n", p=128), writes=[a])
                    pool.dma(b_[:], mw_up.ap()[l, e].rearrange("(c p) n -> p c n", p=128), writes=[b_])
                    pool.dma(c_[:], mw_down.ap()[l, e].rearrange("(c p) n -> p c n", p=128), writes=[c_])
                    return a, b_, c_

                for blk in blocks:
                    nt_b = len(blk)
                    runs = []
                    for (s, j) in blk:
                        g = s * NT + j
                        if runs and runs[-1][0] + runs[-1][1] == g:
                            runs[-1][1] += 1
                        else:
                            runs.append([g, 1])
                    off = 0
                    for g0, n in runs:
                        sp.dma(h2t[:, :, off * 128:(off + n) * 128], H2T.ap()[:, :, g0 * 128:(g0 + n) * 128],
                               reads=[tH2T[g] for g in range(g0, g0 + n)], writes=[h2t])
                        off += n
                    gl = [s * NT + j for (s, j) in blk]
                    wts = {0: load_w(0), 1: load_w(1)}
                    subs = list(range(0, nt_b, 4))

                    def stage_a(e_, sb0):
                        wgt, wut, _ = wts[e_]
                        nsub = min(4, nt_b - sb0)
                        ntk = nsub * 128
                        at = am.next()
                        for f in range(4):
                            pg, pu = pG.next(), pU.next()
                            for k in range(8):
                                pe.op(lambda e: e.matmul(pg[:, 0:ntk], wgt[:, k, f * 128:(f + 1) * 128], h2t[:, k, sb0 * 128:sb0 * 128 + ntk],
                                                         start=(k == 0), stop=(k == 7)), reads=[wgt, h2t], writes=[pg], acc=True)
                            for k in range(8):
                                pe.op(lambda e: e.matmul(pu[:, 0:ntk], wut[:, k, f * 128:(f + 1) * 128], h2t[:, k, sb0 * 128:sb0 * 128 + ntk],
                                                         start=(k == 0), stop=(k == 7)), reads=[wut, h2t], writes=[pu], acc=True)
                            sgt = sg.next()
                            act.op(lambda e: e.activation(out=sgt[:, 0:ntk], in_=pg[:, 0:ntk], func=AF.Silu), reads=[pg], writes=[sgt])
                            dve.op(lambda e: e.tensor_tensor(out=at[:, f, 0:ntk], in0=sgt[:, 0:ntk], in1=pu[:, 0:ntk], op=ALU.mult),
                                   reads=[sgt, pu], writes=[at])
                        return at

                    def stage_b(e_, sb0, at):
                        wdt = wts[e_][2]
                        nsub = min(4, nt_b - sb0)
                        for t in range(nsub):
                            ti = sb0 + t
                            for half in range(2):
                                py = pY.next()
                                for f in range(4):
                                    pe.op(lambda e: e.matmul(py[:], at[:, f, t * 128:(t + 1) * 128], wdt[:, f, half * 512:(half + 1) * 512],
                                                             start=(f == 0), stop=(f == 3)), reads=[at, wdt], writes=[py], acc=True)
                                ya = yacc[:, ti, half * 512:(half + 1) * 512]
                                gcol = gate_all[:, gl[ti], e_:e_ + 1]
                                if e_ == 0:
                                    dve.op(lambda e: e.tensor_scalar(out=ya, in0=py[:], scalar1=gcol, scalar2=None, op0=ALU.mult),
                                           reads=[py, gate_all], writes=[yacc])
                                else:
                                    dve.op(lambda e: e.scalar_tensor_tensor(out=ya, in0=py[:], scalar=gcol, in1=ya, op0=ALU.mult, op1=ALU.add),
                                           reads=[py, gate_all, yacc], writes=[yacc])
                        if sb0 == subs[-1] and e_ + 2 < 16:
                            wts[e_ + 2] = load_w(e_ + 2)

                    pend = None
                    for e_ in range(16):
                        for sb0 in subs:
                            at = stage_a(e_, sb0)
                            if pend is not None:
                                stage_b(*pend)
                            pend = (e_, sb0, at)
                    stage_b(*pend)
                    for ti, (s, j) in enumerate(blk):
                        g = s * NT + j
                        src = s if j < 32 else 2
                        xt = xin.next()
                        sp.dma(xt[:], X.ap()[g * 128:(g + 1) * 128, :], reads=[tX[g]], writes=[xt])
                        pool.op(lambda e: e.tensor_tensor(out=tA[:], in0=yacc[:, ti, :], in1=bc_g2[src][:], op=ALU.mult),
                                reads=[yacc, bc_g2[src]], writes=[tA])
                        dve.op(lambda e: e.scalar_tensor_tensor(out=tA[:], in0=xt[:], scalar=ALPHA, in1=tA[:], op0=ALU.mult, op1=ALU.add),
                               reads=[xt, tA], writes=[tA])
                        layer_norm(tA, tA, stats, mv, rstd)
                        pool.op(lambda e: e.tensor_tensor(out=tA[:], in0=tA[:], in1=bc_lg[:], op=ALU.mult), reads=[tA, bc_lg], writes=[tA])
                        xo = xo4.next()
                        dve.op(lambda e: e.tensor_tensor(out=xo[:], in0=tA[:], in1=bc_lb[:], op=ALU.add), reads=[tA, bc_lb], writes=[xo])
                        if single == "mid":
                            sp.dma(y_out.ap()[g * 128:(g + 1) * 128, :], xo[:], reads=[xo], writes=[tY])
                        elif l == n_layers - 1:
                            if j < 32:
                                r = (s * 32 + j) * 128
                                sp.dma(y_out.ap()[r:r + 128, :], xo[:], reads=[xo], writes=[tY])
                        else:
                            sp.dma(X.ap()[g * 128:(g + 1) * 128, :], xo[:], reads=[xo], writes=[tX[g]])

        fw.barrier()
    return nc


_CONSTS = None


def _consts():
    global _CONSTS
    if _CONSTS is None:
        C64, S64 = _rope_tables(64)
        C32, S32 = _rope_tables(32)
        kk = np.arange(128)
        wmask = np.zeros((128, 2, 128), np.float32)
        wmask[:, 0, :] = np.where(kk[:, None] >= kk[None, :], 0.0, NEG)
        wmask[:, 1, :] = np.where(kk[:, None] <= kk[None, :], 0.0, NEG)
        _CONSTS = dict(ident=np.eye(128, dtype=np.float32), ropeC64=C64, ropeS64=S64, ropeC32=C32, ropeS32=S32, wmask=wmask)
    return _CONSTS


_PER_LAYER = ("w_ada", "b_ada", "w_in", "gqa_q_gain", "gqa_k_gain", "win_sink", "mla_q_gain",
              "mla_w_qb", "mla_kv_gain", "mla_w_kvb", "w_out", "ln1_g", "ln1_b",
              "router_group_w", "router_group_b", "router_expert_w", "router_expert_b",
              "moe_w_gate", "moe_w_up", "moe_w_down", "ln2_g", "ln2_b")


def _f32(a):
    return np.ascontiguousarray(np.asarray(a, dtype=np.float32))


def make_in_maps(inputs, n_cores=8, layer=None, xcur=None):
    sl = slice(None) if layer is None else slice(layer, layer + 1)
    shared = {k: _f32(inputs[k])[sl] for k in _PER_LAYER}
    shared["nab"] = _na_bias_tables(_f32(inputs["na_bias"]))[sl]
    shared.update(_consts())
    c, c_ctx = _f32(inputs["c"]), _f32(inputs["c_ctx"])
    if xcur is None:
        x, ctx = _f32(inputs["x"]), _f32(inputs["ctx"])
    maps = []
    for i in range(n_cores):
        b0 = NS * i
        if xcur is None:
            xc = np.concatenate([np.concatenate([x[b0 + s], ctx[b0 + s]], 0) for s in range(NS)], 0)
        else:
            xc = xcur[i]
        c3 = np.stack([c[b0], c[b0 + 1], c_ctx], 0)
        c3T = np.ascontiguousarray(c3.reshape(3, 8, 128).transpose(2, 1, 0))
        m = dict(shared)
        m["x_in"] = np.ascontiguousarray(xc)
        m["c3T"] = c3T
        maps.append(m)
    return maps


_PROGS = {}


def _prog(kind):
    if kind not in _PROGS:
        _PROGS[kind] = build(single=kind)
    return _PROGS[kind]


def kernel_unfused(**inputs):
    xcur = None
    for l in range(DEPTH):
        kind = "last" if l == DEPTH - 1 else "mid"
        maps = make_in_maps(inputs, 8, layer=l, xcur=xcur)
        res = run_bass_kernel_spmd(_prog(kind), maps, core_ids=list(range(8)))
        xcur = [np.asarray(r["y"]) for r in res.results]
    out = np.concatenate([r.reshape(NS, TL, D) for r in xcur], 0)
    return out.astype(np.float32)


def kernel(**inputs):
    maps = make_in_maps(inputs, 8)
    res = run_bass_kernel_spmd(_prog(None), maps, core_ids=list(range(8)))
    out = np.concatenate([np.asarray(r["y"]).reshape(NS, TL, D) for r in res.results], 0)
    return out.astype(np.float32)
```
